# Optimizing a Trainium2 kernel written in Bass

```python
import math
import jax
import jax.numpy as jnp
from jax import lax
import numpy as np

D_MODEL = 2048
BATCH = 2
SEQ = 8192
DEPTH = 4

N_MEM = 256
N_BRANCHES = 4
BRANCH_WIDTH = D_MODEL // 2
POOL_WIDTH = BRANCH_WIDTH
POOL_WINDOWS = (2, 4, 8, 16)
N_POOL_GROUPS = len(POOL_WINDOWS)
POOL_GROUP_DIM = POOL_WIDTH // N_POOL_GROUPS
RET_WIDTH = BRANCH_WIDTH
N_RET_HEADS = 8
RET_HEAD_DIM = RET_WIDTH // N_RET_HEADS
RET_CHUNK = 128
ROPE_BASE = 10000.0
SGU_WIDTH = BRANCH_WIDTH
SGU_CHUNK = 128
N_SGU_GROUPS = 4
SGU_GROUP_DIM = SGU_WIDTH // N_SGU_GROUPS
MEM_WIDTH = BRANCH_WIDTH
N_MEM_HEADS = 4
MEM_HEAD_DIM = MEM_WIDTH // N_MEM_HEADS
IN_SPLITS = (POOL_WIDTH, RET_WIDTH, RET_WIDTH, RET_WIDTH, RET_WIDTH, SGU_WIDTH, SGU_WIDTH, MEM_WIDTH)
IN_WIDTH = sum(IN_SPLITS)
N_EXPERTS = 32
TOP_K = 4
EXPERT_DIM = 3 * D_MODEL // 8
SWIGLU_LIMIT = 7.0
SWIGLU_ALPHA = 1.702
MOE_BLOCK = 256
LN_EPS = 1e-5
DEEPNORM_ALPHA = (2 * DEPTH) ** 0.25
DEEPNORM_BETA = (8 * DEPTH) ** -0.25

kernel_name = 'hybrid_pool_retention_sgu_memory_moe_encoder'


def layer_norm(x, g, b):
    xf = x.astype(jnp.float32)
    mu = jnp.mean(xf, axis=-1, keepdims=True)
    var = jnp.mean(jnp.square(xf - mu), axis=-1, keepdims=True)
    return ((xf - mu) * lax.rsqrt(var + LN_EPS) * g.astype(jnp.float32) + b.astype(jnp.float32)).astype(x.dtype)


def head_norm(y, g):
    B, S, H, d = y.shape
    yf = y.astype(jnp.float32)
    mu = jnp.mean(yf, axis=-1, keepdims=True)
    var = jnp.mean(jnp.square(yf - mu), axis=-1, keepdims=True)
    yn = ((yf - mu) * lax.rsqrt(var + LN_EPS)).reshape(B, S, H * d)
    return (yn * g.astype(jnp.float32)).astype(y.dtype)


def rotary_tables(seq, dim, dtype):
    pos = jnp.arange(seq, dtype=jnp.float32)
    inv_freq = jnp.exp(-math.log(ROPE_BASE) * jnp.arange(0, dim, 2, dtype=jnp.float32) / dim)
    ang = pos[:, None] * inv_freq[None, :]
    return jnp.cos(ang).astype(dtype), jnp.sin(ang).astype(dtype)


def rotary(x, cos, sin):
    x1, x2 = jnp.split(x, 2, axis=-1)
    c = cos[None, :, None, :]
    s = sin[None, :, None, :]
    return jnp.concatenate([x1 * c - x2 * s, x2 * c + x1 * s], axis=-1)


def pool_mixer(a, pool_w, pool_scale):
    B, S, _ = a.shape
    ag = a.reshape(B, S, N_POOL_GROUPS, POOL_GROUP_DIM)
    cs = jnp.cumsum(ag.astype(jnp.float32), axis=1)
    cs = jnp.pad(cs, ((0, 0), (1, 0), (0, 0), (0, 0)))
    pos = jnp.arange(S)
    pooled = []
    for gi, w in enumerate(POOL_WINDOWS):
        lo = jnp.clip(pos - w // 2, 0, S)
        hi = jnp.clip(pos + w // 2, 0, S)
        csg = cs[:, :, gi]
        cnt = (hi - lo).astype(jnp.float32)[None, :, None]
        pooled.append((csg[:, hi] - csg[:, lo]) / cnt)
    pooled = jnp.stack(pooled, axis=2)
    mixed = (pooled - ag.astype(jnp.float32)).astype(a.dtype)
    y = jnp.einsum('bsgc,gce->bsge', mixed, pool_w)
    return y.reshape(B, S, POOL_WIDTH) * pool_scale


def retention_scan(q, k, v, log_gamma, include_diag):
    B, H, S, d = q.shape
    C = RET_CHUNK
    n = S // C
    f32 = jnp.float32
    qc, kc, vc = (t.reshape(B, H, n, C, d).astype(f32) for t in (q, k, v))
    lg = log_gamma.astype(f32)
    idx = jnp.arange(C, dtype=f32)
    diff = idx[:, None] - idx[None, :]
    mask = (diff >= 0) if include_diag else (diff > 0)
    decay = jnp.where(mask[None], jnp.exp(lg[:, None, None] * jnp.maximum(diff, 0.0)[None]), 0.0)
    scores = jnp.einsum('bhncd,bhnmd->bhncm', qc, kc) * decay[None, :, None]
    intra = jnp.einsum('bhncm,bhnme->bhnce', scores, vc)
    k_w = jnp.exp(lg[:, None] * (C - 1.0 - idx)[None])
    q_w = jnp.exp(lg[:, None] * (idx + 1.0)[None])
    chunk_kv = jnp.einsum('bhncd,bhnce->nbhde', kc * k_w[None, :, None, :, None], vc)
    chunk_decay = jnp.exp(lg * C)[None, :, None, None]

    def step(state, kv):
        return state * chunk_decay + kv, state

    _, prev = lax.scan(step, jnp.zeros((B, H, d, d), f32), chunk_kv)
    cross = jnp.einsum('bhncd,nbhde->bhnce', qc, prev) * q_w[None, :, None, :, None]
    return (intra + cross).reshape(B, H, S, d).astype(q.dtype)


def retention_branch(q, k, v, g, log_gamma, gn_g, cos, sin):
    B, S, _ = q.shape
    H, d = N_RET_HEADS, RET_HEAD_DIM
    qh = rotary(q.reshape(B, S, H, d), cos, sin)
    kh = rotary(k.reshape(B, S, H, d), cos, sin) * (d ** -0.5)
    vh = v.reshape(B, S, H, d)
    qh, kh, vh = (jnp.swapaxes(t, 1, 2) for t in (qh, kh, vh))
    fwd = retention_scan(qh, kh, vh, log_gamma[0], True)
    rq, rk, rv = (jnp.flip(t, axis=2) for t in (qh, kh, vh))
    bwd = jnp.flip(retention_scan(rq, rk, rv, log_gamma[1], False), axis=2)
    y = jnp.swapaxes(fwd + bwd, 1, 2)
    return head_norm(y, gn_g) * jax.nn.silu(g)


def spatial_gating(u, v, ln_g, ln_b, w_s, b_s):
    B, S, _ = v.shape
    vn = layer_norm(v, ln_g, ln_b)
    vg = vn.reshape(B, S // SGU_CHUNK, SGU_CHUNK, N_SGU_GROUPS, SGU_GROUP_DIM)
    mixed = jnp.einsum('gpq,bnqgc->bnpgc', w_s, vg) + jnp.swapaxes(b_s, 0, 1)[:, :, None]
    return u * mixed.reshape(B, S, SGU_WIDTH)


def memory_attention(q, mem, w_mem_kv):
    B, S, _ = q.shape
    M = mem.shape[1]
    k, v = jnp.split(jnp.dot(mem, w_mem_kv), 2, axis=-1)
    qh = q.reshape(B, S, N_MEM_HEADS, MEM_HEAD_DIM)
    kh = k.reshape(B, M, N_MEM_HEADS, MEM_HEAD_DIM)
    vh = v.reshape(B, M, N_MEM_HEADS, MEM_HEAD_DIM)
    s = jnp.einsum('bshd,bmhd->bhsm', qh, kh).astype(jnp.float32) * (MEM_HEAD_DIM ** -0.5)
    p = jax.nn.softmax(s, axis=-1).astype(vh.dtype)
    o = jnp.einsum('bhsm,bmhd->bshd', p, vh)
    return o.reshape(B, S, MEM_WIDTH)


def mixer_sublayer(h, mem, w_in, pool_w, pool_scale, ret_log_gamma, ret_gn_g, sgu_ln_g, sgu_ln_b,
                   sgu_w, sgu_b, w_mem_kv, w_branch, w_gate, b_gate, w_out, cos, sin):
    z = jnp.dot(h, w_in)
    cuts = np.cumsum(IN_SPLITS)[:-1].tolist()
    a, rq, rk, rv, rg, su, sv, mq = jnp.split(z, cuts, axis=-1)
    branches = (
        pool_mixer(a, pool_w, pool_scale),
        retention_branch(rq, rk, rv, rg, ret_log_gamma, ret_gn_g, cos, sin),
        spatial_gating(jax.nn.gelu(su), jax.nn.gelu(sv), sgu_ln_g, sgu_ln_b, sgu_w, sgu_b),
        memory_attention(mq, mem, w_mem_kv),
    )
    merged = None
    for bi, y in enumerate(branches):
        gate = jax.nn.sigmoid(jnp.dot(h, w_gate[bi]) + b_gate[bi])
        term = gate * jnp.dot(y, w_branch[bi])
        merged = term if merged is None else merged + term
    return jnp.dot(merged, w_out)


def clamped_swiglu(gu):
    x_glu, x_lin = jnp.split(gu, 2, axis=-1)
    x_glu = jnp.minimum(x_glu, SWIGLU_LIMIT)
    x_lin = jnp.clip(x_lin, -SWIGLU_LIMIT, SWIGLU_LIMIT)
    return x_glu * jax.nn.sigmoid(SWIGLU_ALPHA * x_glu) * (x_lin + 1.0)


def moe_ffn(h, w_router, b_router, w_up, b_up, w_down, b_down):
    T, D = h.shape
    A = T * TOP_K
    n_blocks = -(-(A + N_EXPERTS * (MOE_BLOCK - 1)) // MOE_BLOCK)
    logits = jnp.dot(h, w_router).astype(jnp.float32) + b_router.astype(jnp.float32)
    top_logits, top_e = lax.top_k(logits, TOP_K)
    gates = jax.nn.softmax(top_logits, axis=-1)
    flat_e = top_e.reshape(A)
    flat_tok = jnp.repeat(jnp.arange(T, dtype=jnp.int32), TOP_K)
    flat_gate = gates.reshape(A)
    order = jnp.argsort(flat_e)
    sorted_e = flat_e[order]
    counts = jnp.bincount(flat_e, length=N_EXPERTS)
    padded = (counts + MOE_BLOCK - 1) // MOE_BLOCK * MOE_BLOCK
    pad_end = jnp.cumsum(padded)
    pad_start = pad_end - padded
    start = jnp.cumsum(counts) - counts
    dest = pad_start[sorted_e] + jnp.arange(A, dtype=jnp.int32) - start[sorted_e]
    n_slots = n_blocks * MOE_BLOCK
    slot_tok = jnp.full((n_slots,), T, jnp.int32).at[dest].set(flat_tok[order])
    slot_gate = jnp.zeros((n_slots,), jnp.float32).at[dest].set(flat_gate[order])
    block_e = jnp.minimum(jnp.searchsorted(pad_end, jnp.arange(n_blocks, dtype=jnp.int32) * MOE_BLOCK, side='right'),
                          N_EXPERTS - 1)
    h_pad = jnp.concatenate([h, jnp.zeros((1, D), h.dtype)], axis=0)

    def block_step(acc, blk):
        tok, gate, e = blk
        gu = jnp.dot(h_pad[tok], w_up[e]) + b_up[e]
        y = jnp.dot(clamped_swiglu(gu), w_down[e]) + b_down[e]
        return acc.at[tok].add(y.astype(jnp.float32) * gate[:, None]), None

    acc, _ = lax.scan(block_step, jnp.zeros((T + 1, D), jnp.float32),
                      (slot_tok.reshape(n_blocks, MOE_BLOCK), slot_gate.reshape(n_blocks, MOE_BLOCK), block_e))
    return acc[:T].astype(h.dtype)


def setup_inputs(seed: int = 0) -> dict:
    key = jax.random.key(seed)
    ks = jax.random.split(key, 32)
    f32 = jnp.float32
    L, D = DEPTH, D_MODEL

    def nrm(k, shape, scale):
        return jax.random.normal(k, shape, f32) * scale

    base_log_gamma = jnp.log1p(-jnp.exp2(-5.0 - jnp.arange(N_RET_HEADS, dtype=f32)))
    w_mem_kv = jnp.concatenate([nrm(ks[13], (L, D, MEM_WIDTH), D ** -0.5),
                                nrm(ks[14], (L, D, MEM_WIDTH), D ** -0.5 * DEEPNORM_BETA)], axis=-1)
    return {
        'x': nrm(ks[0], (BATCH, SEQ, D), 1.0),
        'mem': nrm(ks[1], (BATCH, N_MEM, D), 1.0),
        'ln_in_g': 1.0 + nrm(ks[2], (D,), 0.02),
        'ln_in_b': nrm(ks[3], (D,), 0.02),
        'w_in': nrm(ks[4], (L, D, IN_WIDTH), D ** -0.5),
        'pool_w': nrm(ks[5], (L, N_POOL_GROUPS, POOL_GROUP_DIM, POOL_GROUP_DIM), POOL_GROUP_DIM ** -0.5),
        'pool_scale': 1.0 + nrm(ks[6], (L, POOL_WIDTH), 0.1),
        'ret_log_gamma': base_log_gamma[None, None, :] * jnp.exp(nrm(ks[7], (L, 2, N_RET_HEADS), 0.1)),
        'ret_gn_g': 1.0 + nrm(ks[8], (L, RET_WIDTH), 0.02),
        'sgu_ln_g': 1.0 + nrm(ks[9], (L, SGU_WIDTH), 0.02),
        'sgu_ln_b': nrm(ks[10], (L, SGU_WIDTH), 0.02),
        'sgu_w': nrm(ks[11], (L, N_SGU_GROUPS, SGU_CHUNK, SGU_CHUNK), 0.5 * SGU_CHUNK ** -0.5),
        'sgu_b': 1.0 + nrm(ks[12], (L, N_SGU_GROUPS, SGU_CHUNK), 0.1),
        'w_mem_kv': w_mem_kv,
        'w_branch': nrm(ks[15], (L, N_BRANCHES, BRANCH_WIDTH, D), BRANCH_WIDTH ** -0.5 * DEEPNORM_BETA),
        'w_gate': nrm(ks[16], (L, N_BRANCHES, D, D), D ** -0.5),
        'b_gate': nrm(ks[17], (L, N_BRANCHES, D), 0.02),
        'w_out': nrm(ks[18], (L, D, D), D ** -0.5 * DEEPNORM_BETA),
        'ln1_g': 1.0 + nrm(ks[19], (L, D), 0.02),
        'ln1_b': nrm(ks[20], (L, D), 0.02),
        'w_router': nrm(ks[21], (L, D, N_EXPERTS), D ** -0.5),
        'b_router': nrm(ks[22], (L, N_EXPERTS), 0.01),
        'w_up': nrm(ks[23], (L, N_EXPERTS, D, 2 * EXPERT_DIM), D ** -0.5),
        'b_up': nrm(ks[24], (L, N_EXPERTS, 2 * EXPERT_DIM), 0.02),
        'w_down': nrm(ks[25], (L, N_EXPERTS, EXPERT_DIM, D), EXPERT_DIM ** -0.5 * DEEPNORM_BETA),
        'b_down': nrm(ks[26], (L, N_EXPERTS, D), 0.02),
        'ln2_g': 1.0 + nrm(ks[27], (L, D), 0.02),
        'ln2_b': nrm(ks[28], (L, D), 0.02),
    }


def reference(x, mem, ln_in_g, ln_in_b, w_in, pool_w, pool_scale, ret_log_gamma, ret_gn_g, sgu_ln_g, sgu_ln_b,
              sgu_w, sgu_b, w_mem_kv, w_branch, w_gate, b_gate, w_out, ln1_g, ln1_b, w_router, b_router,
              w_up, b_up, w_down, b_down, ln2_g, ln2_b):
    B, S, D = x.shape
    cos, sin = rotary_tables(S, RET_HEAD_DIM, x.dtype)
    h = layer_norm(x, ln_in_g, ln_in_b)
    for l in range(DEPTH):
        mix = mixer_sublayer(h, mem, w_in[l], pool_w[l], pool_scale[l], ret_log_gamma[l], ret_gn_g[l],
                             sgu_ln_g[l], sgu_ln_b[l], sgu_w[l], sgu_b[l], w_mem_kv[l], w_branch[l],
                             w_gate[l], b_gate[l], w_out[l], cos, sin)
        h = layer_norm(DEEPNORM_ALPHA * h + mix, ln1_g[l], ln1_b[l])
        ffn = moe_ffn(h.reshape(B * S, D), w_router[l], b_router[l], w_up[l], b_up[l], w_down[l], b_down[l])
        h = layer_norm(DEEPNORM_ALPHA * h + ffn.reshape(B, S, D), ln2_g[l], ln2_b[l])
    return h
```

```python
import math
from contextlib import ExitStack
import numpy as np
import concourse.bass as bass
import concourse.mybir as mybir
from concourse.bass_utils import run_bass_kernel_spmd

F32 = mybir.dt.float32
BF16 = mybir.dt.bfloat16
AF = mybir.ActivationFunctionType
ALU = mybir.AluOpType
AX = mybir.AxisListType

D = 2048
T = 2048
NTT = 4
NCH = 16
KC = 16
SEQ = 8192
DEPTH = 4
NEXP = 32
EDIM = 768
LN_EPS = 1e-5
ALPHA = (2 * DEPTH) ** 0.25
SAME_ENGINE_SYNC = True
N_DSEM = 24
POOL_TO_DVE = True
NOCOLL = False
LNIN_LIMIT = None


class Op:
    __slots__ = ("eng", "fn", "deps", "dma", "idx", "needs_inc", "ticket", "dsem", "dcount", "n_inc")

    def __init__(self, eng, fn, dma):
        self.eng = eng
        self.fn = fn
        self.dma = dma
        self.deps = set()
        self.needs_inc = False
        self.ticket = 0
        self.dsem = -1
        self.dcount = 0
        self.n_inc = 16


class Sched:
    ENGS = ("pe", "act", "dve", "pool", "sp")

    def __init__(self, nc):
        self.nc = nc
        self.ops = []
        self.kw = {}
        self.kr = {}
        self.dsem_last = [None] * (N_DSEM + 1)
        self.dsem_cnt = [0] * (N_DSEM + 1)
        self.dsem_rr = 0
        self.dsem_rr_sw = 0
        self.last_op = {}
        self.pending_dma = []
        self.bar = {}

    def barrier(self):
        deps = set(self.last_op.values()) | set(self.pending_dma)
        for e in self.ENGS:
            self.bar[e] = set(deps) | self.bar.get(e, set())
        self.pending_dma = []

    limit = None

    def op(self, eng, fn, reads=(), writes=(), dma=False, n_inc=16):
        if self.limit is not None:
            if self.limit <= 0:
                return None
            self.limit -= 1
        if eng == "pool" and not dma and POOL_TO_DVE:
            eng = "dve"
        o = Op(eng, fn, dma)
        o.n_inc = n_inc
        o.idx = len(self.ops)
        deps = o.deps
        ops = self.ops

        def keep(d, war):
            a = ops[d]
            if a.dma or dma:
                return True
            if a.eng == eng:
                if eng in ("pe", "sp") or not SAME_ENGINE_SYNC:
                    return False
            return True

        for k in reads:
            w = self.kw.get(k)
            if w is not None and keep(w, False):
                deps.add(w)
        for k in writes:
            w = self.kw.get(k)
            if w is not None and keep(w, False):
                deps.add(w)
            for r in self.kr.get(k, ()):
                if keep(r, True):
                    deps.add(r)
        for k in reads:
            lst = self.kr.setdefault(k, [])
            if not dma:
                for i_, r_ in enumerate(lst):
                    if (not ops[r_].dma) and ops[r_].eng == eng:
                        lst[i_] = o.idx
                        break
                else:
                    lst.append(o.idx)
            else:
                lst.append(o.idx)
        for k in writes:
            self.kw[k] = o.idx
            self.kr[k] = []
        b = self.bar.pop(eng, None)
        if b:
            for d in b:
                a = ops[d]
                if a.dma or a.eng != eng:
                    deps.add(d)
        if dma:
            if n_inc == 1:
                s = N_DSEM
            elif eng == "pool":
                s = N_DSEM - 8 + (self.dsem_rr_sw % 8)
                self.dsem_rr_sw += 1
            else:
                s = self.dsem_rr
                self.dsem_rr = (s + 1) % (N_DSEM - 8)
            prev = self.dsem_last[s]
            if prev is not None:
                deps.add(prev)
            self.dsem_cnt[s] += n_inc
            o.dsem = s
            o.dcount = self.dsem_cnt[s]
            self.dsem_last[s] = o.idx
            self.pending_dma.append(o.idx)
        else:
            self.last_op[eng] = o.idx
        deps.discard(o.idx)
        self.ops.append(o)
        return o

    def emit(self):
        nc = self.nc
        ops = self.ops
        for o in ops:
            for d in o.deps:
                if not ops[d].dma:
                    ops[d].needs_inc = True
        cnt = {e: 0 for e in self.ENGS}
        for o in ops:
            if not o.dma and o.needs_inc:
                cnt[o.eng] += 1
                o.ticket = cnt[o.eng]
        with ExitStack() as st:
            esem = {e: st.enter_context(nc.semaphore("s_" + e)) for e in self.ENGS}
            dsem = [st.enter_context(nc.semaphore("d_%d" % i)) for i in range(N_DSEM + 1)]
            block = st.enter_context(nc.Block())
            per = {e: [o for o in ops if o.eng == e] for e in self.ENGS}

            def run(e, eng):
                waited = {}
                for o in per[e]:
                    need = {}
                    for d in o.deps:
                        a = ops[d]
                        if a.dma:
                            k = ("d", a.dsem)
                            v = a.dcount
                        else:
                            k = ("e", a.eng)
                            v = a.ticket
                        if v > need.get(k, 0):
                            need[k] = v
                    for k, v in need.items():
                        if waited.get(k, 0) >= v:
                            continue
                        waited[k] = v
                        sem = dsem[k[1]] if k[0] == "d" else esem[k[1]]
                        eng.wait_ge(sem, v)
                    ins = o.fn(eng)
                    if o.dma:
                        ins.then_inc(dsem[o.dsem], o.n_inc)
                    elif o.needs_inc:
                        ins.then_inc(esem[e], 1)
                if e == "sp":
                    for i in range(N_DSEM + 1):
                        if self.dsem_cnt[i] > 0:
                            eng.wait_ge(dsem[i], self.dsem_cnt[i])

            @block.sync
            def _(eng):
                run("sp", eng)

            @block.tensor
            def _(eng):
                run("pe", eng)

            @block.scalar
            def _(eng):
                run("act", eng)

            @block.vector
            def _(eng):
                run("dve", eng)

            @block.gpsimd
            def _(eng):
                run("pool", eng)


CT = {}
_off = 0
for _name, _w in (("ident", 128), ("ones", 128), ("rrot", 128), ("dpos", 128), ("dneg", 128), ("mge", 128),
                  ("mlt", 128), ("rowc1", 128), ("row128mc", 128), ("col127mc", 1), ("colc", 1),
                  ("colAf", 16), ("colAb", 16), ("xmult", 16), ("xmask", 16), ("selL", 8), ("selR", 8),
                  ("corr", 64)):
    CT[_name] = (_off, _w)
    _off += _w
NCT = _off


def make_ctab(core):
    t = np.zeros((128, NCT), np.float32)

    def put(name, arr):
        o, w = CT[name]
        t[:, o:o + w] = np.asarray(arr, np.float32).reshape(128, w)

    p = np.arange(128)
    put("ident", np.eye(128))
    put("ones", np.ones((128, 128)))
    rr = np.zeros((128, 128))
    for m in range(64):
        rr[m + 64, m] = -1.0
        rr[m, m + 64] = 1.0
    put("rrot", rr)
    mm_, cc_ = np.meshgrid(p, p, indexing="ij")
    s = 128.0 ** -0.5
    put("dpos", np.maximum(cc_ - mm_, 0))
    put("dneg", np.maximum(mm_ - cc_, 0))
    put("mge", (cc_ >= mm_) * s)
    put("mlt", (mm_ > cc_) * s)
    put("rowc1", np.tile((p + 1)[None, :], (128, 1)))
    put("row128mc", np.tile((128 - p)[None, :], (128, 1)))
    put("col127mc", 127 - p)
    put("colc", p)
    n = np.arange(16)
    put("colAf", 2047 - (n[None, :] * 128 + p[:, None]))
    put("colAb", n[None, :] * 128 + p[:, None])
    me = core % 4
    base = (core // 4) * 4
    xm = np.zeros(16)
    xk = np.zeros(16)
    for j in range(8):
        jl = j - base
        if 0 <= jl < 4 and jl < me:
            xm[j] = 2048.0 * (me - 1 - jl)
            xk[j] = 1.0
        if 0 <= jl < 4 and jl > me:
            xm[8 + j] = 2048.0 * (jl - me - 1)
            xk[8 + j] = 1.0
    put("xmult", np.tile(xm[None, :], (128, 1)))
    put("xmask", np.tile(xk[None, :], (128, 1)))
    sl = np.zeros(8)
    sr = np.zeros(8)
    if me > 0:
        sl[core - 1] = 1.0
    if me < 3:
        sr[core + 1] = 1.0
    put("selL", np.tile(sl[None, :], (128, 1)))
    put("selR", np.tile(sr[None, :], (128, 1)))
    corr = np.ones((4, 16))
    for gi, w in enumerate((2, 4, 8, 16)):
        for i in range(8):
            if me == 0:
                pos = i
                cnt = min(pos + w // 2, SEQ) - max(pos - w // 2, 0)
                corr[gi, i] = w / cnt
            if me == 3:
                pos = SEQ - 8 + i
                cnt = min(pos + w // 2, SEQ) - max(pos - w // 2, 0)
                corr[gi, 8 + i] = w / cnt
    put("corr", np.tile(corr.reshape(1, 64), (128, 1)))
    return t


def make_rot(core):
    me = core % 4
    pos = (me * 2048 + np.arange(2048)).astype(np.float32)
    inv = np.exp(-math.log(10000.0) * np.arange(0, 128, 2, dtype=np.float32) / 128).astype(np.float32)
    ang = pos[None, :] * inv[:, None]
    c = np.cos(ang).astype(np.float32)
    s = np.sin(ang).astype(np.float32)
    out = np.zeros((128, 2, 2048), np.float32)
    out[:64, 0] = c
    out[64:, 0] = c
    out[:64, 1] = s
    out[64:, 1] = s
    return out


WNAMES = ("w_in", "pool_w", "pool_scale", "ret_log_gamma", "ret_gn_g", "sgu_ln_g", "sgu_ln_b", "sgu_wT", "sgu_b",
          "w_mem_kv", "w_branch", "w_gate", "b_gate", "w_out", "ln1_g", "ln1_b", "w_router", "b_router",
          "w_up", "b_up", "w_down", "b_down", "ln2_g", "ln2_b")
WSHAPES = {
    "w_in": [D, 8192], "pool_w": [4, 256, 256], "pool_scale": [1024], "ret_log_gamma": [16], "ret_gn_g": [1024],
    "sgu_ln_g": [1024], "sgu_ln_b": [1024], "sgu_wT": [4, 128, 128], "sgu_b": [4, 128], "w_mem_kv": [D, 2048],
    "w_branch": [4, 1024, D], "w_gate": [4, D, D], "b_gate": [4, D], "w_out": [D, D], "ln1_g": [D], "ln1_b": [D],
    "w_router": [D, 32], "b_router": [32], "w_up": [32, D, 1536], "b_up": [32, 1536], "w_down": [32, 768, D],
    "b_down": [32, D], "ln2_g": [D], "ln2_b": [D],
}


class Builder:
    def __init__(self, nlayers, dbg=None, stages=None, mode="F", first=True, last=True):
        self.nlayers = nlayers
        self.dbg = dbg
        self.stages = stages
        self.mode, self.first, self.last = mode, first, last
        nc = self.nc = bass.Bass("TRN2", target_bir_lowering=False)
        self.S = Sched(nc)
        dt = nc.dram_tensor
        self.used_w = []
        self._rot_d = None
        bld = self

        class LazyW(dict):
            def __init__(self, l):
                super().__init__()
                self.l = l

            def __missing__(self, n):
                nm = "%s_%d" % (n, self.l)
                ap = dt(nm, WSHAPES[n], F32, kind="ExternalInput").ap()
                bld.used_w.append(nm)
                self[n] = ap
                return ap

        self.W = [LazyW(l) for l in range(nlayers)]
        self.ctab_d = dt("ctab", [128, NCT], F32, kind="ExternalInput").ap()
        if mode == "F" or (mode == "A" and first):
            self.x = dt("x", [T, D], F32, kind="ExternalInput").ap()
            self.ln_in = dt("ln_in", [2, D], F32, kind="ExternalInput").ap()
        if mode in ("F", "B"):
            self.mem = dt("mem", [256, D], F32, kind="ExternalInput").ap()
        if mode == "F" or (mode == "B" and last):
            self.out = dt("out", [T, D], F32, kind="ExternalOutput").ap()
        if dbg:
            self.dbg_out = dt("dbg", [KC, 128, T], F32, kind="ExternalOutput").ap()
        ko = "ExternalOutput"
        ki = "ExternalInput"
        if mode == "F":
            self.hres = dt("hres", [KC, 128, T], F32).ap()
            self.hTd = dt("hTd", [KC, 128, T], BF16).ap()
            self.aTd = dt("aTd", [8, 128, T], F32).ap()
            self.bounce = dt("bounce", [17 * 128, 128], F32).ap()
            self.gath = dt("gath", [8 * 17 * 128, 128], F32).ap()
        elif mode == "A":
            if first:
                self.hres = dt("hres_o", [KC, 128, T], F32, kind=ko).ap()
                self.hTd = dt("hTd_o", [KC, 128, T], BF16, kind=ko).ap()
            else:
                self.hTd = dt("hTd_i", [KC, 128, T], BF16, kind=ki).ap()
            self.aTd = dt("aTd_o", [8, 128, T], F32, kind=ko).ap()
            self.bounce = dt("bounce_o", [17 * 128, 128], F32, kind=ko).ap()
            self.gath = None
        else:
            self.hres_i = dt("hres_i", [KC, 128, T], F32, kind=ki).ap()
            self.hTd_i = dt("hTd_i", [KC, 128, T], BF16, kind=ki).ap()
            self.hres = dt("hres_o", [KC, 128, T], F32, kind=ko).ap()
            self.hTd = dt("hTd_o", [KC, 128, T], BF16, kind=ko).ap()
            self.aTd = dt("aTd_i", [8, 128, T], F32, kind=ki).ap()
            self.gath = dt("gath_i", [8 * 17 * 128, 128], F32, kind=ki).ap()
            self.bounce = None
        if mode != "A":
            self.yb = [dt("yb%d" % i, [8, 128, T], BF16).ap() for i in range(4)]
        self.psn = 0
        self.wsn = 0

    @property
    def rot_d(self):
        if self._rot_d is None:
            self._rot_d = self.nc.dram_tensor("rot", [128, 2, T], F32, kind="ExternalInput").ap()
            self.used_w.append("rot")
        return self._rot_d

    def un(self, name):
        self.uid = getattr(self, "uid", 0) + 1
        return "%s_u%d" % (name, self.uid)

    def next_ps(self):
        i = self.psn % len(self.ps)
        self.psn += 1
        return self.ps[i], ("ps", i)

    def next_psb(self):
        i = self.psn % len(self.psb)
        self.psn += 1
        return self.psb[i], ("psb", i)

    def ct(self, name):
        o, w = CT[name]
        return self.ctab[:, o:o + w]

    def bcast_load(self, st, name, vec_ap, n):
        t = st.enter_context(self.nc.sbuf_tensor(self.un(name), [128, n], F32))
        self.S.op("sp", lambda e: e.dma_start(out=t[:], in_=vec_ap.partition_broadcast(128)), writes=[name], dma=True)
        return t

    def col_load(self, st, name, vec_ap, nchunk):
        t = st.enter_context(self.nc.sbuf_tensor(self.un(name), [128, nchunk], F32))
        self.S.op("sp", lambda e: e.dma_start(out=t[:], in_=vec_ap.rearrange("(c p) -> p c", p=128), allow_slow_non_contiguous=True),
                  writes=[name], dma=True)
        return t

    def gemm(self, Wap, K, col0, ncols, in_fn, ntt, epi, slab=256):
        S = self.S
        kc_n = K // 128
        per = slab // 128
        for s in range(ncols // slab):
            si = self.wsn % len(self.wslots)
            self.wsn += 1
            slot = self.wslots[si]
            skey = ("wslot", si)
            src = Wap[:, col0 + s * slab: col0 + (s + 1) * slab].rearrange("(kc p) n -> p kc n", p=128)
            S.op("pool", lambda e, slot=slot, src=src: e.dma_start(out=slot[:, 0:kc_n, 0:slab], in_=src),
                 writes=[skey], dma=True)
            for jj in range(per):
                j = s * per + jj
                for t in range(ntt):
                    ps, pk = self.next_ps()
                    for kc in range(kc_n):
                        rhs, rkeys = in_fn(kc, t)
                        S.op("pe", lambda e, ps=ps, slot=slot, kc=kc, jj=jj, rhs=rhs: e.matmul(
                            ps[:], lhsT=slot[:, kc, jj * 128:(jj + 1) * 128], rhs=rhs, start=(kc == 0), stop=(kc == kc_n - 1)),
                            reads=[skey] + rkeys, writes=[pk])
                    epi(j, t, ps, pk)

    def load_hT(self, st):
        self.hT = st.enter_context(self.nc.sbuf_tensor(self.un("hT"), [128, KC, T], BF16))
        for kc in range(KC):
            self.S.op("sp", lambda e, kc=kc, hT=self.hT: e.dma_start(out=hT[:, kc, :], in_=self.hTd[kc]), reads=["hTd"],
                      writes=[("hT", kc, t) for t in range(NTT)], dma=True)

    def hT_in(self, kc, t):
        return self.hT[:, kc, t * 512:(t + 1) * 512], [("hT", kc, t)]

    def build(self):
        nc, S = self.nc, self.S
        with ExitStack() as st:
            sb = lambda name, shape, dt: st.enter_context(nc.sbuf_tensor(self.un(name), shape, dt))
            self.ctab = sb("ctab_sb", [128, NCT], F32)
            self.identb = sb("identb", [128, 128], BF16)
            self.onesb = sb("onesb", [128, 128], BF16)
            self.rrotb = sb("rrotb", [128, 128], BF16)
            self.wslots = [sb("wslot%d" % i, [128, 16, 256], BF16) for i in range(2)]
            self.ps = [st.enter_context(nc.psum_tensor("ps%d" % i, [128, 512], F32)) for i in range(6)]
            self.psb = [st.enter_context(nc.psum_tensor("psb%d" % i, [128, 1024], BF16)) for i in range(2)]
            S.op("sp", lambda e: e.dma_start(out=self.ctab[:], in_=self.ctab_d), writes=["ctab"], dma=True)
            S.op("dve", lambda e: e.tensor_copy(out=self.identb[:], in_=self.ct("ident")), reads=["ctab"], writes=["identb"])
            S.op("dve", lambda e: e.tensor_copy(out=self.onesb[:], in_=self.ct("ones")), reads=["ctab"], writes=["onesb"])
            S.op("dve", lambda e: e.tensor_copy(out=self.rrotb[:], in_=self.ct("rrot")), reads=["ctab"], writes=["rrotb"])
            sg = self.stages
            if self.mode == "F":
                if sg is None or "nolnin" not in sg:
                    self.stage_ln_in()
                for l in range(self.nlayers):
                    self.layer(l)
                if sg is None or "nooutput" not in sg:
                    self.stage_output()
            elif self.mode == "A":
                if self.first:
                    self.stage_ln_in()
                with ExitStack() as st_tab:
                    self.layer_tables(0, st_tab)
                    self.stage_pre_exchange(0, st_tab)
            else:
                S.op("sp", lambda e: e.dma_start(out=self.hres, in_=self.hres_i), writes=["hres"], dma=True)
                S.op("sp", lambda e: e.dma_start(out=self.hTd, in_=self.hTd_i), writes=["hTd"], dma=True)
                S.barrier()
                with ExitStack() as st_tab:
                    self.layer_tables(0, st_tab)
                    self.stage_sgu(0)
                    self.stage_mem(0)
                    self.stage_pool_b(0)
                    self.stage_ret_b(0)
                    self.stage_post(0)
                if self.last:
                    self.stage_output()
            S.barrier()
            S.emit()
        return nc

    def stage_ln_in(self):
        nc, S = self.nc, self.S
        S.barrier()
        S.limit = LNIN_LIMIT
        with ExitStack() as st:
            sb = lambda name, shape, dt: st.enter_context(nc.sbuf_tensor(self.un(name), shape, dt))
            self.hT = sb("hT_lnin", [128, KC, T], BF16)
            gt = self.bcast_load(st, "lnin_g", self.ln_in[0], D)
            bt = self.bcast_load(st, "lnin_b", self.ln_in[1], D)
            xt = [sb("lnin_x%d" % i, [128, D], F32) for i in range(2)]
            sq = sb("lnin_sq", [128, D], F32)
            hr = [sb("lnin_hr%d" % i, [128, KC, 128], F32) for i in range(2)]
            stat = [sb("lnin_st%d" % i, [128, 4], F32) for i in range(2)]
            for n in range(NCH):
                b = n % 2
                x_, s_, h_ = xt[b], stat[b], hr[b]
                xk, sk, hk = ("lnx", b), ("lnst", b), ("lnhr", b)
                S.op("sp", lambda e, x_=x_, n=n: e.dma_start(out=x_[:], in_=self.x[n * 128:(n + 1) * 128, :]), writes=[xk], dma=True)
                S.op("dve", lambda e, x_=x_, s_=s_: e.reduce_sum(out=s_[:, 0:1], in_=x_[:], axis=AX.X), reads=[xk], writes=[sk])
                S.op("dve", lambda e, s_=s_: e.tensor_scalar(out=s_[:, 1:2], in0=s_[:, 0:1], scalar1=1.0 / D, scalar2=None, op0=ALU.mult), reads=[sk], writes=[sk])
                S.op("dve", lambda e, x_=x_, s_=s_: e.tensor_scalar(out=x_[:], in0=x_[:], scalar1=s_[:, 1:2], scalar2=None, op0=ALU.subtract), reads=[xk, sk], writes=[xk])
                S.op("dve", lambda e, x_=x_: e.tensor_tensor(out=sq[:], in0=x_[:], in1=x_[:], op=ALU.mult), reads=[xk], writes=["lnsq"])
                S.op("dve", lambda e, s_=s_: e.reduce_sum(out=s_[:, 2:3], in_=sq[:], axis=AX.X), reads=["lnsq"], writes=[sk])
                S.op("dve", lambda e, s_=s_: e.tensor_scalar(out=s_[:, 2:3], in0=s_[:, 2:3], scalar1=1.0 / D, scalar2=LN_EPS, op0=ALU.mult, op1=ALU.add), reads=[sk], writes=[sk])
                S.op("act", lambda e, s_=s_: e.activation(out=s_[:, 3:4], in_=s_[:, 2:3], func=AF.Sqrt), reads=[sk], writes=[sk])
                S.op("dve", lambda e, s_=s_: e.reciprocal(out=s_[:, 3:4], in_=s_[:, 3:4]), reads=[sk], writes=[sk])
                S.op("dve", lambda e, x_=x_, s_=s_: e.tensor_scalar(out=x_[:], in0=x_[:], scalar1=s_[:, 3:4], scalar2=None, op0=ALU.mult), reads=[xk, sk], writes=[xk])
                S.op("dve", lambda e, x_=x_: e.tensor_tensor(out=x_[:], in0=x_[:], in1=gt[:], op=ALU.mult), reads=[xk, "lnin_g"], writes=[xk])
                S.op("dve", lambda e, x_=x_: e.tensor_tensor(out=x_[:], in0=x_[:], in1=bt[:], op=ALU.add), reads=[xk, "lnin_b"], writes=[xk])
                for k4 in range(4):
                    ps, pk = self.next_ps()
                    for i in range(4):
                        k = k4 * 4 + i
                        S.op("pe", lambda e, ps=ps, x_=x_, k=k, i=i: e.transpose(out=ps[:, i * 128:(i + 1) * 128], in_=x_[:, k * 128:(k + 1) * 128], identity=self.ct("ident")),
                             reads=[xk, "ctab"], writes=[pk])
                    for i in range(4):
                        S.op("act", lambda e, ps=ps, k4=k4, n=n, i=i, hT=self.hT: e.copy(out=hT[:, k4 * 4 + i, n * 128:(n + 1) * 128], in_=ps[:, i * 128:(i + 1) * 128]),
                             reads=[pk], writes=[("hT", k4 * 4 + i, n // 4)])
                    S.op("dve", lambda e, ps=ps, k4=k4, h_=h_: e.tensor_copy(out=h_[:, k4 * 4:k4 * 4 + 4, :], in_=ps[:].rearrange("p (a b) -> p a b", a=4)),
                         reads=[pk] + [("hT", k4 * 4 + i, n // 4) for i in range(4)], writes=[hk])
                S.op("sp", lambda e, h_=h_, n=n: e.dma_start(out=self.hres[:, :, n * 128:(n + 1) * 128].rearrange("k p t -> p k t"), in_=h_[:]),
                     reads=[hk], writes=["hres"], dma=True)
            for kc in range(KC):
                S.op("sp", lambda e, kc=kc, hT=self.hT: e.dma_start(out=self.hTd[kc], in_=hT[:, kc, :]), reads=[("hT", kc, t) for t in range(NTT)], writes=["hTd"], dma=True)
        S.limit = None
        S.barrier()

    def stage_mem_prep(self, st_outer):
        nc, S = self.nc, self.S
        self.memT = st_outer.enter_context(nc.sbuf_tensor(self.un("memT"), [128, KC, 256], BF16))
        with ExitStack() as st:
            sb = lambda name, shape, dt: st.enter_context(nc.sbuf_tensor(self.un(name), shape, dt))
            mf = sb("mem_f", [128, 2, D], F32)
            mb = sb("mem_b", [128, 2, D], BF16)
            S.op("sp", lambda e: e.dma_start(out=mf[:], in_=self.mem.rearrange("(c p) n -> p c n", p=128)), writes=["mem_f"], dma=True)
            S.op("dve", lambda e: e.tensor_copy(out=mb[:], in_=mf[:]), reads=["mem_f"], writes=["mem_b"])
            for c in range(2):
                for k8 in range(2):
                    ps, pk = self.next_psb()
                    for i in range(8):
                        k = k8 * 8 + i
                        S.op("pe", lambda e, ps=ps, c=c, k=k, i=i: e.transpose(out=ps[:, i * 128:(i + 1) * 128], in_=mb[:, c, k * 128:(k + 1) * 128], identity=self.identb[:]),
                             reads=["mem_b", "identb"], writes=[pk])
                    S.op("dve", lambda e, ps=ps, c=c, k8=k8, memT=self.memT: e.tensor_copy(out=memT[:, k8 * 8:k8 * 8 + 8, c * 128:(c + 1) * 128], in_=ps[:].rearrange("p (a b) -> p a b", a=8)),
                         reads=[pk], writes=["memT"])
        S.barrier()

    def dbg_dump_yb(self, bi):
        nc, S = self.nc, self.S
        S.barrier()
        with ExitStack() as st:
            a = st.enter_context(nc.sbuf_tensor("dbg_a", [128, 8, T], BF16))
            b = st.enter_context(nc.sbuf_tensor("dbg_b", [128, 8, T], F32))
            S.op("sp", lambda e: e.dma_start(out=a[:], in_=self.yb[bi].rearrange("k p t -> p k t")), writes=["dbg_a"], dma=True)
            S.op("dve", lambda e: e.tensor_copy(out=b[:], in_=a[:]), reads=["dbg_a"], writes=["dbg_b"])
            S.op("sp", lambda e: e.dma_start(out=self.dbg_out[0:8].rearrange("k p t -> p k t"), in_=b[:]), reads=["dbg_b"], writes=["dbg"], dma=True)
        S.barrier()

    def stage_output(self):
        nc, S = self.nc, self.S
        S.barrier()
        with ExitStack() as st:
            sb = lambda name, shape, dt: st.enter_context(nc.sbuf_tensor(self.un(name), shape, dt))
            hin = [sb("o_in%d" % i, [128, KC, 128], F32) for i in range(2)]
            ot = [sb("o_t%d" % i, [128, D], F32) for i in range(2)]
            for n in range(NCH):
                b = n % 2
                S.op("sp", lambda e, b=b, n=n: e.dma_start(out=hin[b][:], in_=self.hres[:, :, n * 128:(n + 1) * 128].rearrange("k p t -> p k t")),
                     reads=["hres"], writes=[("o_in", b)], dma=True)
                for k4 in range(4):
                    ps, pk = self.next_ps()
                    for i in range(4):
                        k = k4 * 4 + i
                        S.op("pe", lambda e, ps=ps, b=b, k=k, i=i: e.transpose(out=ps[:, i * 128:(i + 1) * 128], in_=hin[b][:, k, :], identity=self.ct("ident")),
                             reads=[("o_in", b), "ctab"], writes=[pk])
                    S.op("dve" if k4 % 2 == 0 else "act",
                         (lambda e, ps=ps, b=b, k4=k4: e.tensor_copy(out=ot[b][:, k4 * 512:(k4 + 1) * 512], in_=ps[:])) if k4 % 2 == 0 else
                         (lambda e, ps=ps, b=b, k4=k4: e.copy(out=ot[b][:, k4 * 512:(k4 + 1) * 512], in_=ps[:])),
                         reads=[pk], writes=[("o_t", b)])
                S.op("sp", lambda e, b=b, n=n: e.dma_start(out=self.out[n * 128:(n + 1) * 128, :], in_=ot[b][:]), reads=[("o_t", b)], writes=["out"], dma=True)
        S.barrier()

    def layer(self, l):
        stg = self.stages
        with ExitStack() as st_tab:
            self._layer(l, st_tab)

    def _layer(self, l, st_tab):
        stg = self.stages
        self.S.barrier()
        if stg is None or "pre" in stg:
            self.layer_tables(l, st_tab)
            self.stage_pre_exchange(l, st_tab)
        if stg is None or "sgu" in stg:
            self.stage_sgu(l)
        if stg is None or "mem" in stg:
            self.stage_mem(l)
        if stg is None or "poolb" in stg:
            self.stage_pool_b(l)
        if stg is None or "retb" in stg:
            self.stage_ret_b(l)
        if stg is None or "post" in stg:
            self.stage_post(l)
        if self.dbg and self.dbg.startswith("yb") and l == 0:
            self.dbg_dump_yb(int(self.dbg[2]))

    def gelu_epi(self, ps, pk, out_ap, out_keys, tmps):
        S = self.S
        i = self.psn % len(tmps)
        tm, tk = tmps[i], ("gelu_tmp", i)
        S.op("act", lambda e: e.activation(out=tm[:], in_=ps[:], func=AF.Square), reads=[pk], writes=[tk])
        S.op("dve", lambda e: e.tensor_scalar(out=tm[:], in0=tm[:], scalar1=0.044715, scalar2=1.0, op0=ALU.mult, op1=ALU.add), reads=[tk], writes=[tk])
        S.op("dve", lambda e: e.tensor_tensor(out=tm[:], in0=tm[:], in1=ps[:], op=ALU.mult), reads=[tk, pk], writes=[tk])
        S.op("act", lambda e: e.activation(out=tm[:], in_=tm[:], func=AF.Sigmoid, scale=1.5957691216057308), reads=[tk], writes=[tk])
        S.op("dve", lambda e: e.tensor_tensor(out=out_ap, in0=tm[:], in1=ps[:], op=ALU.mult), reads=[tk, pk], writes=out_keys)

    def stage_sgu(self, l):
        nc, S, W = self.nc, self.S, self.W[l]
        S.barrier()
        with ExitStack() as st:
            sb = lambda name, shape, dt: st.enter_context(nc.sbuf_tensor(self.un(name), shape, dt))
            self.load_hT(st)
            guT = sb("sgu_gu", [128, 8, T], BF16)
            gvT = sb("sgu_gv", [128, 8, T], BF16)
            lng = self.col_load(st, "sgu_lng", W["sgu_ln_g"], 8)
            lnb = self.col_load(st, "sgu_lnb", W["sgu_ln_b"], 8)
            bsb = self.bcast_load(st, "sgu_bs", W["sgu_b"].rearrange("g p -> (g p)"), 512)
            wsf = sb("sgu_wsf", [128, 4, 128], F32)
            wsb = sb("sgu_wsb", [128, 4, 128], BF16)
            S.op("sp", lambda e: e.dma_start(out=wsf[:], in_=W["sgu_wT"].rearrange("g q p -> q g p")), writes=["sgu_wsf"], dma=True)
            S.op("dve", lambda e: e.tensor_copy(out=wsb[:], in_=wsf[:]), reads=["sgu_wsf"], writes=["sgu_wsb"])
            tmps = [sb("sgu_tmp%d" % i, [128, 512], F32) for i in range(3)]

            def epi_u(j, t, ps, pk):
                self.gelu_epi(ps, pk, guT[:, j, t * 512:(t + 1) * 512], [("sgu_gu", j, t)], tmps)

            def epi_v(j, t, ps, pk):
                self.gelu_epi(ps, pk, gvT[:, j, t * 512:(t + 1) * 512], [("sgu_gv", j, t)], tmps)

            self.gemm(W["w_in"], D, 5120, 1024, self.hT_in, NTT, epi_u)
            self.gemm(W["w_in"], D, 6144, 1024, self.hT_in, NTT, epi_v)

            sqb = [sb("sgu_sq%d" % i, [128, 512], BF16) for i in range(2)]
            mean = sb("sgu_mean", [128, 512], F32)
            rstd = sb("sgu_rstd", [128, 512], F32)
            vtmp = tmps[0:2]
            vn = sb("sgu_vn", [128, 8, 512], BF16)
            vtm = sb("sgu_vtm", [128, 4, 1024], BF16)
            ot = [sb("sgu_o%d" % i, [128, 8, 512], BF16) for i in range(1)]
            otmp = vtmp
            for t in range(NTT):
                tsl = slice(t * 512, (t + 1) * 512)
                ps_s, pk_s = self.next_ps()
                for c in range(8):
                    S.op("pe", lambda e, tsl=tsl, c=c, ps_s=ps_s: e.matmul(ps_s[:], lhsT=self.onesb[:], rhs=gvT[:, c, tsl], start=(c == 0), stop=(c == 7)),
                         reads=["onesb", ("sgu_gv", c, t)], writes=[pk_s])
                ps_q, pk_q = self.next_ps()
                for c in range(8):
                    q_, qk = sqb[c % 2], ("sgu_sq", c % 2)
                    S.op("dve", lambda e, tsl=tsl, c=c, q_=q_: e.tensor_tensor(out=q_[:], in0=gvT[:, c, tsl], in1=gvT[:, c, tsl], op=ALU.mult),
                         reads=[("sgu_gv", c, t)], writes=[qk])
                    S.op("pe", lambda e, c=c, q_=q_, ps_q=ps_q: e.matmul(ps_q[:], lhsT=self.onesb[:], rhs=q_[:], start=(c == 0), stop=(c == 7)),
                         reads=["onesb", qk], writes=[pk_q])
                S.op("act", lambda e, ps_s=ps_s: e.mul(out=mean[:], in_=ps_s[:], mul=1.0 / 1024), reads=[pk_s], writes=["sgu_mean"])
                S.op("dve", lambda e: e.tensor_tensor(out=rstd[:], in0=mean[:], in1=mean[:], op=ALU.mult), reads=["sgu_mean"], writes=["sgu_rstd"])
                S.op("dve", lambda e, ps_q=ps_q: e.scalar_tensor_tensor(out=rstd[:], in0=ps_q[:], scalar=1.0 / 1024, in1=rstd[:], op0=ALU.mult, op1=ALU.subtract),
                     reads=[pk_q, "sgu_rstd"], writes=["sgu_rstd"])
                S.op("dve", lambda e: e.tensor_scalar(out=rstd[:], in0=rstd[:], scalar1=LN_EPS, scalar2=None, op0=ALU.add), reads=["sgu_rstd"], writes=["sgu_rstd"])
                S.op("act", lambda e: e.activation(out=rstd[:], in_=rstd[:], func=AF.Sqrt), reads=["sgu_rstd"], writes=["sgu_rstd"])
                S.op("dve", lambda e: e.reciprocal(out=rstd[:], in_=rstd[:]), reads=["sgu_rstd"], writes=["sgu_rstd"])
                for c in range(8):
                    v_, vk = vtmp[c % 2], ("gelu_tmp", c % 2)
                    S.op("dve", lambda e, tsl=tsl, c=c, v_=v_: e.tensor_tensor(out=v_[:], in0=gvT[:, c, tsl], in1=mean[:], op=ALU.subtract),
                         reads=[("sgu_gv", c, t), "sgu_mean"], writes=[vk])
                    S.op("dve", lambda e, v_=v_: e.tensor_tensor(out=v_[:], in0=v_[:], in1=rstd[:], op=ALU.mult), reads=[vk, "sgu_rstd"], writes=[vk])
                    S.op("act", lambda e, c=c, v_=v_: e.activation(out=vn[:, c, :], in_=v_[:], func=AF.Identity, scale=lng[:, c:c + 1], bias=lnb[:, c:c + 1]),
                         reads=[vk, "sgu_lng", "sgu_lnb"], writes=[("sgu_vn", c)])
                for n4 in range(4):
                    pb, pbk = self.next_psb()
                    for c in range(8):
                        S.op("pe", lambda e, pb=pb, c=c, n4=n4: e.transpose(out=pb[:, c * 128:(c + 1) * 128], in_=vn[:, c, n4 * 128:(n4 + 1) * 128], identity=self.identb[:]),
                             reads=[("sgu_vn", c), "identb"], writes=[pbk])
                    if n4 % 2 == 0:
                        S.op("dve", lambda e, pb=pb, n4=n4: e.tensor_copy(out=vtm[:, n4, :], in_=pb[:]), reads=[pbk], writes=[("sgu_vtm", n4)])
                    else:
                        S.op("act", lambda e, pb=pb, n4=n4: e.copy(out=vtm[:, n4, :], in_=pb[:]), reads=[pbk], writes=[("sgu_vtm", n4)])
                o_, ok = ot[0], ("sgu_o", 0)
                for c in range(8):
                    g = c // 2
                    ps, pk = self.next_ps()
                    for n4 in range(4):
                        S.op("pe", lambda e, ps=ps, c=c, n4=n4, g=g: e.matmul(ps[:, n4 * 128:(n4 + 1) * 128], lhsT=vtm[:, n4, c * 128:(c + 1) * 128], rhs=wsb[:, g, :], start=True, stop=True),
                             reads=[("sgu_vtm", n4), "sgu_wsb"], writes=[pk])
                    x_, xk = otmp[c % 2], ("gelu_tmp", c % 2)
                    for n4 in range(4):
                        S.op("dve", lambda e, ps=ps, x_=x_, n4=n4, g=g: e.tensor_tensor(out=x_[:, n4 * 128:(n4 + 1) * 128], in0=ps[:, n4 * 128:(n4 + 1) * 128], in1=bsb[:, g * 128:(g + 1) * 128], op=ALU.add),
                             reads=[pk, "sgu_bs"], writes=[xk])
                    S.op("dve", lambda e, tsl=tsl, x_=x_, o_=o_, c=c: e.tensor_tensor(out=o_[:, c, :], in0=x_[:], in1=guT[:, c, tsl], op=ALU.mult),
                         reads=[xk, ("sgu_gu", c, t)], writes=[ok])
                S.op("sp", lambda e, tsl=tsl, o_=o_: e.dma_start(out=self.yb[2][:, :, tsl].rearrange("k p t -> p k t"), in_=o_[:]), reads=[ok], writes=["yb2"], dma=True)
        S.barrier()

    def stage_mem(self, l):
        nc, S, W = self.nc, self.S, self.W[l]
        S.barrier()
        with ExitStack() as st:
            sb = lambda name, shape, dt: st.enter_context(nc.sbuf_tensor(self.un(name), shape, dt))
            self.load_hT(st)
            self.stage_mem_prep(st)
            kvT = sb("mem_kvT", [128, 16, 256], BF16)
            vtm = sb("mem_vtm", [128, 2, 1024], BF16)
            qT = sb("mem_qT", [128, 8, T], BF16)

            def memT_in(kc, t):
                return self.memT[:, kc, :], ["memT"]

            def epi_kv(j, t, ps, pk):
                if j % 2 == 0:
                    S.op("act", lambda e: e.copy(out=kvT[:, j, :], in_=ps[:, 0:256]), reads=[pk], writes=[("mem_kvT", j)])
                else:
                    S.op("dve", lambda e: e.tensor_copy(out=kvT[:, j, :], in_=ps[:, 0:256]), reads=[pk], writes=[("mem_kvT", j)])

            self.gemm_n(W["w_mem_kv"], D, 0, 2048, memT_in, 1, epi_kv, 256)
            for c in range(8):
                pb, pbk = self.next_psb()
                for mc in range(2):
                    S.op("pe", lambda e, pb=pb, c=c, mc=mc: e.transpose(out=pb[:, mc * 128:(mc + 1) * 128], in_=kvT[:, 8 + c, mc * 128:(mc + 1) * 128], identity=self.identb[:]),
                         reads=[("mem_kvT", 8 + c), "identb"], writes=[pbk])
                S.op("dve", lambda e, pb=pb, c=c: e.tensor_copy(out=vtm[:, :, c * 128:(c + 1) * 128], in_=pb[:, 0:256].rearrange("p (a b) -> p a b", a=2)),
                     reads=[pbk], writes=["mem_vtm"])

            def epi_q(j, t, ps, pk):
                if (j + t) % 2 == 0:
                    S.op("act", lambda e: e.copy(out=qT[:, j, t * 512:(t + 1) * 512], in_=ps[:]), reads=[pk], writes=[("mem_qT", j, t)])
                else:
                    S.op("dve", lambda e: e.tensor_copy(out=qT[:, j, t * 512:(t + 1) * 512], in_=ps[:]), reads=[pk], writes=[("mem_qT", j, t)])

            self.gemm(W["w_in"], D, 7168, 1024, self.hT_in, NTT, epi_q)
            ex = [sb("mem_ex%d" % i, [128, 2, 512], BF16) for i in range(2)]
            rc = [sb("mem_rc%d" % i, [128, 512], F32) for i in range(2)]
            ot = [sb("mem_o%d" % i, [128, 8, 512], BF16) for i in range(2)]
            it = 0
            for t in range(NTT):
                tsl = slice(t * 512, (t + 1) * 512)
                o_, ok = ot[t % 2], ("mem_o", t % 2)
                for hd in range(4):
                    e_, ek = ex[it % 2], ("mem_ex", it % 2)
                    r_, rk = rc[it % 2], ("mem_rc", it % 2)
                    it += 1
                    for mc in range(2):
                        ps, pk = self.next_ps()
                        for dc in range(2):
                            S.op("pe", lambda e, tsl=tsl, ps=ps, hd=hd, dc=dc, mc=mc: e.matmul(ps[:], lhsT=kvT[:, hd * 2 + dc, mc * 128:(mc + 1) * 128], rhs=qT[:, hd * 2 + dc, tsl], start=(dc == 0), stop=(dc == 1)),
                                 reads=[("mem_kvT", hd * 2 + dc), ("mem_qT", hd * 2 + dc, t)], writes=[pk])
                        S.op("act", lambda e, ps=ps, e_=e_, mc=mc: e.activation(out=e_[:, mc, :], in_=ps[:], func=AF.Exp, scale=1.0 / 16), reads=[pk], writes=[ek])
                    ps_s, pk_s = self.next_ps()
                    for mc in range(2):
                        S.op("pe", lambda e, ps_s=ps_s, e_=e_, mc=mc: e.matmul(ps_s[:], lhsT=self.onesb[:], rhs=e_[:, mc, :], start=(mc == 0), stop=(mc == 1)),
                             reads=["onesb", ek], writes=[pk_s])
                    S.op("dve", lambda e, ps_s=ps_s, r_=r_: e.reciprocal(out=r_[:], in_=ps_s[:]), reads=[pk_s], writes=[rk])
                    for c2 in range(2):
                        ps, pk = self.next_ps()
                        for mc in range(2):
                            S.op("pe", lambda e, ps=ps, hd=hd, c2=c2, mc=mc, e_=e_: e.matmul(ps[:], lhsT=vtm[:, mc, hd * 256 + c2 * 128: hd * 256 + (c2 + 1) * 128], rhs=e_[:, mc, :], start=(mc == 0), stop=(mc == 1)),
                                 reads=["mem_vtm", ek], writes=[pk])
                        S.op("dve", lambda e, ps=ps, o_=o_, hd=hd, c2=c2, r_=r_: e.tensor_tensor(out=o_[:, hd * 2 + c2, :], in0=ps[:], in1=r_[:], op=ALU.mult),
                             reads=[pk, rk], writes=[ok])
                S.op("sp", lambda e, tsl=tsl, o_=o_: e.dma_start(out=self.yb[3][:, :, tsl].rearrange("k p t -> p k t"), in_=o_[:]), reads=[ok], writes=["yb3"], dma=True)
        S.barrier()

    def layer_tables(self, l, st):
        nc, S, W = self.nc, self.S, self.W[l]
        sb = lambda name, shape, dt: st.enter_context(nc.sbuf_tensor(self.un(name), shape, dt))
        lg = self.bcast_load(st, "lgbc", W["ret_log_gamma"], 16)
        self.lgbc = lg
        self.wA = sb("wA", [128, 2, 16, 8], F32)
        self.wk = sb("wk", [128, 2, 8], F32)
        self.g128 = sb("g128", [128, 16], F32)
        self.cf = sb("cf", [128, 16, 8], F32)
        wA, wk, g128, cf = self.wA, self.wk, self.g128, self.cf
        colAf, colAb = self.ct("colAf"), self.ct("colAb")
        for n in range(16):
            S.op("act", lambda e, n=n: e.activation(out=wA[:, 0, n, :], in_=lg[:, 0:8], func=AF.Exp, scale=colAf[:, n:n + 1]),
                 reads=["lgbc", "ctab"], writes=["wA"])
            S.op("act", lambda e, n=n: e.activation(out=wA[:, 1, n, :], in_=lg[:, 8:16], func=AF.Exp, scale=colAb[:, n:n + 1]),
                 reads=["lgbc", "ctab"], writes=["wA"])
        S.op("act", lambda e: e.activation(out=wk[:, 0, :], in_=lg[:, 0:8], func=AF.Exp, scale=self.ct("col127mc")), reads=["lgbc", "ctab"], writes=["wk"])
        S.op("act", lambda e: e.activation(out=wk[:, 1, :], in_=lg[:, 8:16], func=AF.Exp, scale=self.ct("colc")), reads=["lgbc", "ctab"], writes=["wk"])
        S.op("act", lambda e: e.activation(out=g128[:], in_=lg[:], func=AF.Exp, scale=128.0), reads=["lgbc"], writes=["g128"])
        xm, xk = self.ct("xmult"), self.ct("xmask")
        for r in range(16):
            src = lg[:, 0:8] if r < 8 else lg[:, 8:16]
            S.op("act", lambda e, r=r, src=src: e.activation(out=cf[:, r, :], in_=src, func=AF.Exp, scale=xm[:, r:r + 1]), reads=["lgbc", "ctab"], writes=["cf"])
            S.op("dve", lambda e, r=r: e.tensor_scalar(out=cf[:, r, :], in0=cf[:, r, :], scalar1=xk[:, r:r + 1], scalar2=None, op0=ALU.mult), reads=["cf", "ctab"], writes=["cf"])

    def rot_tables(self, st):
        nc, S = self.nc, self.S
        rt = st.enter_context(nc.sbuf_tensor(self.un("rot_sb"), [128, 2, T], F32))
        S.op("sp", lambda e: e.dma_start(out=rt[:], in_=self.rot_d), writes=["rot_sb"], dma=True)
        return rt

    def rotary(self, pre, prekey, out, outkey, rt, tmps, t):
        S = self.S
        tsl = slice(t * 512, (t + 1) * 512)
        ps, pk = self.next_ps()
        S.op("pe", lambda e: e.matmul(ps[:], lhsT=self.rrotb[:], rhs=pre[:, tsl], start=True, stop=True), reads=["rrotb", prekey], writes=[pk])
        i = self.psn % len(tmps)
        tm, tk = tmps[i], ("gelu_tmp", i)
        S.op("dve", lambda e: e.tensor_tensor(out=tm[:], in0=ps[:], in1=rt[:, 1, tsl], op=ALU.mult), reads=[pk, "rot_sb"], writes=[tk])
        j = (i + 1) % len(tmps)
        tm2, tk2 = tmps[j], ("gelu_tmp", j)
        S.op("pool", lambda e: e.tensor_tensor(out=tm2[:], in0=pre[:, tsl], in1=rt[:, 0, tsl], op=ALU.mult), reads=[prekey, "rot_sb"], writes=[tk2])
        S.op("dve", lambda e: e.tensor_tensor(out=out[:, tsl], in0=tm[:], in1=tm2[:], op=ALU.add), reads=[tk, tk2], writes=[outkey])

    def stage_pre_exchange(self, l, st_tab):
        nc, S, W = self.nc, self.S, self.W[l]
        wA = self.wA
        S.barrier()
        with ExitStack() as st:
            sb = lambda name, shape, dt: st.enter_context(nc.sbuf_tensor(self.un(name), shape, dt))
            self.load_hT(st)
            stg = [sb("pa_stg%d" % i, [128, 512], F32) for i in range(3)]
            edge = sb("pa_edge", [128, 128], F32)
            cnt = [0]

            def epi_a(j, t, ps, pk):
                i = cnt[0] % 3
                cnt[0] += 1
                g_, gk = stg[i], ("pa_stg", i)
                S.op("act", lambda e: e.copy(out=g_[:], in_=ps[:]), reads=[pk], writes=[gk])
                S.op("sp", lambda e: e.dma_start(out=self.aTd[j, :, t * 512:(t + 1) * 512], in_=g_[:]), reads=[gk], writes=["aTd"], dma=True)
                if t == 0:
                    S.op("dve", lambda e: e.tensor_copy(out=edge[:, j * 16:j * 16 + 8], in_=g_[:, 0:8]), reads=[gk], writes=["pa_edge"])
                if t == NTT - 1:
                    S.op("dve", lambda e: e.tensor_copy(out=edge[:, j * 16 + 8:j * 16 + 16], in_=g_[:, 504:512]), reads=[gk], writes=["pa_edge"])

            self.gemm(W["w_in"], D, 0, 1024, self.hT_in, NTT, epi_a)
            S.op("sp", lambda e: e.dma_start(out=self.bounce[16 * 128:17 * 128, :], in_=edge[:]), reads=["pa_edge"], writes=["bounce"], dma=True)

            rt = self.rot_tables(st)
            kpre = sb("ra_kpre", [128, T], BF16)
            krot = sb("ra_krot", [128, T], BF16)
            vT = sb("ra_vT", [128, T], BF16)
            tmps = [sb("ra_tmp%d" % i, [128, 512], F32) for i in range(3)]
            tm8 = [sb("ra_tm8%d" % i, [128, 1024], BF16) for i in range(2)]
            kA = sb("ra_kA", [128, 2, NCH, 128], BF16)
            vtm = sb("ra_vtm", [128, NCH, 128], BF16)
            send = sb("ra_send", [128, 16, 128], F32)
            for h in range(8):
                def epi_k(j, t, ps, pk):
                    S.op("act", lambda e: e.copy(out=kpre[:, t * 512:(t + 1) * 512], in_=ps[:]), reads=[pk], writes=[("ra_kpre", t)])
                    self.rotary(kpre, ("ra_kpre", t), krot, ("ra_krot", t), rt, tmps, t)

                def epi_v(j, t, ps, pk):
                    S.op("act", lambda e: e.copy(out=vT[:, t * 512:(t + 1) * 512], in_=ps[:]), reads=[pk], writes=[("ra_vT", t)])

                self.gemm(W["w_in"], D, 2048 + h * 128, 128, self.hT_in, NTT, epi_k, slab=128)
                self.gemm(W["w_in"], D, 3072 + h * 128, 128, self.hT_in, NTT, epi_v, slab=128)
                for q4 in range(4):
                    pb, pbk = self.next_psb()
                    for i in range(4):
                        n = q4 * 4 + i
                        S.op("pe", lambda e, pb=pb, i=i, n=n: e.transpose(out=pb[:, i * 128:(i + 1) * 128], in_=krot[:, n * 128:(n + 1) * 128], identity=self.identb[:]),
                             reads=[("ra_krot", n // 4), "identb"], writes=[pbk])
                        S.op("pe", lambda e, pb=pb, i=i, n=n: e.transpose(out=pb[:, (4 + i) * 128:(5 + i) * 128], in_=vT[:, n * 128:(n + 1) * 128], identity=self.identb[:]),
                             reads=[("ra_vT", n // 4), "identb"], writes=[pbk])
                    t8, t8k = tm8[q4 % 2], ("ra_tm8", q4 % 2)
                    S.op("dve", lambda e, pb=pb, t8=t8: e.tensor_copy(out=t8[:], in_=pb[:]), reads=[pbk], writes=[t8k])
                    S.op("pool", lambda e, t8=t8, q4=q4: e.tensor_copy(out=vtm[:, q4 * 4:q4 * 4 + 4, :], in_=t8[:, 512:1024].rearrange("p (a b) -> p a b", a=4)), reads=[t8k], writes=["ra_vtm"])
                    for i in range(4):
                        n = q4 * 4 + i
                        S.op("dve", lambda e, t8=t8, i=i, n=n, h=h: e.tensor_scalar(out=kA[:, 0, n, :], in0=t8[:, i * 128:(i + 1) * 128], scalar1=wA[:, 0, n, h:h + 1], scalar2=None, op0=ALU.mult),
                             reads=[t8k, "wA"], writes=["ra_kA"])
                        S.op("dve", lambda e, t8=t8, i=i, n=n, h=h: e.tensor_scalar(out=kA[:, 1, n, :], in0=t8[:, i * 128:(i + 1) * 128], scalar1=wA[:, 1, n, h:h + 1], scalar2=None, op0=ALU.mult),
                             reads=[t8k, "wA"], writes=["ra_kA"])
                for dr in range(2):
                    ps, pk = self.next_ps()
                    for n in range(NCH):
                        S.op("pe", lambda e, ps=ps, dr=dr, n=n: e.matmul(ps[:, 0:128], lhsT=kA[:, dr, n, :], rhs=vtm[:, n, :], start=(n == 0), stop=(n == NCH - 1)),
                             reads=["ra_kA", "ra_vtm"], writes=[pk])
                    S.op("act" if dr == 0 else "dve",
                         (lambda e, ps=ps, dr=dr, h=h: e.copy(out=send[:, dr * 8 + h, :], in_=ps[:, 0:128])) if dr == 0 else
                         (lambda e, ps=ps, dr=dr, h=h: e.tensor_copy(out=send[:, dr * 8 + h, :], in_=ps[:, 0:128])),
                         reads=[pk], writes=["ra_send"])
            S.op("sp", lambda e: e.dma_start(out=self.bounce[0:16 * 128, :].rearrange("(b p) e -> p b e", p=128), in_=send[:]), reads=["ra_send"], writes=["bounce"], dma=True)
        S.barrier()
        if self.mode == "A":
            pass
        elif NOCOLL:
            S.op("sp", lambda e: e.dma_start(out=self.gath[0:17 * 128, :], in_=self.bounce), reads=["bounce"], writes=["gath"], dma=True)
        else:
            S.op("pool", lambda e: e.collective_compute("AllGather", ALU.bypass, replica_groups=[list(range(8))], ins=[self.bounce.opt()], outs=[self.gath.opt()]),
                 reads=["bounce"], writes=["gath"], dma=True, n_inc=1)
            S.op("pool", lambda e: e.nop(), reads=["gath"], writes=["gath"])
        S.barrier()

    def stage_pool_b(self, l):
        nc, S, W = self.nc, self.S, self.W[l]
        S.barrier()
        with ExitStack() as st:
            sb = lambda name, shape, dt: st.enter_context(nc.sbuf_tensor(self.un(name), shape, dt))
            E = sb("pb_E", [128, 8, 128], F32)
            S.op("sp", lambda e: e.dma_start(out=E[:], in_=self.gath.rearrange("(r b p) e -> p r b e", r=8, b=17)[:, :, 16, :]), reads=["gath"], writes=["pb_E"], dma=True)
            hal = sb("pb_hal", [128, 2, 8, 8], F32)
            selL, selR = self.ct("selL"), self.ct("selR")
            Ev = E[:].rearrange("p r (j s) -> p r j s", s=16)
            for side, sel, lo in ((0, selL, 8), (1, selR, 0)):
                for r in range(8):
                    if r == 0:
                        S.op("dve", lambda e, side=side, sel=sel, lo=lo, r=r: e.tensor_scalar(out=hal[:, side], in0=Ev[:, r, :, lo:lo + 8], scalar1=sel[:, r:r + 1], scalar2=None, op0=ALU.mult),
                             reads=["pb_E", "ctab"], writes=["pb_hal"])
                    else:
                        S.op("dve", lambda e, side=side, sel=sel, lo=lo, r=r: e.scalar_tensor_tensor(out=hal[:, side], in0=Ev[:, r, :, lo:lo + 8], scalar=sel[:, r:r + 1], in1=hal[:, side], op0=ALU.mult, op1=ALU.add),
                             reads=["pb_E", "ctab"], writes=["pb_hal"])
            pwf = sb("pb_pwf", [128, 8, 256], F32)
            pwb = sb("pb_pwb", [128, 8, 256], BF16)
            S.op("sp", lambda e: e.dma_start(out=pwf[:], in_=W["pool_w"].rearrange("g (ji p) o -> p (g ji) o", p=128)), writes=["pb_pwf"], dma=True)
            S.op("dve", lambda e: e.tensor_copy(out=pwb[:], in_=pwf[:]), reads=["pb_pwf"], writes=["pb_pwb"])
            psc = self.col_load(st, "pb_psc", W["pool_scale"], 8)
            X = [sb("pb_X%d" % i, [128, T + 16], F32) for i in range(2)]
            A = [sb("pb_A%d" % i, [128, T + 16], F32) for i in range(2)]
            mixed = sb("pb_mix", [128, 2, T], BF16)
            ot = sb("pb_o", [128, 2, T], BF16)
            corr = self.ct("corr")
            L = T + 16
            for g in range(4):
                w = (2, 4, 8, 16)[g]
                for ji in range(2):
                    j = g * 2 + ji
                    X_, Xk = X[ji], ("pb_X", ji)
                    S.op("sp", lambda e, X_=X_, j=j: e.dma_start(out=X_[:, 8:8 + T], in_=self.aTd[j]), reads=["aTd"], writes=[Xk], dma=True)
                    S.op("dve", lambda e, X_=X_, j=j: e.tensor_copy(out=X_[:, 0:8], in_=hal[:, 0, j, :]), reads=["pb_hal"], writes=[Xk])
                    S.op("dve", lambda e, X_=X_, j=j: e.tensor_copy(out=X_[:, 8 + T:16 + T], in_=hal[:, 1, j, :]), reads=["pb_hal"], writes=[Xk])
                    a0, a1 = A[0], A[1]
                    S.op("dve", lambda e, X_=X_: e.tensor_tensor(out=a0[:, 1:L], in0=X_[:, 0:L - 1], in1=X_[:, 1:L], op=ALU.add), reads=[Xk], writes=["pb_A0"])
                    cur, curk, oth, othk = a0, "pb_A0", a1, "pb_A1"
                    sh = 1
                    lo, hi = 1, L
                    while sh * 2 < w:
                        nlo, nhi = lo + sh, hi - sh
                        S.op("dve", lambda e, cur=cur, oth=oth, sh=sh, nlo=nlo, nhi=nhi: e.tensor_tensor(out=oth[:, nlo:nhi], in0=cur[:, nlo - sh:nhi - sh], in1=cur[:, nlo + sh:nhi + sh], op=ALU.add),
                             reads=[curk], writes=[othk])
                        cur, curk, oth, othk = oth, othk, cur, curk
                        lo, hi = nlo, nhi
                        sh *= 2
                    S.op("dve", lambda e, cur=cur, w=w: e.tensor_scalar(out=cur[:, 8:8 + T], in0=cur[:, 8:8 + T], scalar1=1.0 / w, scalar2=None, op0=ALU.mult), reads=[curk], writes=[curk])
                    S.op("dve", lambda e, cur=cur, g=g: e.tensor_tensor(out=cur[:, 8:16], in0=cur[:, 8:16], in1=corr[:, g * 16:g * 16 + 8], op=ALU.mult), reads=[curk, "ctab"], writes=[curk])
                    S.op("dve", lambda e, cur=cur, g=g: e.tensor_tensor(out=cur[:, T:T + 8], in0=cur[:, T:T + 8], in1=corr[:, g * 16 + 8:g * 16 + 16], op=ALU.mult), reads=[curk, "ctab"], writes=[curk])
                    S.op("dve", lambda e, cur=cur, X_=X_, ji=ji: e.tensor_tensor(out=mixed[:, ji, :], in0=cur[:, 8:8 + T], in1=X_[:, 8:8 + T], op=ALU.subtract), reads=[curk, Xk], writes=[("pb_mix", ji)])
                for jo in range(2):
                    j = g * 2 + jo
                    for t in range(NTT):
                        ps, pk = self.next_ps()
                        for ji in range(2):
                            S.op("pe", lambda e, ps=ps, g=g, ji=ji, jo=jo, t=t: e.matmul(ps[:], lhsT=pwb[:, g * 2 + ji, jo * 128:(jo + 1) * 128], rhs=mixed[:, ji, t * 512:(t + 1) * 512], start=(ji == 0), stop=(ji == 1)),
                                 reads=["pb_pwb", ("pb_mix", ji)], writes=[pk])
                        S.op("dve", lambda e, ps=ps, jo=jo, t=t, j=j: e.tensor_scalar(out=ot[:, jo, t * 512:(t + 1) * 512], in0=ps[:], scalar1=psc[:, j:j + 1], scalar2=None, op0=ALU.mult), reads=[pk, "pb_psc"], writes=["pb_o"])
                S.op("sp", lambda e, g=g: e.dma_start(out=self.yb[0][g * 2:g * 2 + 2].rearrange("k p t -> p k t"), in_=ot[:]), reads=["pb_o"], writes=["yb0"], dma=True)
        S.barrier()

    def stage_ret_b(self, l):
        nc, S, W = self.nc, self.S, self.W[l]
        wk_ = self.wk
        S.barrier()
        with ExitStack() as st:
            sb = lambda name, shape, dt: st.enter_context(nc.sbuf_tensor(self.un(name), shape, dt))
            self.load_hT(st)
            rt = self.rot_tables(st)
            lg = self.lgbc
            gng = self.col_load(st, "rb_gng", W["ret_gn_g"], 8)
            Dm = sb("rb_D", [128, 8, 128], F32)
            wq = sb("rb_wq", [128, 2, 8, 128], F32)
            tq = sb("rb_tq", [128, 128], F32)
            s_ = 128.0 ** -0.5
            for h in range(8):
                S.op("act", lambda e, h=h: e.activation(out=Dm[:, h, :], in_=self.ct("dpos"), func=AF.Exp, scale=lg[:, h:h + 1]), reads=["ctab", "lgbc"], writes=["rb_D"])
                S.op("dve", lambda e, h=h: e.tensor_tensor(out=Dm[:, h, :], in0=Dm[:, h, :], in1=self.ct("mge"), op=ALU.mult), reads=["rb_D", "ctab"], writes=["rb_D"])
                S.op("act", lambda e, h=h: e.activation(out=tq[:], in_=self.ct("dneg"), func=AF.Exp, scale=lg[:, 8 + h:9 + h]), reads=["ctab", "lgbc"], writes=["rb_tq"])
                S.op("dve", lambda e: e.tensor_tensor(out=tq[:], in0=tq[:], in1=self.ct("mlt"), op=ALU.mult), reads=["rb_tq", "ctab"], writes=["rb_tq"])
                S.op("dve", lambda e, h=h: e.tensor_tensor(out=Dm[:, h, :], in0=Dm[:, h, :], in1=tq[:], op=ALU.add), reads=["rb_D", "rb_tq"], writes=["rb_D"])
                S.op("act", lambda e, h=h: e.activation(out=wq[:, 0, h, :], in_=self.ct("rowc1"), func=AF.Exp, scale=lg[:, h:h + 1]), reads=["ctab", "lgbc"], writes=["rb_wq"])
                S.op("act", lambda e, h=h: e.activation(out=wq[:, 1, h, :], in_=self.ct("row128mc"), func=AF.Exp, scale=lg[:, 8 + h:9 + h]), reads=["ctab", "lgbc"], writes=["rb_wq"])
            S.op("dve", lambda e: e.tensor_scalar(out=wq[:], in0=wq[:], scalar1=s_, scalar2=None, op0=ALU.mult), reads=["rb_wq"], writes=["rb_wq"])
            qpre = sb("rb_qpre", [128, T], BF16)
            kpre = sb("rb_kpre", [128, T], BF16)
            qrot = sb("rb_qrot", [128, T], BF16)
            krot = sb("rb_krot", [128, T], BF16)
            qw = sb("rb_qw", [128, 2, T], BF16)
            vT = sb("rb_vT", [128, T], BF16)
            sg = sb("rb_sg", [128, T], F32)
            tmps = [sb("rb_tmp%d" % i, [128, 512], F32) for i in range(3)]
            tm8 = [sb("rb_tm8%d" % i, [128, 1024], BF16) for i in range(2)]
            kl = sb("rb_kl", [128, 2, NCH, 128], BF16)
            vtm = sb("rb_vtm", [128, NCH, 128], BF16)
            Sg = sb("rb_Sg", [128, 2, 8, 128], F32)
            Scur = sb("rb_Scur", [128, 2, 128], F32)
            Sbf = sb("rb_Sbf", [128, 2, NCH, 128], BF16)
            sm = [sb("rb_sm%d" % i, [128, 512], BF16) for i in range(2)]
            y = tmps[2]
            ybf = sb("rb_ybf", [128, 512], BF16)
            ysq = sb("rb_ysq", [128, 512], BF16)
            mean = tmps[0]
            rstd = tmps[1]
            ot = sb("rb_o", [128, T], BF16)
            gv = self.gath.rearrange("(r b p) e -> p r b e", r=8, b=17)
            for h in range(8):
                for dr in range(2):
                    S.op("sp", lambda e, dr=dr, h=h: e.dma_start(out=Sg[:, dr], in_=gv[:, :, dr * 8 + h, :]), reads=["gath"], writes=[("rb_Sg", dr)], dma=True)
                    for r in range(8):
                        cfc = self.cf[:, dr * 8 + r, h:h + 1]
                        if r == 0:
                            S.op("dve", lambda e, dr=dr, r=r, cfc=cfc: e.tensor_scalar(out=Scur[:, dr, :], in0=Sg[:, dr, r, :], scalar1=cfc, scalar2=None, op0=ALU.mult),
                                 reads=[("rb_Sg", dr), "cf"], writes=[("rb_Scur", dr)])
                        else:
                            S.op("dve", lambda e, dr=dr, r=r, cfc=cfc: e.scalar_tensor_tensor(out=Scur[:, dr, :], in0=Sg[:, dr, r, :], scalar=cfc, in1=Scur[:, dr, :], op0=ALU.mult, op1=ALU.add),
                                 reads=[("rb_Sg", dr), "cf"], writes=[("rb_Scur", dr)])

                def epi_q(j, t, ps, pk):
                    S.op("act", lambda e: e.copy(out=qpre[:, t * 512:(t + 1) * 512], in_=ps[:]), reads=[pk], writes=[("rb_qpre", t)])
                    self.rotary(qpre, ("rb_qpre", t), qrot, ("rb_qrot", t), rt, tmps, t)

                def epi_k(j, t, ps, pk):
                    S.op("act", lambda e: e.copy(out=kpre[:, t * 512:(t + 1) * 512], in_=ps[:]), reads=[pk], writes=[("rb_kpre", t)])
                    self.rotary(kpre, ("rb_kpre", t), krot, ("rb_krot", t), rt, tmps, t)

                def epi_v(j, t, ps, pk):
                    S.op("act", lambda e: e.copy(out=vT[:, t * 512:(t + 1) * 512], in_=ps[:]), reads=[pk], writes=[("rb_vT", t)])

                def epi_g(j, t, ps, pk):
                    S.op("act", lambda e: e.activation(out=sg[:, t * 512:(t + 1) * 512], in_=ps[:], func=AF.Silu), reads=[pk], writes=[("rb_sg", t)])

                self.gemm(W["w_in"], D, 1024 + h * 128, 128, self.hT_in, NTT, epi_q, slab=128)
                self.gemm(W["w_in"], D, 2048 + h * 128, 128, self.hT_in, NTT, epi_k, slab=128)
                self.gemm(W["w_in"], D, 3072 + h * 128, 128, self.hT_in, NTT, epi_v, slab=128)
                self.gemm(W["w_in"], D, 4096 + h * 128, 128, self.hT_in, NTT, epi_g, slab=128)
                for n in range(NCH):
                    csl = slice(n * 128, (n + 1) * 128)
                    S.op("dve", lambda e, csl=csl, h=h: e.tensor_tensor(out=qw[:, 0, csl], in0=qrot[:, csl], in1=wq[:, 0, h, :], op=ALU.mult), reads=[("rb_qrot", n // 4), "rb_wq"], writes=[("rb_qw", n)])
                    S.op("pool", lambda e, csl=csl, h=h: e.tensor_tensor(out=qw[:, 1, csl], in0=qrot[:, csl], in1=wq[:, 1, h, :], op=ALU.mult), reads=[("rb_qrot", n // 4), "rb_wq"], writes=[("rb_qw", n)])
                for q4 in range(4):
                    pb, pbk = self.next_psb()
                    for i in range(4):
                        n = q4 * 4 + i
                        S.op("pe", lambda e, pb=pb, i=i, n=n: e.transpose(out=pb[:, i * 128:(i + 1) * 128], in_=krot[:, n * 128:(n + 1) * 128], identity=self.identb[:]),
                             reads=[("rb_krot", n // 4), "identb"], writes=[pbk])
                        S.op("pe", lambda e, pb=pb, i=i, n=n: e.transpose(out=pb[:, (4 + i) * 128:(5 + i) * 128], in_=vT[:, n * 128:(n + 1) * 128], identity=self.identb[:]),
                             reads=[("rb_vT", n // 4), "identb"], writes=[pbk])
                    t8, t8k = tm8[q4 % 2], ("rb_tm8", q4 % 2)
                    S.op("dve", lambda e, pb=pb, t8=t8: e.tensor_copy(out=t8[:], in_=pb[:]), reads=[pbk], writes=[t8k])
                    S.op("pool", lambda e, t8=t8, q4=q4: e.tensor_copy(out=vtm[:, q4 * 4:q4 * 4 + 4, :], in_=t8[:, 512:1024].rearrange("p (a b) -> p a b", a=4)), reads=[t8k], writes=[("rb_vtm", q4)])
                    S.op("dve", lambda e, t8=t8, q4=q4, h=h: e.tensor_scalar(out=kl[:, 0, q4 * 4:q4 * 4 + 4, :].rearrange("p a b -> p (a b)"), in0=t8[:, 0:512], scalar1=wk_[:, 0, h:h + 1], scalar2=None, op0=ALU.mult),
                         reads=[t8k, "wk"], writes=[("rb_kl", q4)])
                    S.op("dve", lambda e, t8=t8, q4=q4, h=h: e.tensor_scalar(out=kl[:, 1, q4 * 4:q4 * 4 + 4, :].rearrange("p a b -> p (a b)"), in0=t8[:, 0:512], scalar1=wk_[:, 1, h:h + 1], scalar2=None, op0=ALU.mult),
                         reads=[t8k, "wk"], writes=[("rb_kl", q4)])
                for dr in range(2):
                    order = list(range(NCH)) if dr == 0 else list(range(NCH - 1, -1, -1))
                    gcol = self.g128[:, dr * 8 + h:dr * 8 + h + 1]
                    for n in order:
                        S.op("act", lambda e, dr=dr, n=n: e.copy(out=Sbf[:, dr, n, :], in_=Scur[:, dr, :]), reads=[("rb_Scur", dr)], writes=[("rb_Sbf", dr, n)])
                        ps, pk = self.next_ps()
                        S.op("pe", lambda e, ps=ps, dr=dr, n=n: e.matmul(ps[:, 0:128], lhsT=kl[:, dr, n, :], rhs=vtm[:, n, :], start=True, stop=True),
                             reads=[("rb_kl", n // 4), ("rb_vtm", n // 4)], writes=[pk])
                        S.op("dve", lambda e, ps=ps, dr=dr, gcol=gcol: e.scalar_tensor_tensor(out=Scur[:, dr, :], in0=Scur[:, dr, :], scalar=gcol, in1=ps[:, 0:128], op0=ALU.mult, op1=ALU.add),
                             reads=[("rb_Scur", dr), pk, "g128", ("rb_Sbf", dr, n)], writes=[("rb_Scur", dr)])
                for t in range(NTT):
                    tsl = slice(t * 512, (t + 1) * 512)
                    ps_s, pk_s = self.next_ps()
                    for i in range(4):
                        n = t * 4 + i
                        S.op("pe", lambda e, ps_s=ps_s, i=i, n=n: e.matmul(ps_s[:, i * 128:(i + 1) * 128], lhsT=krot[:, n * 128:(n + 1) * 128], rhs=qrot[:, n * 128:(n + 1) * 128], start=True, stop=True),
                             reads=[("rb_krot", t), ("rb_qrot", t)], writes=[pk_s])
                    sm_, smk = sm[t % 2], ("rb_sm", t % 2)
                    for i in range(4):
                        S.op("dve", lambda e, ps_s=ps_s, sm_=sm_, i=i, h=h: e.tensor_tensor(out=sm_[:, i * 128:(i + 1) * 128], in0=ps_s[:, i * 128:(i + 1) * 128], in1=Dm[:, h, :], op=ALU.mult),
                             reads=[pk_s, "rb_D"], writes=[smk])
                    ps_y, pk_y = self.next_ps()
                    for i in range(4):
                        n = t * 4 + i
                        csl = slice(n * 128, (n + 1) * 128)
                        S.op("pe", lambda e, ps_y=ps_y, sm_=sm_, i=i, n=n: e.matmul(ps_y[:, i * 128:(i + 1) * 128], lhsT=vtm[:, n, :], rhs=sm_[:, i * 128:(i + 1) * 128], start=True, stop=False),
                             reads=[("rb_vtm", n // 4), smk], writes=[pk_y])
                        S.op("pe", lambda e, ps_y=ps_y, i=i, n=n, csl=csl: e.matmul(ps_y[:, i * 128:(i + 1) * 128], lhsT=Sbf[:, 0, n, :], rhs=qw[:, 0, csl], start=False, stop=False),
                             reads=[("rb_Sbf", 0, n), ("rb_qw", n)], writes=[pk_y])
                        S.op("pe", lambda e, ps_y=ps_y, i=i, n=n, csl=csl: e.matmul(ps_y[:, i * 128:(i + 1) * 128], lhsT=Sbf[:, 1, n, :], rhs=qw[:, 1, csl], start=False, stop=True),
                             reads=[("rb_Sbf", 1, n), ("rb_qw", n)], writes=[pk_y])
                    S.op("act", lambda e, ps_y=ps_y: e.copy(out=y[:], in_=ps_y[:]), reads=[pk_y], writes=[("gelu_tmp", 2)])
                    S.op("dve", lambda e: e.tensor_copy(out=ybf[:], in_=y[:]), reads=[("gelu_tmp", 2)], writes=["rb_ybf"])
                    S.op("pool", lambda e: e.tensor_tensor(out=ysq[:], in0=y[:], in1=y[:], op=ALU.mult), reads=[("gelu_tmp", 2)], writes=["rb_ysq"])
                    ps_m, pk_m = self.next_ps()
                    S.op("pe", lambda e, ps_m=ps_m: e.matmul(ps_m[:], lhsT=self.onesb[:], rhs=ybf[:], start=True, stop=True), reads=["onesb", "rb_ybf"], writes=[pk_m])
                    ps_q, pk_q = self.next_ps()
                    S.op("pe", lambda e, ps_q=ps_q: e.matmul(ps_q[:], lhsT=self.onesb[:], rhs=ysq[:], start=True, stop=True), reads=["onesb", "rb_ysq"], writes=[pk_q])
                    S.op("act", lambda e, ps_m=ps_m: e.mul(out=mean[:], in_=ps_m[:], mul=1.0 / 128), reads=[pk_m], writes=[("gelu_tmp", 0)])
                    S.op("dve", lambda e: e.tensor_tensor(out=rstd[:], in0=mean[:], in1=mean[:], op=ALU.mult), reads=[("gelu_tmp", 0)], writes=[("gelu_tmp", 1)])
                    S.op("dve", lambda e, ps_q=ps_q: e.scalar_tensor_tensor(out=rstd[:], in0=ps_q[:], scalar=1.0 / 128, in1=rstd[:], op0=ALU.mult, op1=ALU.subtract), reads=[pk_q, ("gelu_tmp", 1)], writes=[("gelu_tmp", 1)])
                    S.op("dve", lambda e: e.tensor_scalar(out=rstd[:], in0=rstd[:], scalar1=LN_EPS, scalar2=None, op0=ALU.add), reads=[("gelu_tmp", 1)], writes=[("gelu_tmp", 1)])
                    S.op("act", lambda e: e.activation(out=rstd[:], in_=rstd[:], func=AF.Sqrt), reads=[("gelu_tmp", 1)], writes=[("gelu_tmp", 1)])
                    S.op("dve", lambda e: e.reciprocal(out=rstd[:], in_=rstd[:]), reads=[("gelu_tmp", 1)], writes=[("gelu_tmp", 1)])
                    S.op("dve", lambda e: e.tensor_tensor(out=y[:], in0=y[:], in1=mean[:], op=ALU.subtract), reads=[("gelu_tmp", 2), ("gelu_tmp", 0), "rb_ybf", "rb_ysq"], writes=[("gelu_tmp", 2)])
                    S.op("dve", lambda e: e.tensor_tensor(out=y[:], in0=y[:], in1=rstd[:], op=ALU.mult), reads=[("gelu_tmp", 2), ("gelu_tmp", 1)], writes=[("gelu_tmp", 2)])
                    S.op("dve", lambda e, tsl=tsl, h=h: e.scalar_tensor_tensor(out=ot[:, tsl], in0=y[:], scalar=gng[:, h:h + 1], in1=sg[:, tsl], op0=ALU.mult, op1=ALU.mult),
                         reads=[("gelu_tmp", 2), "rb_gng", ("rb_sg", t)], writes=["rb_o"])
                S.op("sp", lambda e, h=h: e.dma_start(out=self.yb[1][h], in_=ot[:]), reads=["rb_o"], writes=["yb1"], dma=True)
        S.barrier()

    def load_slab(self, Wap, K, col0, slab=256):
        S = self.S
        kc_n = K // 128
        si = self.wsn % len(self.wslots)
        self.wsn += 1
        slot = self.wslots[si]
        skey = ("wslot", si)
        src = Wap[:, col0:col0 + slab].rearrange("(kc p) n -> p kc n", p=128)
        S.op("pool", lambda e: e.dma_start(out=slot[:, 0:kc_n, 0:slab], in_=src), writes=[skey], dma=True)
        return slot, skey

    def mm_acc(self, ps, pk, slot, skey, kc_n, jj, rhs_fn):
        S = self.S
        for kc in range(kc_n):
            rhs, rkeys = rhs_fn(kc)
            S.op("pe", lambda e, kc=kc, rhs=rhs: e.matmul(ps[:], lhsT=slot[:, kc, jj * 128:(jj + 1) * 128], rhs=rhs, start=(kc == 0), stop=(kc == kc_n - 1)),
                 reads=[skey] + rkeys, writes=[pk])

    def ln_fm(self, acc, ybuf, tmps, gcol, bcol, hf, want_alpha, write_hT):
        nc, S = self.nc, self.S
        sqb, xbf = tmps["sqb"], tmps["xbf"]
        mean, rstd, o32 = tmps["f0"], tmps["f1"], tmps["f2"]
        for t in range(2):
            tsl = slice(t * 512, (t + 1) * 512)
            gsl = slice(hf * 1024 + t * 512, hf * 1024 + (t + 1) * 512)
            ps_s, pk_s = self.next_ps()
            ps_q, pk_q = self.next_ps()
            for j in range(KC):
                S.op("act", lambda e, j=j, tsl=tsl: e.copy(out=xbf[:], in_=acc[:, j, tsl]), reads=[("acc", j, t)], writes=["ln_xbf"])
                S.op("pe", lambda e, ps_s=ps_s, j=j: e.matmul(ps_s[:], lhsT=self.onesb[:], rhs=xbf[:], start=(j == 0), stop=(j == KC - 1)), reads=["onesb", "ln_xbf"], writes=[pk_s])
                S.op("dve", lambda e, j=j, tsl=tsl: e.tensor_tensor(out=sqb[:], in0=acc[:, j, tsl], in1=acc[:, j, tsl], op=ALU.mult), reads=[("acc", j, t)], writes=["ln_sqb"])
                S.op("pe", lambda e, ps_q=ps_q, j=j: e.matmul(ps_q[:], lhsT=self.onesb[:], rhs=sqb[:], start=(j == 0), stop=(j == KC - 1)), reads=["onesb", "ln_sqb"], writes=[pk_q])
            S.op("act", lambda e, ps_s=ps_s: e.mul(out=mean[:], in_=ps_s[:], mul=1.0 / D), reads=[pk_s], writes=["ln_f0"])
            S.op("dve", lambda e: e.tensor_tensor(out=rstd[:], in0=mean[:], in1=mean[:], op=ALU.mult), reads=["ln_f0"], writes=["ln_f1"])
            S.op("dve", lambda e, ps_q=ps_q: e.scalar_tensor_tensor(out=rstd[:], in0=ps_q[:], scalar=1.0 / D, in1=rstd[:], op0=ALU.mult, op1=ALU.subtract), reads=[pk_q, "ln_f1"], writes=["ln_f1"])
            S.op("dve", lambda e: e.tensor_scalar(out=rstd[:], in0=rstd[:], scalar1=LN_EPS, scalar2=None, op0=ALU.add), reads=["ln_f1"], writes=["ln_f1"])
            S.op("act", lambda e: e.activation(out=rstd[:], in_=rstd[:], func=AF.Sqrt), reads=["ln_f1"], writes=["ln_f1"])
            S.op("dve", lambda e: e.reciprocal(out=rstd[:], in_=rstd[:]), reads=["ln_f1"], writes=["ln_f1"])
            for j in range(KC):
                S.op("dve", lambda e, j=j, tsl=tsl: e.tensor_tensor(out=o32[:], in0=acc[:, j, tsl], in1=mean[:], op=ALU.subtract), reads=[("acc", j, t), "ln_f0"], writes=["ln_f2"])
                S.op("dve", lambda e: e.tensor_tensor(out=o32[:], in0=o32[:], in1=rstd[:], op=ALU.mult), reads=["ln_f2", "ln_f1"], writes=["ln_f2"])
                S.op("act", lambda e, j=j: e.activation(out=o32[:], in_=o32[:], func=AF.Identity, scale=gcol[:, j:j + 1], bias=bcol[:, j:j + 1]), reads=["ln_f2", "ln_g", "ln_b"], writes=["ln_f2"])
                S.op("sp", lambda e, j=j, gsl=gsl: e.dma_start(out=self.hres[j, :, gsl], in_=o32[:]), reads=["ln_f2"], writes=["hres"], dma=True)
                if write_hT:
                    S.op("act", lambda e: e.copy(out=xbf[:], in_=o32[:]), reads=["ln_f2"], writes=["ln_xbf"])
                    S.op("sp", lambda e, j=j, gsl=gsl: e.dma_start(out=self.hTd[j, :, gsl], in_=xbf[:]), reads=["ln_xbf"], writes=["hTd"], dma=True)
                else:
                    S.op("act", lambda e, j=j, tsl=tsl: e.copy(out=ybuf[j // 8][:, j % 8, tsl], in_=o32[:]), reads=["ln_f2"], writes=[("ybuf", j // 8)])
                if want_alpha:
                    S.op("dve", lambda e, j=j, tsl=tsl: e.tensor_scalar(out=acc[:, j, tsl], in0=o32[:], scalar1=ALPHA, scalar2=None, op0=ALU.mult), reads=["ln_f2"], writes=[("acc", j, t)])

    def stage_post(self, l):
        nc, S, W = self.nc, self.S, self.W[l]
        S.barrier()
        with ExitStack() as st:
            sb = lambda name, shape, dt: st.enter_context(nc.sbuf_tensor(self.un(name), shape, dt))
            acc = sb("acc", [128, KC, 1024], F32)
            ybuf = [sb("ybuf%d" % i, [128, 8, 1024], BF16) for i in range(2)]
            f = [sb("pf%d" % i, [128, 512], F32) for i in range(4)]
            tm = {"sqb": sb("ln_sqb", [128, 512], BF16), "xbf": sb("ln_xbf", [128, 512], BF16), "f0": f[0], "f1": f[1], "f2": f[2]}
            bg = self.col_load(st, "bgate", W["b_gate"].rearrange("b d -> (b d)"), 64)
            l1g = self.col_load(st, "ln_g1", W["ln1_g"], KC)
            l1b = self.col_load(st, "ln_b1", W["ln1_b"], KC)
            l2g = self.col_load(st, "ln_g2", W["ln2_g"], KC)
            l2b = self.col_load(st, "ln_b2", W["ln2_b"], KC)
            bup = sb("bup", [128, NEXP, 12], F32)
            S.op("sp", lambda e: e.dma_start(out=bup[:], in_=W["b_up"].rearrange("e (c p) -> p e c", p=128), allow_slow_non_contiguous=True), writes=["bup"], dma=True)
            wrb = sb("wrb", [128, KC, 32], BF16)
            S.op("pool", lambda e: e.dma_start(out=wrb[:], in_=W["w_router"].rearrange("(c p) n -> p c n", p=128)), writes=["wrb"], dma=True)
            brt = self.bcast_load(st, "brt", W["b_router"], 32)
            bdb = sb("bdb", [32, D], BF16)
            S.op("pool", lambda e: e.dma_start(out=bdb[:], in_=W["b_down"]), writes=["bdb"], dma=True)
            lgt = sb("lgt", [128, 8, 32], F32)
            top8 = sb("top8", [128, 8], F32)
            msk = sb("msk", [128, 32], F32)
            sml = sb("sml", [128, 4], F32)
            GT = sb("GT", [32, 1024], F32)
            GTb = sb("GTb", [32, 1024], BF16)
            gbc = sb("gbc", [128, 1024], F32)
            gact = sb("gact", [128, 6, 1024], BF16)
            hrt = [sb("hrt%d" % i, [128, 1024], F32) for i in range(1)]
            hTh = sb("hTh", [128, KC, 1024], BF16)
            GTd = nc.dram_tensor(self.un("GTd"), [32, T], F32).ap()
            for hf in range(2):
                hsl = slice(hf * 1024, (hf + 1) * 1024)
                for kc in range(KC):
                    S.op("sp", lambda e, kc=kc, hsl=hsl: e.dma_start(out=hTh[:, kc, :], in_=self.hTd[kc, :, hsl]), reads=["hTd"], writes=[("hTh", kc)], dma=True)
                for b in range(4):
                    yb_, ybk = ybuf[b % 2], ("ybuf", b % 2)
                    S.op("sp", lambda e, b=b, yb_=yb_, hsl=hsl: e.dma_start(out=yb_[:], in_=self.yb[b][:, :, hsl].rearrange("k p t -> p k t")), reads=["yb%d" % b], writes=[ybk], dma=True)
                    for s in range(8):
                        gslot, gk = self.load_slab(W["w_gate"][b], D, s * 256)
                        bslot, bk = self.load_slab(W["w_branch"][b], 1024, s * 256)
                        for jj in range(2):
                            j = s * 2 + jj
                            for t in range(2):
                                tsl = slice(t * 512, (t + 1) * 512)
                                gt_ = hf * 2 + t
                                psg, pkg = self.next_ps()
                                self.mm_acc(psg, pkg, gslot, gk, KC, jj, lambda kc, tsl=tsl: (hTh[:, kc, tsl], [("hTh", kc)]))
                                S.op("act", lambda e, psg=psg, b=b, j=j: e.activation(out=f[3][:], in_=psg[:], func=AF.Sigmoid, bias=bg[:, b * 16 + j:b * 16 + j + 1]), reads=[pkg, "bgate"], writes=["pf3"])
                                psb_, pkb = self.next_ps()
                                self.mm_acc(psb_, pkb, bslot, bk, 8, jj, lambda kc, yb_=yb_, tsl=tsl, ybk=ybk: (yb_[:, kc, tsl], [ybk]))
                                if b == 0:
                                    S.op("dve", lambda e, psb_=psb_, j=j, tsl=tsl: e.tensor_tensor(out=acc[:, j, tsl], in0=f[3][:], in1=psb_[:], op=ALU.mult), reads=["pf3", pkb], writes=[("acc", j, t)])
                                else:
                                    S.op("dve", lambda e, psb_=psb_: e.tensor_tensor(out=f[2][:], in0=f[3][:], in1=psb_[:], op=ALU.mult), reads=["pf3", pkb], writes=["ln_f2"])
                                    S.op("dve", lambda e, j=j, tsl=tsl: e.tensor_tensor(out=acc[:, j, tsl], in0=acc[:, j, tsl], in1=f[2][:], op=ALU.add), reads=["ln_f2", ("acc", j, t)], writes=[("acc", j, t)])
                for j in range(KC):
                    S.op("act" if j % 2 else "dve",
                         (lambda e, j=j: e.copy(out=ybuf[j // 8][:, j % 8, :], in_=acc[:, j, :])) if j % 2 else
                         (lambda e, j=j: e.tensor_copy(out=ybuf[j // 8][:, j % 8, :], in_=acc[:, j, :])),
                         reads=[("acc", j, 0), ("acc", j, 1)], writes=[("ybuf", j // 8)])
                for s in range(8):
                    oslot, okk = self.load_slab(W["w_out"], D, s * 256)
                    for jj in range(2):
                        j = s * 2 + jj
                        hr_, hrk = hrt[0], ("hrt", 0)
                        S.op("sp", lambda e, j=j, hr_=hr_, hsl=hsl: e.dma_start(out=hr_[:], in_=self.hres[j, :, hsl]), reads=["hres"], writes=[hrk], dma=True)
                        for t in range(2):
                            tsl = slice(t * 512, (t + 1) * 512)
                            ps, pk = self.next_ps()
                            self.mm_acc(ps, pk, oslot, okk, KC, jj, lambda kc, tsl=tsl: (ybuf[kc // 8][:, kc % 8, tsl], [("ybuf", kc // 8)]))
                            S.op("dve", lambda e, ps=ps, j=j, tsl=tsl, hr_=hr_: e.scalar_tensor_tensor(out=acc[:, j, tsl], in0=hr_[:, tsl], scalar=ALPHA, in1=ps[:], op0=ALU.mult, op1=ALU.add),
                                 reads=[pk, hrk, ("ybuf", 0), ("ybuf", 1)], writes=[("acc", j, t)])
                self.ln_fm(acc, ybuf, tm, l1g, l1b, hf, True, False)
                ps, pk = self.next_ps()
                for g8 in range(8):
                    for kc in range(KC):
                        S.op("pe", lambda e, ps=ps, g8=g8, kc=kc: e.matmul(ps[:, g8 * 32:(g8 + 1) * 32], lhsT=ybuf[kc // 8][:, kc % 8, g8 * 128:(g8 + 1) * 128], rhs=wrb[:, kc, :], start=(kc == 0), stop=(kc == KC - 1)),
                             reads=[("ybuf", kc // 8), "wrb"], writes=[pk])
                for g8 in range(8):
                    S.op("dve", lambda e, ps=ps, g8=g8: e.tensor_tensor(out=lgt[:, g8, :], in0=ps[:, g8 * 32:(g8 + 1) * 32], in1=brt[:], op=ALU.add), reads=[pk, "brt"], writes=["lgt"])
                for g8 in range(8):
                    S.op("dve", lambda e, g8=g8: e.max(out=top8[:], in_=lgt[:, g8, :]), reads=["lgt"], writes=["top8"])
                    S.op("dve", lambda e, g8=g8: e.tensor_scalar(out=msk[:], in0=lgt[:, g8, :], scalar1=top8[:, 3:4], scalar2=None, op0=ALU.is_ge), reads=["lgt", "top8"], writes=["msk"])
                    S.op("dve", lambda e: e.tensor_scalar(out=sml[:, 0:1], in0=top8[:, 0:1], scalar1=-1.0, scalar2=None, op0=ALU.mult), reads=["top8"], writes=["sml"])
                    S.op("act", lambda e, g8=g8: e.activation(out=lgt[:, g8, :], in_=lgt[:, g8, :], func=AF.Exp, bias=sml[:, 0:1]), reads=["lgt", "sml"], writes=["lgt"])
                    S.op("dve", lambda e, g8=g8: e.tensor_tensor(out=lgt[:, g8, :], in0=lgt[:, g8, :], in1=msk[:], op=ALU.mult), reads=["lgt", "msk"], writes=["lgt"])
                    S.op("dve", lambda e, g8=g8: e.reduce_sum(out=sml[:, 1:2], in_=lgt[:, g8, :], axis=AX.X), reads=["lgt"], writes=["sml"])
                    S.op("dve", lambda e: e.reciprocal(out=sml[:, 2:3], in_=sml[:, 1:2]), reads=["sml"], writes=["sml"])
                    S.op("dve", lambda e, g8=g8: e.tensor_scalar(out=lgt[:, g8, :], in0=lgt[:, g8, :], scalar1=sml[:, 2:3], scalar2=None, op0=ALU.mult), reads=["lgt", "sml"], writes=["lgt"])
                for half2 in range(2):
                    ps, pk = self.next_ps()
                    for i in range(4):
                        g8 = half2 * 4 + i
                        S.op("pe", lambda e, ps=ps, g8=g8, i=i: e.transpose(out=ps[0:32, i * 128:(i + 1) * 128], in_=lgt[:, g8, :], identity=self.ct("ident")), reads=["lgt", "ctab"], writes=[pk])
                    S.op("dve", lambda e, ps=ps, half2=half2: e.tensor_copy(out=GT[:, half2 * 512:(half2 + 1) * 512], in_=ps[0:32, :]), reads=[pk], writes=["GT"])
                S.op("dve", lambda e: e.tensor_copy(out=GTb[:], in_=GT[:]), reads=["GT"], writes=["GTb"])
                S.op("sp", lambda e, hsl=hsl: e.dma_start(out=GTd[:, hsl], in_=GT[:]), reads=["GT"], writes=["GTd"], dma=True)
                for ex in range(NEXP):
                    S.op("sp", lambda e, ex=ex, hsl=hsl: e.dma_start(out=gbc[:], in_=GTd[ex, hsl].partition_broadcast(128)), reads=["GTd"], writes=["gbc"], dma=True)
                    for s in range(3):
                        gslot, gk = self.load_slab(W["w_up"][ex], D, s * 256)
                        lslot, lk = self.load_slab(W["w_up"][ex], D, 768 + s * 256)
                        for jj in range(2):
                            c = s * 2 + jj
                            for t in range(2):
                                tsl = slice(t * 512, (t + 1) * 512)
                                hin = lambda kc, tsl=tsl: (ybuf[kc // 8][:, kc % 8, tsl], [("ybuf", kc // 8)])
                                psg, pkg = self.next_ps()
                                self.mm_acc(psg, pkg, gslot, gk, KC, jj, hin)
                                psl, pkl = self.next_ps()
                                self.mm_acc(psl, pkl, lslot, lk, KC, jj, hin)
                                S.op("dve", lambda e, psg=psg, ex=ex, c=c: e.tensor_scalar(out=f[0][:], in0=psg[:], scalar1=bup[:, ex, c:c + 1], scalar2=7.0, op0=ALU.add, op1=ALU.min), reads=[pkg, "bup"], writes=["ln_f0"])
                                S.op("act", lambda e: e.activation(out=f[1][:], in_=f[0][:], func=AF.Sigmoid, scale=1.702), reads=["ln_f0"], writes=["ln_f1"])
                                S.op("dve", lambda e, psl=psl, ex=ex, c=c: e.tensor_scalar(out=f[2][:], in0=psl[:], scalar1=bup[:, ex, 6 + c:7 + c], scalar2=7.0, op0=ALU.add, op1=ALU.min), reads=[pkl, "bup"], writes=["ln_f2"])
                                S.op("dve", lambda e: e.tensor_scalar(out=f[2][:], in0=f[2][:], scalar1=-7.0, scalar2=1.0, op0=ALU.max, op1=ALU.add), reads=["ln_f2"], writes=["ln_f2"])
                                S.op("pool", lambda e: e.tensor_tensor(out=f[0][:], in0=f[0][:], in1=f[1][:], op=ALU.mult), reads=["ln_f0", "ln_f1"], writes=["ln_f0"])
                                S.op("pool", lambda e: e.tensor_tensor(out=f[0][:], in0=f[0][:], in1=f[2][:], op=ALU.mult), reads=["ln_f0", "ln_f2"], writes=["ln_f0"])
                                S.op("dve", lambda e, c=c, tsl=tsl: e.tensor_tensor(out=gact[:, c, tsl], in0=f[0][:], in1=gbc[:, tsl], op=ALU.mult), reads=["ln_f0", "gbc"], writes=[("gact", c, t)])
                    for s in range(8):
                        dslot, dk = self.load_slab(W["w_down"][ex], EDIM, s * 256)
                        for jj in range(2):
                            j = s * 2 + jj
                            for t in range(2):
                                tsl = slice(t * 512, (t + 1) * 512)
                                ps, pk = self.next_ps()
                                self.mm_acc(ps, pk, dslot, dk, 6, jj, lambda kc, tsl=tsl, t=t: (gact[:, kc, tsl], [("gact", kc, t)]))
                                S.op("dve", lambda e, ps=ps, j=j, tsl=tsl: e.tensor_tensor(out=acc[:, j, tsl], in0=acc[:, j, tsl], in1=ps[:], op=ALU.add), reads=[pk, ("acc", j, t)], writes=[("acc", j, t)])
                for j in range(KC):
                    for t in range(2):
                        tsl = slice(t * 512, (t + 1) * 512)
                        ps, pk = self.next_ps()
                        S.op("pe", lambda e, ps=ps, j=j, tsl=tsl: e.matmul(ps[:], lhsT=bdb[:, j * 128:(j + 1) * 128], rhs=GTb[:, tsl], start=True, stop=True), reads=["bdb", "GTb"], writes=[pk])
                        S.op("dve", lambda e, ps=ps, j=j, tsl=tsl: e.tensor_tensor(out=acc[:, j, tsl], in0=acc[:, j, tsl], in1=ps[:], op=ALU.add), reads=[pk, ("acc", j, t)], writes=[("acc", j, t)])
                self.ln_fm(acc, ybuf, tm, l2g, l2b, hf, False, True)
        S.barrier()

    def gemm_n(self, Wap, K, col0, ncols, in_fn, ntt, epi, n, slab=256):
        S = self.S
        kc_n = K // 128
        per = slab // 128
        for s in range(ncols // slab):
            si = self.wsn % len(self.wslots)
            self.wsn += 1
            slot = self.wslots[si]
            skey = ("wslot", si)
            src = Wap[:, col0 + s * slab: col0 + (s + 1) * slab].rearrange("(kc p) n -> p kc n", p=128)
            S.op("pool", lambda e, slot=slot, src=src: e.dma_start(out=slot[:, 0:kc_n, 0:slab], in_=src), writes=[skey], dma=True)
            for jj in range(per):
                j = s * per + jj
                for t in range(ntt):
                    ps, pk = self.next_ps()
                    for kc in range(kc_n):
                        rhs, rkeys = in_fn(kc, t)
                        S.op("pe", lambda e, ps=ps, slot=slot, kc=kc, jj=jj, rhs=rhs: e.matmul(
                            ps[:, 0:n], lhsT=slot[:, kc, jj * 128:(jj + 1) * 128], rhs=rhs, start=(kc == 0), stop=(kc == kc_n - 1)),
                            reads=[skey] + rkeys, writes=[pk])
                    epi(j, t, ps, pk)


def make_in_maps(inputs, nlayers, used=None):
    f = lambda a: np.ascontiguousarray(np.asarray(a, dtype=np.float32))
    x = f(inputs["x"]).reshape(8, T, D)
    mem = f(inputs["mem"])
    ln_in = np.stack([f(inputs["ln_in_g"]), f(inputs["ln_in_b"])], 0)
    shared = {}
    for l in range(nlayers):
        for n in WNAMES:
            if used is not None and ("%s_%d" % (n, l)) not in used:
                continue
            if n == "sgu_wT":
                a = np.ascontiguousarray(np.swapaxes(f(inputs["sgu_w"][l]), 1, 2))
            else:
                a = f(inputs[n][l])
            shared["%s_%d" % (n, l)] = np.ascontiguousarray(a.reshape(WSHAPES[n]))
    maps = []
    for c in range(8):
        m = dict(shared)
        m["x"] = x[c]
        m["mem"] = mem[c // 4]
        m["ln_in"] = ln_in
        m["ctab"] = make_ctab(c)
        if used is None or "rot" in used:
            m["rot"] = make_rot(c)
        maps.append(m)
    return maps


_CACHE = {}


def _get_prog(mode, first, last):
    key = (mode, first, last)
    if key not in _CACHE:
        b = Builder(1, mode=mode, first=first, last=last)
        nc = b.build()
        _CACHE[key] = (nc, set(b.used_w))
    return _CACHE[key]


def _layer_weights(inputs, l, used):
    f = lambda a: np.ascontiguousarray(np.asarray(a, dtype=np.float32))
    out = {}
    for n in WNAMES:
        nm = "%s_0" % n
        if nm not in used:
            continue
        if n == "sgu_wT":
            a = np.ascontiguousarray(np.swapaxes(f(inputs["sgu_w"][l]), 1, 2))
        else:
            a = f(inputs[n][l])
        out[nm] = np.ascontiguousarray(a.reshape(WSHAPES[n]))
    return out


def kernel(**inputs):
    f = lambda a: np.ascontiguousarray(np.asarray(a, dtype=np.float32))
    x = f(inputs["x"]).reshape(8, T, D)
    mem = f(inputs["mem"])
    ln_in = np.stack([f(inputs["ln_in_g"]), f(inputs["ln_in_b"])], 0)
    ctabs = [make_ctab(c) for c in range(8)]
    rots = [make_rot(c) for c in range(8)]
    cores = list(range(8))
    hTd = hres = None
    out = None
    for l in range(DEPTH):
        first, last = (l == 0), (l == DEPTH - 1)
        ncA, usedA = _get_prog("A", first, False)
        wA = _layer_weights(inputs, l, usedA)
        mapsA = []
        for c in cores:
            m = dict(wA)
            m["ctab"] = ctabs[c]
            if "rot" in usedA:
                m["rot"] = rots[c]
            if first:
                m["x"] = x[c]
                m["ln_in"] = ln_in
            else:
                m["hTd_i"] = hTd[c]
            mapsA.append(m)
        rA = run_bass_kernel_spmd(ncA, mapsA, core_ids=cores).results
        if first:
            hTd = [rA[c]["hTd_o"] for c in cores]
            hres = [rA[c]["hres_o"] for c in cores]
        gath = np.ascontiguousarray(np.concatenate([np.asarray(rA[c]["bounce_o"]) for c in cores], 0))
        ncB, usedB = _get_prog("B", False, last)
        wB = _layer_weights(inputs, l, usedB)
        mapsB = []
        for c in cores:
            m = dict(wB)
            m["ctab"] = ctabs[c]
            if "rot" in usedB:
                m["rot"] = rots[c]
            m["mem"] = mem[c // 4]
            m["hTd_i"] = hTd[c]
            m["hres_i"] = hres[c]
            m["aTd_i"] = rA[c]["aTd_o"]
            m["gath_i"] = gath
            mapsB.append(m)
        rB = run_bass_kernel_spmd(ncB, mapsB, core_ids=cores).results
        hTd = [rB[c]["hTd_o"] for c in cores]
        hres = [rB[c]["hres_o"] for c in cores]
        if last:
            out = np.stack([np.asarray(rB[c]["out"], dtype=np.float32) for c in cores], 0)
    return out.reshape(2, SEQ, D)
```

```python
import math
from contextlib import ExitStack
import numpy as np
import concourse.bass as bass
import concourse.mybir as mybir
from concourse.bass_utils import run_bass_kernel_spmd

F32 = mybir.dt.float32
BF16 = mybir.dt.bfloat16
AF = mybir.ActivationFunctionType
ALU = mybir.AluOpType
AX = mybir.AxisListType

D = 2048
T = 2048
NTT = 4
NCH = 16
KC = 16
SEQ = 8192
DEPTH = 4
NEXP = 32
EDIM = 768
LN_EPS = 1e-5
ALPHA = (2 * DEPTH) ** 0.25
SAME_ENGINE_SYNC = True
N_DSEM = 24
POOL_TO_DVE = True
NOCOLL = False
LNIN_LIMIT = None


class Op:
    __slots__ = ("eng", "fn", "deps", "dma", "idx", "needs_inc", "ticket", "dsem", "dcount", "n_inc")

    def __init__(self, eng, fn, dma):
        self.eng = eng
        self.fn = fn
        self.dma = dma
        self.deps = set()
        self.needs_inc = False
        self.ticket = 0
        self.dsem = -1
        self.dcount = 0
        self.n_inc = 16


class Sched:
    ENGS = ("pe", "act", "dve", "pool", "sp")

    def __init__(self, nc):
        self.nc = nc
        self.ops = []
        self.kw = {}
        self.kr = {}
        self.dsem_last = [None] * (N_DSEM + 1)
        self.dsem_cnt = [0] * (N_DSEM + 1)
        self.dsem_rr = 0
        self.dsem_rr_sw = 0
        self.last_op = {}
        self.pending_dma = []
        self.bar = {}

    def barrier(self):
        deps = set(self.last_op.values()) | set(self.pending_dma)
        for e in self.ENGS:
            self.bar[e] = set(deps) | self.bar.get(e, set())
        self.pending_dma = []

    limit = None

    def op(self, eng, fn, reads=(), writes=(), dma=False, n_inc=16):
        if self.limit is not None:
            if self.limit <= 0:
                return None
            self.limit -= 1
        if eng == "pool" and not dma and POOL_TO_DVE:
            eng = "dve"
        o = Op(eng, fn, dma)
        o.n_inc = n_inc
        o.idx = len(self.ops)
        deps = o.deps
        ops = self.ops

        def keep(d, war):
            a = ops[d]
            if a.dma or dma:
                return True
            if a.eng == eng:
                if eng in ("pe", "sp") or not SAME_ENGINE_SYNC:
                    return False
            return True

        for k in reads:
            w = self.kw.get(k)
            if w is not None and keep(w, False):
                deps.add(w)
        for k in writes:
            w = self.kw.get(k)
            if w is not None and keep(w, False):
                deps.add(w)
            for r in self.kr.get(k, ()):
                if keep(r, True):
                    deps.add(r)
        for k in reads:
            lst = self.kr.setdefault(k, [])
            if not dma:
                for i_, r_ in enumerate(lst):
                    if (not ops[r_].dma) and ops[r_].eng == eng:
                        lst[i_] = o.idx
                        break
                else:
                    lst.append(o.idx)
            else:
                lst.append(o.idx)
        for k in writes:
            self.kw[k] = o.idx
            self.kr[k] = []
        b = self.bar.pop(eng, None)
        if b:
            for d in b:
                a = ops[d]
                if a.dma or a.eng != eng:
                    deps.add(d)
        if dma:
            if n_inc == 1:
                s = N_DSEM
            elif eng == "pool":
                s = N_DSEM - 8 + (self.dsem_rr_sw % 8)
                self.dsem_rr_sw += 1
            else:
                s = self.dsem_rr
                self.dsem_rr = (s + 1) % (N_DSEM - 8)
            prev = self.dsem_last[s]
            if prev is not None:
                deps.add(prev)
            self.dsem_cnt[s] += n_inc
            o.dsem = s
            o.dcount = self.dsem_cnt[s]
            self.dsem_last[s] = o.idx
            self.pending_dma.append(o.idx)
        else:
            self.last_op[eng] = o.idx
        deps.discard(o.idx)
        self.ops.append(o)
        return o

    def emit(self):
        nc = self.nc
        ops = self.ops
        for o in ops:
            for d in o.deps:
                if not ops[d].dma:
                    ops[d].needs_inc = True
        cnt = {e: 0 for e in self.ENGS}
        for o in ops:
            if not o.dma and o.needs_inc:
                cnt[o.eng] += 1
                o.ticket = cnt[o.eng]
        with ExitStack() as st:
            esem = {e: st.enter_context(nc.semaphore("s_" + e)) for e in self.ENGS}
            dsem = [st.enter_context(nc.semaphore("d_%d" % i)) for i in range(N_DSEM + 1)]
            block = st.enter_context(nc.Block())
            per = {e: [o for o in ops if o.eng == e] for e in self.ENGS}

            def run(e, eng):
                waited = {}
                for o in per[e]:
                    need = {}
                    for d in o.deps:
                        a = ops[d]
                        if a.dma:
                            k = ("d", a.dsem)
                            v = a.dcount
                        else:
                            k = ("e", a.eng)
                            v = a.ticket
                        if v > need.get(k, 0):
                            need[k] = v
                    for k, v in need.items():
                        if waited.get(k, 0) >= v:
                            continue
                        waited[k] = v
                        sem = dsem[k[1]] if k[0] == "d" else esem[k[1]]
                        eng.wait_ge(sem, v)
                    ins = o.fn(eng)
                    if o.dma:
                        ins.then_inc(dsem[o.dsem], o.n_inc)
                    elif o.needs_inc:
                        ins.then_inc(esem[e], 1)
                if e == "sp":
                    for i in range(N_DSEM + 1):
                        if self.dsem_cnt[i] > 0:
                            eng.wait_ge(dsem[i], self.dsem_cnt[i])

            @block.sync
            def _(eng):
                run("sp", eng)

            @block.tensor
            def _(eng):
                run("pe", eng)

            @block.scalar
            def _(eng):
                run("act", eng)

            @block.vector
            def _(eng):
                run("dve", eng)

            @block.gpsimd
            def _(eng):
                run("pool", eng)


CT = {}
_off = 0
for _name, _w in (("ident", 128), ("ones", 128), ("rrot", 128), ("dpos", 128), ("dneg", 128), ("mge", 128),
                  ("mlt", 128), ("rowc1", 128), ("row128mc", 128), ("col127mc", 1), ("colc", 1),
                  ("colAf", 16), ("colAb", 16), ("xmult", 16), ("xmask", 16), ("selL", 8), ("selR", 8),
                  ("corr", 64)):
    CT[_name] = (_off, _w)
    _off += _w
NCT = _off


def make_ctab(core):
    t = np.zeros((128, NCT), np.float32)

    def put(name, arr):
        o, w = CT[name]
        t[:, o:o + w] = np.asarray(arr, np.float32).reshape(128, w)

    p = np.arange(128)
    put("ident", np.eye(128))
    put("ones", np.ones((128, 128)))
    rr = np.zeros((128, 128))
    for m in range(64):
        rr[m + 64, m] = -1.0
        rr[m, m + 64] = 1.0
    put("rrot", rr)
    mm_, cc_ = np.meshgrid(p, p, indexing="ij")
    s = 128.0 ** -0.5
    put("dpos", np.maximum(cc_ - mm_, 0))
    put("dneg", np.maximum(mm_ - cc_, 0))
    put("mge", (cc_ >= mm_) * s)
    put("mlt", (mm_ > cc_) * s)
    put("rowc1", np.tile((p + 1)[None, :], (128, 1)))
    put("row128mc", np.tile((128 - p)[None, :], (128, 1)))
    put("col127mc", 127 - p)
    put("colc", p)
    n = np.arange(16)
    put("colAf", 2047 - (n[None, :] * 128 + p[:, None]))
    put("colAb", n[None, :] * 128 + p[:, None])
    me = core % 4
    base = (core // 4) * 4
    xm = np.zeros(16)
    xk = np.zeros(16)
    for j in range(8):
        jl = j - base
        if 0 <= jl < 4 and jl < me:
            xm[j] = 2048.0 * (me - 1 - jl)
            xk[j] = 1.0
        if 0 <= jl < 4 and jl > me:
            xm[8 + j] = 2048.0 * (jl - me - 1)
            xk[8 + j] = 1.0
    put("xmult", np.tile(xm[None, :], (128, 1)))
    put("xmask", np.tile(xk[None, :], (128, 1)))
    sl = np.zeros(8)
    sr = np.zeros(8)
    if me > 0:
        sl[core - 1] = 1.0
    if me < 3:
        sr[core + 1] = 1.0
    put("selL", np.tile(sl[None, :], (128, 1)))
    put("selR", np.tile(sr[None, :], (128, 1)))
    corr = np.ones((4, 16))
    for gi, w in enumerate((2, 4, 8, 16)):
        for i in range(8):
            if me == 0:
                pos = i
                cnt = min(pos + w // 2, SEQ) - max(pos - w // 2, 0)
                corr[gi, i] = w / cnt
            if me == 3:
                pos = SEQ - 8 + i
                cnt = min(pos + w // 2, SEQ) - max(pos - w // 2, 0)
                corr[gi, 8 + i] = w / cnt
    put("corr", np.tile(corr.reshape(1, 64), (128, 1)))
    return t


def make_rot(core):
    me = core % 4
    pos = (me * 2048 + np.arange(2048)).astype(np.float32)
    inv = np.exp(-math.log(10000.0) * np.arange(0, 128, 2, dtype=np.float32) / 128).astype(np.float32)
    ang = pos[None, :] * inv[:, None]
    c = np.cos(ang).astype(np.float32)
    s = np.sin(ang).astype(np.float32)
    out = np.zeros((128, 2, 2048), np.float32)
    out[:64, 0] = c
    out[64:, 0] = c
    out[:64, 1] = s
    out[64:, 1] = s
    return out


WNAMES = ("w_in", "pool_w", "pool_scale", "ret_log_gamma", "ret_gn_g", "sgu_ln_g", "sgu_ln_b", "sgu_wT", "sgu_b",
          "w_mem_kv", "w_branch", "w_gate", "b_gate", "w_out", "ln1_g", "ln1_b", "w_router", "b_router",
          "w_up", "b_up", "w_down", "b_down", "ln2_g", "ln2_b")
WSHAPES = {
    "w_in": [D, 8192], "pool_w": [4, 256, 256], "pool_scale": [1024], "ret_log_gamma": [16], "ret_gn_g": [1024],
    "sgu_ln_g": [1024], "sgu_ln_b": [1024], "sgu_wT": [4, 128, 128], "sgu_b": [4, 128], "w_mem_kv": [D, 2048],
    "w_branch": [4, 1024, D], "w_gate": [4, D, D], "b_gate": [4, D], "w_out": [D, D], "ln1_g": [D], "ln1_b": [D],
    "w_router": [D, 32], "b_router": [32], "w_up": [32, D, 1536], "b_up": [32, 1536], "w_down": [32, 768, D],
    "b_down": [32, D], "ln2_g": [D], "ln2_b": [D],
}


class Builder:
    def __init__(self, nlayers, dbg=None, stages=None, mode="F", first=True, last=True):
        self.nlayers = nlayers
        self.dbg = dbg
        self.stages = stages
        self.mode, self.first, self.last = mode, first, last
        nc = self.nc = bass.Bass("TRN2", target_bir_lowering=False)
        self.S = Sched(nc)
        dt = nc.dram_tensor
        self.used_w = []
        self._rot_d = None
        bld = self

        class LazyW(dict):
            def __init__(self, l):
                super().__init__()
                self.l = l

            def __missing__(self, n):
                nm = "%s_%d" % (n, self.l)
                ap = dt(nm, WSHAPES[n], F32, kind="ExternalInput").ap()
                bld.used_w.append(nm)
                self[n] = ap
                return ap

        self.W = [LazyW(l) for l in range(nlayers)]
        self.ctab_d = dt("ctab", [128, NCT], F32, kind="ExternalInput").ap()
        if mode == "F" or (mode == "A" and first):
            self.x = dt("x", [T, D], F32, kind="ExternalInput").ap()
            self.ln_in = dt("ln_in", [2, D], F32, kind="ExternalInput").ap()
        if mode in ("F", "B"):
            self.mem = dt("mem", [256, D], F32, kind="ExternalInput").ap()
        if mode == "F" or (mode == "B" and last):
            self.out = dt("out", [T, D], F32, kind="ExternalOutput").ap()
        if dbg:
            self.dbg_out = dt("dbg", [KC, 128, T], F32, kind="ExternalOutput").ap()
        ko = "ExternalOutput"
        ki = "ExternalInput"
        if mode == "F":
            self.hres = dt("hres", [KC, 128, T], F32).ap()
            self.hTd = dt("hTd", [KC, 128, T], BF16).ap()
            self.aTd = dt("aTd", [8, 128, T], F32).ap()
            self.bounce = dt("bounce", [17 * 128, 128], F32).ap()
            self.gath = dt("gath", [8 * 17 * 128, 128], F32).ap()
        elif mode == "A":
            if first:
                self.hres = dt("hres_o", [KC, 128, T], F32, kind=ko).ap()
                self.hTd = dt("hTd_o", [KC, 128, T], BF16, kind=ko).ap()
            else:
                self.hTd = dt("hTd_i", [KC, 128, T], BF16, kind=ki).ap()
            self.aTd = dt("aTd_o", [8, 128, T], F32, kind=ko).ap()
            self.bounce = dt("bounce_o", [17 * 128, 128], F32, kind=ko).ap()
            self.gath = None
        else:
            self.hres_i = dt("hres_i", [KC, 128, T], F32, kind=ki).ap()
            self.hTd_i = dt("hTd_i", [KC, 128, T], BF16, kind=ki).ap()
            self.hres = dt("hres_o", [KC, 128, T], F32, kind=ko).ap()
            self.hTd = dt("hTd_o", [KC, 128, T], BF16, kind=ko).ap()
            self.aTd = dt("aTd_i", [8, 128, T], F32, kind=ki).ap()
            self.gath = dt("gath_i", [8 * 17 * 128, 128], F32, kind=ki).ap()
            self.bounce = None
        if mode != "A":
            self.yb = [dt("yb%d" % i, [8, 128, T], BF16).ap() for i in range(4)]
        self.psn = 0
        self.wsn = 0

    @property
    def rot_d(self):
        if self._rot_d is None:
            self._rot_d = self.nc.dram_tensor("rot", [128, 2, T], F32, kind="ExternalInput").ap()
            self.used_w.append("rot")
        return self._rot_d

    def un(self, name):
        self.uid = getattr(self, "uid", 0) + 1
        return "%s_u%d" % (name, self.uid)

    def next_ps(self):
        i = self.psn % len(self.ps)
        self.psn += 1
        return self.ps[i], ("ps", i)

    def next_psb(self):
        i = self.psn % len(self.psb)
        self.psn += 1
        return self.psb[i], ("psb", i)

    def ct(self, name):
        o, w = CT[name]
        return self.ctab[:, o:o + w]

    def bcast_load(self, st, name, vec_ap, n):
        t = st.enter_context(self.nc.sbuf_tensor(self.un(name), [128, n], F32))
        self.S.op("sp", lambda e: e.dma_start(out=t[:], in_=vec_ap.partition_broadcast(128)), writes=[name], dma=True)
        return t

    def col_load(self, st, name, vec_ap, nchunk):
        t = st.enter_context(self.nc.sbuf_tensor(self.un(name), [128, nchunk], F32))
        self.S.op("sp", lambda e: e.dma_start(out=t[:], in_=vec_ap.rearrange("(c p) -> p c", p=128), allow_slow_non_contiguous=True),
                  writes=[name], dma=True)
        return t

    def gemm(self, Wap, K, col0, ncols, in_fn, ntt, epi, slab=256):
        S = self.S
        kc_n = K // 128
        per = slab // 128
        for s in range(ncols // slab):
            si = self.wsn % len(self.wslots)
            self.wsn += 1
            slot = self.wslots[si]
            skey = ("wslot", si)
            src = Wap[:, col0 + s * slab: col0 + (s + 1) * slab].rearrange("(kc p) n -> p kc n", p=128)
            S.op("pool", lambda e, slot=slot, src=src: e.dma_start(out=slot[:, 0:kc_n, 0:slab], in_=src),
                 writes=[skey], dma=True)
            for jj in range(per):
                j = s * per + jj
                for t in range(ntt):
                    ps, pk = self.next_ps()
                    for kc in range(kc_n):
                        rhs, rkeys = in_fn(kc, t)
                        S.op("pe", lambda e, ps=ps, slot=slot, kc=kc, jj=jj, rhs=rhs: e.matmul(
                            ps[:], lhsT=slot[:, kc, jj * 128:(jj + 1) * 128], rhs=rhs, start=(kc == 0), stop=(kc == kc_n - 1)),
                            reads=[skey] + rkeys, writes=[pk])
                    epi(j, t, ps, pk)

    def load_hT(self, st):
        self.hT = st.enter_context(self.nc.sbuf_tensor(self.un("hT"), [128, KC, T], BF16))
        for kc in range(KC):
            self.S.op("sp", lambda e, kc=kc, hT=self.hT: e.dma_start(out=hT[:, kc, :], in_=self.hTd[kc]), reads=["hTd"],
                      writes=[("hT", kc, t) for t in range(NTT)], dma=True)

    def hT_in(self, kc, t):
        return self.hT[:, kc, t * 512:(t + 1) * 512], [("hT", kc, t)]

    def build(self):
        nc, S = self.nc, self.S
        with ExitStack() as st:
            sb = lambda name, shape, dt: st.enter_context(nc.sbuf_tensor(self.un(name), shape, dt))
            self.ctab = sb("ctab_sb", [128, NCT], F32)
            self.identb = sb("identb", [128, 128], BF16)
            self.onesb = sb("onesb", [128, 128], BF16)
            self.rrotb = sb("rrotb", [128, 128], BF16)
            self.wslots = [sb("wslot%d" % i, [128, 16, 256], BF16) for i in range(3)]
            self.ps = [st.enter_context(nc.psum_tensor("ps%d" % i, [128, 512], F32)) for i in range(6)]
            self.psb = [st.enter_context(nc.psum_tensor("psb%d" % i, [128, 1024], BF16)) for i in range(2)]
            S.op("sp", lambda e: e.dma_start(out=self.ctab[:], in_=self.ctab_d), writes=["ctab"], dma=True)
            S.op("dve", lambda e: e.tensor_copy(out=self.identb[:], in_=self.ct("ident")), reads=["ctab"], writes=["identb"])
            S.op("dve", lambda e: e.tensor_copy(out=self.onesb[:], in_=self.ct("ones")), reads=["ctab"], writes=["onesb"])
            S.op("dve", lambda e: e.tensor_copy(out=self.rrotb[:], in_=self.ct("rrot")), reads=["ctab"], writes=["rrotb"])
            sg = self.stages
            if self.mode == "F":
                if sg is None or "nolnin" not in sg:
                    self.stage_ln_in()
                for l in range(self.nlayers):
                    self.layer(l)
                if sg is None or "nooutput" not in sg:
                    self.stage_output()
            elif self.mode == "A":
                if self.first:
                    self.stage_ln_in()
                with ExitStack() as st_tab:
                    self.layer_tables(0, st_tab)
                    self.stage_pre_exchange(0, st_tab)
            else:
                S.op("sp", lambda e: e.dma_start(out=self.hres, in_=self.hres_i), writes=["hres"], dma=True)
                S.op("sp", lambda e: e.dma_start(out=self.hTd, in_=self.hTd_i), writes=["hTd"], dma=True)
                S.barrier()
                with ExitStack() as st_tab:
                    self.layer_tables(0, st_tab)
                    self.stage_sgu(0)
                    self.stage_mem(0)
                    self.stage_pool_b(0)
                    self.stage_ret_b(0)
                    self.stage_post(0)
                if self.last:
                    self.stage_output()
            S.barrier()
            S.emit()
        return nc

    def stage_ln_in(self):
        nc, S = self.nc, self.S
        S.barrier()
        S.limit = LNIN_LIMIT
        with ExitStack() as st:
            sb = lambda name, shape, dt: st.enter_context(nc.sbuf_tensor(self.un(name), shape, dt))
            self.hT = sb("hT_lnin", [128, KC, T], BF16)
            gt = self.bcast_load(st, "lnin_g", self.ln_in[0], D)
            bt = self.bcast_load(st, "lnin_b", self.ln_in[1], D)
            xt = [sb("lnin_x%d" % i, [128, D], F32) for i in range(2)]
            sq = sb("lnin_sq", [128, D], F32)
            hr = [sb("lnin_hr%d" % i, [128, KC, 128], F32) for i in range(2)]
            stat = [sb("lnin_st%d" % i, [128, 4], F32) for i in range(2)]
            for n in range(NCH):
                b = n % 2
                x_, s_, h_ = xt[b], stat[b], hr[b]
                xk, sk, hk = ("lnx", b), ("lnst", b), ("lnhr", b)
                S.op("sp", lambda e, x_=x_, n=n: e.dma_start(out=x_[:], in_=self.x[n * 128:(n + 1) * 128, :]), writes=[xk], dma=True)
                S.op("dve", lambda e, x_=x_, s_=s_: e.reduce_sum(out=s_[:, 0:1], in_=x_[:], axis=AX.X), reads=[xk], writes=[sk])
                S.op("dve", lambda e, s_=s_: e.tensor_scalar(out=s_[:, 1:2], in0=s_[:, 0:1], scalar1=1.0 / D, scalar2=None, op0=ALU.mult), reads=[sk], writes=[sk])
                S.op("dve", lambda e, x_=x_, s_=s_: e.tensor_scalar(out=x_[:], in0=x_[:], scalar1=s_[:, 1:2], scalar2=None, op0=ALU.subtract), reads=[xk, sk], writes=[xk])
                S.op("dve", lambda e, x_=x_: e.tensor_tensor(out=sq[:], in0=x_[:], in1=x_[:], op=ALU.mult), reads=[xk], writes=["lnsq"])
                S.op("dve", lambda e, s_=s_: e.reduce_sum(out=s_[:, 2:3], in_=sq[:], axis=AX.X), reads=["lnsq"], writes=[sk])
                S.op("dve", lambda e, s_=s_: e.tensor_scalar(out=s_[:, 2:3], in0=s_[:, 2:3], scalar1=1.0 / D, scalar2=LN_EPS, op0=ALU.mult, op1=ALU.add), reads=[sk], writes=[sk])
                S.op("act", lambda e, s_=s_: e.activation(out=s_[:, 3:4], in_=s_[:, 2:3], func=AF.Sqrt), reads=[sk], writes=[sk])
                S.op("dve", lambda e, s_=s_: e.reciprocal(out=s_[:, 3:4], in_=s_[:, 3:4]), reads=[sk], writes=[sk])
                S.op("dve", lambda e, x_=x_, s_=s_: e.tensor_scalar(out=x_[:], in0=x_[:], scalar1=s_[:, 3:4], scalar2=None, op0=ALU.mult), reads=[xk, sk], writes=[xk])
                S.op("dve", lambda e, x_=x_: e.tensor_tensor(out=x_[:], in0=x_[:], in1=gt[:], op=ALU.mult), reads=[xk, "lnin_g"], writes=[xk])
                S.op("dve", lambda e, x_=x_: e.tensor_tensor(out=x_[:], in0=x_[:], in1=bt[:], op=ALU.add), reads=[xk, "lnin_b"], writes=[xk])
                for k4 in range(4):
                    ps, pk = self.next_ps()
                    for i in range(4):
                        k = k4 * 4 + i
                        S.op("pe", lambda e, ps=ps, x_=x_, k=k, i=i: e.transpose(out=ps[:, i * 128:(i + 1) * 128], in_=x_[:, k * 128:(k + 1) * 128], identity=self.ct("ident")),
                             reads=[xk, "ctab"], writes=[pk])
                    for i in range(4):
                        S.op("act", lambda e, ps=ps, k4=k4, n=n, i=i, hT=self.hT: e.copy(out=hT[:, k4 * 4 + i, n * 128:(n + 1) * 128], in_=ps[:, i * 128:(i + 1) * 128]),
                             reads=[pk], writes=[("hT", k4 * 4 + i, n // 4)])
                    S.op("dve", lambda e, ps=ps, k4=k4, h_=h_: e.tensor_copy(out=h_[:, k4 * 4:k4 * 4 + 4, :], in_=ps[:].rearrange("p (a b) -> p a b", a=4)),
                         reads=[pk] + [("hT", k4 * 4 + i, n // 4) for i in range(4)], writes=[hk])
                S.op("sp", lambda e, h_=h_, n=n: e.dma_start(out=self.hres[:, :, n * 128:(n + 1) * 128].rearrange("k p t -> p k t"), in_=h_[:]),
                     reads=[hk], writes=["hres"], dma=True)
            for kc in range(KC):
                S.op("sp", lambda e, kc=kc, hT=self.hT: e.dma_start(out=self.hTd[kc], in_=hT[:, kc, :]), reads=[("hT", kc, t) for t in range(NTT)], writes=["hTd"], dma=True)
        S.limit = None
        S.barrier()

    def stage_mem_prep(self, st_outer):
        nc, S = self.nc, self.S
        self.memT = st_outer.enter_context(nc.sbuf_tensor(self.un("memT"), [128, KC, 256], BF16))
        with ExitStack() as st:
            sb = lambda name, shape, dt: st.enter_context(nc.sbuf_tensor(self.un(name), shape, dt))
            mf = sb("mem_f", [128, 2, D], F32)
            mb = sb("mem_b", [128, 2, D], BF16)
            S.op("sp", lambda e: e.dma_start(out=mf[:], in_=self.mem.rearrange("(c p) n -> p c n", p=128)), writes=["mem_f"], dma=True)
            S.op("dve", lambda e: e.tensor_copy(out=mb[:], in_=mf[:]), reads=["mem_f"], writes=["mem_b"])
            for c in range(2):
                for k8 in range(2):
                    ps, pk = self.next_psb()
                    for i in range(8):
                        k = k8 * 8 + i
                        S.op("pe", lambda e, ps=ps, c=c, k=k, i=i: e.transpose(out=ps[:, i * 128:(i + 1) * 128], in_=mb[:, c, k * 128:(k + 1) * 128], identity=self.identb[:]),
                             reads=["mem_b", "identb"], writes=[pk])
                    S.op("dve", lambda e, ps=ps, c=c, k8=k8, memT=self.memT: e.tensor_copy(out=memT[:, k8 * 8:k8 * 8 + 8, c * 128:(c + 1) * 128], in_=ps[:].rearrange("p (a b) -> p a b", a=8)),
                         reads=[pk], writes=["memT"])
        S.barrier()

    def dbg_dump_yb(self, bi):
        nc, S = self.nc, self.S
        S.barrier()
        with ExitStack() as st:
            a = st.enter_context(nc.sbuf_tensor("dbg_a", [128, 8, T], BF16))
            b = st.enter_context(nc.sbuf_tensor("dbg_b", [128, 8, T], F32))
            S.op("sp", lambda e: e.dma_start(out=a[:], in_=self.yb[bi].rearrange("k p t -> p k t")), writes=["dbg_a"], dma=True)
            S.op("dve", lambda e: e.tensor_copy(out=b[:], in_=a[:]), reads=["dbg_a"], writes=["dbg_b"])
            S.op("sp", lambda e: e.dma_start(out=self.dbg_out[0:8].rearrange("k p t -> p k t"), in_=b[:]), reads=["dbg_b"], writes=["dbg"], dma=True)
        S.barrier()

    def stage_output(self):
        nc, S = self.nc, self.S
        S.barrier()
        with ExitStack() as st:
            sb = lambda name, shape, dt: st.enter_context(nc.sbuf_tensor(self.un(name), shape, dt))
            hin = [sb("o_in%d" % i, [128, KC, 128], F32) for i in range(2)]
            ot = [sb("o_t%d" % i, [128, D], F32) for i in range(2)]
            for n in range(NCH):
                b = n % 2
                S.op("sp", lambda e, b=b, n=n: e.dma_start(out=hin[b][:], in_=self.hres[:, :, n * 128:(n + 1) * 128].rearrange("k p t -> p k t")),
                     reads=["hres"], writes=[("o_in", b)], dma=True)
                for k4 in range(4):
                    ps, pk = self.next_ps()
                    for i in range(4):
                        k = k4 * 4 + i
                        S.op("pe", lambda e, ps=ps, b=b, k=k, i=i: e.transpose(out=ps[:, i * 128:(i + 1) * 128], in_=hin[b][:, k, :], identity=self.ct("ident")),
                             reads=[("o_in", b), "ctab"], writes=[pk])
                    S.op("dve" if k4 % 2 == 0 else "act",
                         (lambda e, ps=ps, b=b, k4=k4: e.tensor_copy(out=ot[b][:, k4 * 512:(k4 + 1) * 512], in_=ps[:])) if k4 % 2 == 0 else
                         (lambda e, ps=ps, b=b, k4=k4: e.copy(out=ot[b][:, k4 * 512:(k4 + 1) * 512], in_=ps[:])),
                         reads=[pk], writes=[("o_t", b)])
                S.op("sp", lambda e, b=b, n=n: e.dma_start(out=self.out[n * 128:(n + 1) * 128, :], in_=ot[b][:]), reads=[("o_t", b)], writes=["out"], dma=True)
        S.barrier()

    def layer(self, l):
        stg = self.stages
        with ExitStack() as st_tab:
            self._layer(l, st_tab)

    def _layer(self, l, st_tab):
        stg = self.stages
        self.S.barrier()
        if stg is None or "pre" in stg:
            self.layer_tables(l, st_tab)
            self.stage_pre_exchange(l, st_tab)
        if stg is None or "sgu" in stg:
            self.stage_sgu(l)
        if stg is None or "mem" in stg:
            self.stage_mem(l)
        if stg is None or "poolb" in stg:
            self.stage_pool_b(l)
        if stg is None or "retb" in stg:
            self.stage_ret_b(l)
        if stg is None or "post" in stg:
            self.stage_post(l)
        if self.dbg and self.dbg.startswith("yb") and l == 0:
            self.dbg_dump_yb(int(self.dbg[2]))

    def gelu_epi(self, ps, pk, out_ap, out_keys, tmps):
        S = self.S
        i = self.psn % len(tmps)
        tm, tk = tmps[i], ("gelu_tmp", i)
        S.op("act", lambda e: e.activation(out=tm[:], in_=ps[:], func=AF.Square), reads=[pk], writes=[tk])
        S.op("dve", lambda e: e.tensor_scalar(out=tm[:], in0=tm[:], scalar1=0.044715, scalar2=1.0, op0=ALU.mult, op1=ALU.add), reads=[tk], writes=[tk])
        S.op("dve", lambda e: e.tensor_tensor(out=tm[:], in0=tm[:], in1=ps[:], op=ALU.mult), reads=[tk, pk], writes=[tk])
        S.op("act", lambda e: e.activation(out=tm[:], in_=tm[:], func=AF.Sigmoid, scale=1.5957691216057308), reads=[tk], writes=[tk])
        S.op("dve", lambda e: e.tensor_tensor(out=out_ap, in0=tm[:], in1=ps[:], op=ALU.mult), reads=[tk, pk], writes=out_keys)

    def stage_sgu(self, l):
        nc, S, W = self.nc, self.S, self.W[l]
        S.barrier()
        with ExitStack() as st:
            sb = lambda name, shape, dt: st.enter_context(nc.sbuf_tensor(self.un(name), shape, dt))
            self.load_hT(st)
            guT = sb("sgu_gu", [128, 8, T], BF16)
            gvT = sb("sgu_gv", [128, 8, T], BF16)
            lng = self.col_load(st, "sgu_lng", W["sgu_ln_g"], 8)
            lnb = self.col_load(st, "sgu_lnb", W["sgu_ln_b"], 8)
            bsb = self.bcast_load(st, "sgu_bs", W["sgu_b"].rearrange("g p -> (g p)"), 512)
            wsf = sb("sgu_wsf", [128, 4, 128], F32)
            wsb = sb("sgu_wsb", [128, 4, 128], BF16)
            S.op("sp", lambda e: e.dma_start(out=wsf[:], in_=W["sgu_wT"].rearrange("g q p -> q g p")), writes=["sgu_wsf"], dma=True)
            S.op("dve", lambda e: e.tensor_copy(out=wsb[:], in_=wsf[:]), reads=["sgu_wsf"], writes=["sgu_wsb"])
            tmps = [sb("sgu_tmp%d" % i, [128, 512], F32) for i in range(3)]

            def epi_u(j, t, ps, pk):
                self.gelu_epi(ps, pk, guT[:, j, t * 512:(t + 1) * 512], [("sgu_gu", j, t)], tmps)

            def epi_v(j, t, ps, pk):
                self.gelu_epi(ps, pk, gvT[:, j, t * 512:(t + 1) * 512], [("sgu_gv", j, t)], tmps)

            self.gemm(W["w_in"], D, 5120, 1024, self.hT_in, NTT, epi_u)
            self.gemm(W["w_in"], D, 6144, 1024, self.hT_in, NTT, epi_v)

            sqb = [sb("sgu_sq%d" % i, [128, 512], BF16) for i in range(2)]
            mean = sb("sgu_mean", [128, 512], F32)
            rstd = sb("sgu_rstd", [128, 512], F32)
            vtmp = tmps[0:2]
            vn = sb("sgu_vn", [128, 8, 512], BF16)
            vtm = sb("sgu_vtm", [128, 4, 1024], BF16)
            ot = [sb("sgu_o%d" % i, [128, 8, 512], BF16) for i in range(1)]
            otmp = vtmp
            for t in range(NTT):
                tsl = slice(t * 512, (t + 1) * 512)
                ps_s, pk_s = self.next_ps()
                for c in range(8):
                    S.op("pe", lambda e, tsl=tsl, c=c, ps_s=ps_s: e.matmul(ps_s[:], lhsT=self.onesb[:], rhs=gvT[:, c, tsl], start=(c == 0), stop=(c == 7)),
                         reads=["onesb", ("sgu_gv", c, t)], writes=[pk_s])
                ps_q, pk_q = self.next_ps()
                for c in range(8):
                    q_, qk = sqb[c % 2], ("sgu_sq", c % 2)
                    S.op("dve", lambda e, tsl=tsl, c=c, q_=q_: e.tensor_tensor(out=q_[:], in0=gvT[:, c, tsl], in1=gvT[:, c, tsl], op=ALU.mult),
                         reads=[("sgu_gv", c, t)], writes=[qk])
                    S.op("pe", lambda e, c=c, q_=q_, ps_q=ps_q: e.matmul(ps_q[:], lhsT=self.onesb[:], rhs=q_[:], start=(c == 0), stop=(c == 7)),
                         reads=["onesb", qk], writes=[pk_q])
                S.op("act", lambda e, ps_s=ps_s: e.mul(out=mean[:], in_=ps_s[:], mul=1.0 / 1024), reads=[pk_s], writes=["sgu_mean"])
                S.op("dve", lambda e: e.tensor_tensor(out=rstd[:], in0=mean[:], in1=mean[:], op=ALU.mult), reads=["sgu_mean"], writes=["sgu_rstd"])
                S.op("dve", lambda e, ps_q=ps_q: e.scalar_tensor_tensor(out=rstd[:], in0=ps_q[:], scalar=1.0 / 1024, in1=rstd[:], op0=ALU.mult, op1=ALU.subtract),
                     reads=[pk_q, "sgu_rstd"], writes=["sgu_rstd"])
                S.op("dve", lambda e: e.tensor_scalar(out=rstd[:], in0=rstd[:], scalar1=LN_EPS, scalar2=None, op0=ALU.add), reads=["sgu_rstd"], writes=["sgu_rstd"])
                S.op("act", lambda e: e.activation(out=rstd[:], in_=rstd[:], func=AF.Sqrt), reads=["sgu_rstd"], writes=["sgu_rstd"])
                S.op("dve", lambda e: e.reciprocal(out=rstd[:], in_=rstd[:]), reads=["sgu_rstd"], writes=["sgu_rstd"])
                for c in range(8):
                    v_, vk = vtmp[c % 2], ("gelu_tmp", c % 2)
                    S.op("dve", lambda e, tsl=tsl, c=c, v_=v_: e.tensor_tensor(out=v_[:], in0=gvT[:, c, tsl], in1=mean[:], op=ALU.subtract),
                         reads=[("sgu_gv", c, t), "sgu_mean"], writes=[vk])
                    S.op("dve", lambda e, v_=v_: e.tensor_tensor(out=v_[:], in0=v_[:], in1=rstd[:], op=ALU.mult), reads=[vk, "sgu_rstd"], writes=[vk])
                    S.op("act", lambda e, c=c, v_=v_: e.activation(out=vn[:, c, :], in_=v_[:], func=AF.Identity, scale=lng[:, c:c + 1], bias=lnb[:, c:c + 1]),
                         reads=[vk, "sgu_lng", "sgu_lnb"], writes=[("sgu_vn", c)])
                for n4 in range(4):
                    pb, pbk = self.next_psb()
                    for c in range(8):
                        S.op("pe", lambda e, pb=pb, c=c, n4=n4: e.transpose(out=pb[:, c * 128:(c + 1) * 128], in_=vn[:, c, n4 * 128:(n4 + 1) * 128], identity=self.identb[:]),
                             reads=[("sgu_vn", c), "identb"], writes=[pbk])
                    if n4 % 2 == 0:
                        S.op("dve", lambda e, pb=pb, n4=n4: e.tensor_copy(out=vtm[:, n4, :], in_=pb[:]), reads=[pbk], writes=[("sgu_vtm", n4)])
                    else:
                        S.op("act", lambda e, pb=pb, n4=n4: e.copy(out=vtm[:, n4, :], in_=pb[:]), reads=[pbk], writes=[("sgu_vtm", n4)])
                o_, ok = ot[0], ("sgu_o", 0)
                for c in range(8):
                    g = c // 2
                    ps, pk = self.next_ps()
                    for n4 in range(4):
                        S.op("pe", lambda e, ps=ps, c=c, n4=n4, g=g: e.matmul(ps[:, n4 * 128:(n4 + 1) * 128], lhsT=vtm[:, n4, c * 128:(c + 1) * 128], rhs=wsb[:, g, :], start=True, stop=True),
                             reads=[("sgu_vtm", n4), "sgu_wsb"], writes=[pk])
                    x_, xk = otmp[c % 2], ("gelu_tmp", c % 2)
                    for n4 in range(4):
                        S.op("dve", lambda e, ps=ps, x_=x_, n4=n4, g=g: e.tensor_tensor(out=x_[:, n4 * 128:(n4 + 1) * 128], in0=ps[:, n4 * 128:(n4 + 1) * 128], in1=bsb[:, g * 128:(g + 1) * 128], op=ALU.add),
                             reads=[pk, "sgu_bs"], writes=[xk])
                    S.op("dve", lambda e, tsl=tsl, x_=x_, o_=o_, c=c: e.tensor_tensor(out=o_[:, c, :], in0=x_[:], in1=guT[:, c, tsl], op=ALU.mult),
                         reads=[xk, ("sgu_gu", c, t)], writes=[ok])
                S.op("sp", lambda e, tsl=tsl, o_=o_: e.dma_start(out=self.yb[2][:, :, tsl].rearrange("k p t -> p k t"), in_=o_[:]), reads=[ok], writes=["yb2"], dma=True)
        S.barrier()

    def stage_mem(self, l):
        nc, S, W = self.nc, self.S, self.W[l]
        S.barrier()
        with ExitStack() as st:
            sb = lambda name, shape, dt: st.enter_context(nc.sbuf_tensor(self.un(name), shape, dt))
            self.load_hT(st)
            self.stage_mem_prep(st)
            kvT = sb("mem_kvT", [128, 16, 256], BF16)
            vtm = sb("mem_vtm", [128, 2, 1024], BF16)
            qT = sb("mem_qT", [128, 8, T], BF16)

            def memT_in(kc, t):
                return self.memT[:, kc, :], ["memT"]

            def epi_kv(j, t, ps, pk):
                if j % 2 == 0:
                    S.op("act", lambda e: e.copy(out=kvT[:, j, :], in_=ps[:, 0:256]), reads=[pk], writes=[("mem_kvT", j)])
                else:
                    S.op("dve", lambda e: e.tensor_copy(out=kvT[:, j, :], in_=ps[:, 0:256]), reads=[pk], writes=[("mem_kvT", j)])

            self.gemm_n(W["w_mem_kv"], D, 0, 2048, memT_in, 1, epi_kv, 256)
            for c in range(8):
                pb, pbk = self.next_psb()
                for mc in range(2):
                    S.op("pe", lambda e, pb=pb, c=c, mc=mc: e.transpose(out=pb[:, mc * 128:(mc + 1) * 128], in_=kvT[:, 8 + c, mc * 128:(mc + 1) * 128], identity=self.identb[:]),
                         reads=[("mem_kvT", 8 + c), "identb"], writes=[pbk])
                S.op("dve", lambda e, pb=pb, c=c: e.tensor_copy(out=vtm[:, :, c * 128:(c + 1) * 128], in_=pb[:, 0:256].rearrange("p (a b) -> p a b", a=2)),
                     reads=[pbk], writes=["mem_vtm"])

            def epi_q(j, t, ps, pk):
                if (j + t) % 2 == 0:
                    S.op("act", lambda e: e.copy(out=qT[:, j, t * 512:(t + 1) * 512], in_=ps[:]), reads=[pk], writes=[("mem_qT", j, t)])
                else:
                    S.op("dve", lambda e: e.tensor_copy(out=qT[:, j, t * 512:(t + 1) * 512], in_=ps[:]), reads=[pk], writes=[("mem_qT", j, t)])

            self.gemm(W["w_in"], D, 7168, 1024, self.hT_in, NTT, epi_q)
            ex = [sb("mem_ex%d" % i, [128, 2, 512], BF16) for i in range(2)]
            rc = [sb("mem_rc%d" % i, [128, 512], F32) for i in range(2)]
            ot = [sb("mem_o%d" % i, [128, 8, 512], BF16) for i in range(2)]
            it = 0
            for t in range(NTT):
                tsl = slice(t * 512, (t + 1) * 512)
                o_, ok = ot[t % 2], ("mem_o", t % 2)
                for hd in range(4):
                    e_, ek = ex[it % 2], ("mem_ex", it % 2)
                    r_, rk = rc[it % 2], ("mem_rc", it % 2)
                    it += 1
                    for mc in range(2):
                        ps, pk = self.next_ps()
                        for dc in range(2):
                            S.op("pe", lambda e, tsl=tsl, ps=ps, hd=hd, dc=dc, mc=mc: e.matmul(ps[:], lhsT=kvT[:, hd * 2 + dc, mc * 128:(mc + 1) * 128], rhs=qT[:, hd * 2 + dc, tsl], start=(dc == 0), stop=(dc == 1)),
                                 reads=[("mem_kvT", hd * 2 + dc), ("mem_qT", hd * 2 + dc, t)], writes=[pk])
                        S.op("act", lambda e, ps=ps, e_=e_, mc=mc: e.activation(out=e_[:, mc, :], in_=ps[:], func=AF.Exp, scale=1.0 / 16), reads=[pk], writes=[ek])
                    ps_s, pk_s = self.next_ps()
                    for mc in range(2):
                        S.op("pe", lambda e, ps_s=ps_s, e_=e_, mc=mc: e.matmul(ps_s[:], lhsT=self.onesb[:], rhs=e_[:, mc, :], start=(mc == 0), stop=(mc == 1)),
                             reads=["onesb", ek], writes=[pk_s])
                    S.op("dve", lambda e, ps_s=ps_s, r_=r_: e.reciprocal(out=r_[:], in_=ps_s[:]), reads=[pk_s], writes=[rk])
                    for c2 in range(2):
                        ps, pk = self.next_ps()
                        for mc in range(2):
                            S.op("pe", lambda e, ps=ps, hd=hd, c2=c2, mc=mc, e_=e_: e.matmul(ps[:], lhsT=vtm[:, mc, hd * 256 + c2 * 128: hd * 256 + (c2 + 1) * 128], rhs=e_[:, mc, :], start=(mc == 0), stop=(mc == 1)),
                                 reads=["mem_vtm", ek], writes=[pk])
                        S.op("dve", lambda e, ps=ps, o_=o_, hd=hd, c2=c2, r_=r_: e.tensor_tensor(out=o_[:, hd * 2 + c2, :], in0=ps[:], in1=r_[:], op=ALU.mult),
                             reads=[pk, rk], writes=[ok])
                S.op("sp", lambda e, tsl=tsl, o_=o_: e.dma_start(out=self.yb[3][:, :, tsl].rearrange("k p t -> p k t"), in_=o_[:]), reads=[ok], writes=["yb3"], dma=True)
        S.barrier()

    def layer_tables(self, l, st):
        nc, S, W = self.nc, self.S, self.W[l]
        sb = lambda name, shape, dt: st.enter_context(nc.sbuf_tensor(self.un(name), shape, dt))
        lg = self.bcast_load(st, "lgbc", W["ret_log_gamma"], 16)
        self.lgbc = lg
        self.wA = sb("wA", [128, 2, 16, 8], F32)
        self.wk = sb("wk", [128, 2, 8], F32)
        self.g128 = sb("g128", [128, 16], F32)
        self.cf = sb("cf", [128, 16, 8], F32)
        wA, wk, g128, cf = self.wA, self.wk, self.g128, self.cf
        colAf, colAb = self.ct("colAf"), self.ct("colAb")
        for n in range(16):
            S.op("act", lambda e, n=n: e.activation(out=wA[:, 0, n, :], in_=lg[:, 0:8], func=AF.Exp, scale=colAf[:, n:n + 1]),
                 reads=["lgbc", "ctab"], writes=["wA"])
            S.op("act", lambda e, n=n: e.activation(out=wA[:, 1, n, :], in_=lg[:, 8:16], func=AF.Exp, scale=colAb[:, n:n + 1]),
                 reads=["lgbc", "ctab"], writes=["wA"])
        S.op("act", lambda e: e.activation(out=wk[:, 0, :], in_=lg[:, 0:8], func=AF.Exp, scale=self.ct("col127mc")), reads=["lgbc", "ctab"], writes=["wk"])
        S.op("act", lambda e: e.activation(out=wk[:, 1, :], in_=lg[:, 8:16], func=AF.Exp, scale=self.ct("colc")), reads=["lgbc", "ctab"], writes=["wk"])
        S.op("act", lambda e: e.activation(out=g128[:], in_=lg[:], func=AF.Exp, scale=128.0), reads=["lgbc"], writes=["g128"])
        xm, xk = self.ct("xmult"), self.ct("xmask")
        for r in range(16):
            src = lg[:, 0:8] if r < 8 else lg[:, 8:16]
            S.op("act", lambda e, r=r, src=src: e.activation(out=cf[:, r, :], in_=src, func=AF.Exp, scale=xm[:, r:r + 1]), reads=["lgbc", "ctab"], writes=["cf"])
            S.op("dve", lambda e, r=r: e.tensor_scalar(out=cf[:, r, :], in0=cf[:, r, :], scalar1=xk[:, r:r + 1], scalar2=None, op0=ALU.mult), reads=["cf", "ctab"], writes=["cf"])

    def rot_tables(self, st):
        nc, S = self.nc, self.S
        rt = st.enter_context(nc.sbuf_tensor(self.un("rot_sb"), [128, 2, T], F32))
        S.op("sp", lambda e: e.dma_start(out=rt[:], in_=self.rot_d), writes=["rot_sb"], dma=True)
        return rt

    def rotary(self, pre, prekey, out, outkey, rt, tmps, t):
        S = self.S
        tsl = slice(t * 512, (t + 1) * 512)
        ps, pk = self.next_ps()
        S.op("pe", lambda e: e.matmul(ps[:], lhsT=self.rrotb[:], rhs=pre[:, tsl], start=True, stop=True), reads=["rrotb", prekey], writes=[pk])
        i = self.psn % len(tmps)
        tm, tk = tmps[i], ("gelu_tmp", i)
        S.op("dve", lambda e: e.tensor_tensor(out=tm[:], in0=ps[:], in1=rt[:, 1, tsl], op=ALU.mult), reads=[pk, "rot_sb"], writes=[tk])
        j = (i + 1) % len(tmps)
        tm2, tk2 = tmps[j], ("gelu_tmp", j)
        S.op("pool", lambda e: e.tensor_tensor(out=tm2[:], in0=pre[:, tsl], in1=rt[:, 0, tsl], op=ALU.mult), reads=[prekey, "rot_sb"], writes=[tk2])
        S.op("dve", lambda e: e.tensor_tensor(out=out[:, tsl], in0=tm[:], in1=tm2[:], op=ALU.add), reads=[tk, tk2], writes=[outkey])

    def stage_pre_exchange(self, l, st_tab):
        nc, S, W = self.nc, self.S, self.W[l]
        wA = self.wA
        S.barrier()
        with ExitStack() as st:
            sb = lambda name, shape, dt: st.enter_context(nc.sbuf_tensor(self.un(name), shape, dt))
            self.load_hT(st)
            stg = [sb("pa_stg%d" % i, [128, 512], F32) for i in range(3)]
            edge = sb("pa_edge", [128, 128], F32)
            cnt = [0]

            def epi_a(j, t, ps, pk):
                i = cnt[0] % 3
                cnt[0] += 1
                g_, gk = stg[i], ("pa_stg", i)
                S.op("act", lambda e: e.copy(out=g_[:], in_=ps[:]), reads=[pk], writes=[gk])
                S.op("sp", lambda e: e.dma_start(out=self.aTd[j, :, t * 512:(t + 1) * 512], in_=g_[:]), reads=[gk], writes=["aTd"], dma=True)
                if t == 0:
                    S.op("dve", lambda e: e.tensor_copy(out=edge[:, j * 16:j * 16 + 8], in_=g_[:, 0:8]), reads=[gk], writes=["pa_edge"])
                if t == NTT - 1:
                    S.op("dve", lambda e: e.tensor_copy(out=edge[:, j * 16 + 8:j * 16 + 16], in_=g_[:, 504:512]), reads=[gk], writes=["pa_edge"])

            self.gemm(W["w_in"], D, 0, 1024, self.hT_in, NTT, epi_a)
            S.op("sp", lambda e: e.dma_start(out=self.bounce[16 * 128:17 * 128, :], in_=edge[:]), reads=["pa_edge"], writes=["bounce"], dma=True)

            rt = self.rot_tables(st)
            kpre = sb("ra_kpre", [128, T], BF16)
            krot = sb("ra_krot", [128, T], BF16)
            vT = sb("ra_vT", [128, T], BF16)
            tmps = [sb("ra_tmp%d" % i, [128, 512], F32) for i in range(3)]
            tm8 = [sb("ra_tm8%d" % i, [128, 1024], BF16) for i in range(2)]
            kA = sb("ra_kA", [128, 2, NCH, 128], BF16)
            vtm = sb("ra_vtm", [128, NCH, 128], BF16)
            send = sb("ra_send", [128, 16, 128], F32)
            for h in range(8):
                def epi_k(j, t, ps, pk):
                    S.op("act", lambda e: e.copy(out=kpre[:, t * 512:(t + 1) * 512], in_=ps[:]), reads=[pk], writes=[("ra_kpre", t)])
                    self.rotary(kpre, ("ra_kpre", t), krot, ("ra_krot", t), rt, tmps, t)

                def epi_v(j, t, ps, pk):
                    S.op("act", lambda e: e.copy(out=vT[:, t * 512:(t + 1) * 512], in_=ps[:]), reads=[pk], writes=[("ra_vT", t)])

                self.gemm(W["w_in"], D, 2048 + h * 128, 128, self.hT_in, NTT, epi_k, slab=128)
                self.gemm(W["w_in"], D, 3072 + h * 128, 128, self.hT_in, NTT, epi_v, slab=128)
                for q4 in range(4):
                    pb, pbk = self.next_psb()
                    for i in range(4):
                        n = q4 * 4 + i
                        S.op("pe", lambda e, pb=pb, i=i, n=n: e.transpose(out=pb[:, i * 128:(i + 1) * 128], in_=krot[:, n * 128:(n + 1) * 128], identity=self.identb[:]),
                             reads=[("ra_krot", n // 4), "identb"], writes=[pbk])
                        S.op("pe", lambda e, pb=pb, i=i, n=n: e.transpose(out=pb[:, (4 + i) * 128:(5 + i) * 128], in_=vT[:, n * 128:(n + 1) * 128], identity=self.identb[:]),
                             reads=[("ra_vT", n // 4), "identb"], writes=[pbk])
                    t8, t8k = tm8[q4 % 2], ("ra_tm8", q4 % 2)
                    S.op("dve", lambda e, pb=pb, t8=t8: e.tensor_copy(out=t8[:], in_=pb[:]), reads=[pbk], writes=[t8k])
                    S.op("pool", lambda e, t8=t8, q4=q4: e.tensor_copy(out=vtm[:, q4 * 4:q4 * 4 + 4, :], in_=t8[:, 512:1024].rearrange("p (a b) -> p a b", a=4)), reads=[t8k], writes=["ra_vtm"])
                    for i in range(4):
                        n = q4 * 4 + i
                        S.op("dve", lambda e, t8=t8, i=i, n=n, h=h: e.tensor_scalar(out=kA[:, 0, n, :], in0=t8[:, i * 128:(i + 1) * 128], scalar1=wA[:, 0, n, h:h + 1], scalar2=None, op0=ALU.mult),
                             reads=[t8k, "wA"], writes=["ra_kA"])
                        S.op("dve", lambda e, t8=t8, i=i, n=n, h=h: e.tensor_scalar(out=kA[:, 1, n, :], in0=t8[:, i * 128:(i + 1) * 128], scalar1=wA[:, 1, n, h:h + 1], scalar2=None, op0=ALU.mult),
                             reads=[t8k, "wA"], writes=["ra_kA"])
                for dr in range(2):
                    ps, pk = self.next_ps()
                    for n in range(NCH):
                        S.op("pe", lambda e, ps=ps, dr=dr, n=n: e.matmul(ps[:, 0:128], lhsT=kA[:, dr, n, :], rhs=vtm[:, n, :], start=(n == 0), stop=(n == NCH - 1)),
                             reads=["ra_kA", "ra_vtm"], writes=[pk])
                    S.op("act" if dr == 0 else "dve",
                         (lambda e, ps=ps, dr=dr, h=h: e.copy(out=send[:, dr * 8 + h, :], in_=ps[:, 0:128])) if dr == 0 else
                         (lambda e, ps=ps, dr=dr, h=h: e.tensor_copy(out=send[:, dr * 8 + h, :], in_=ps[:, 0:128])),
                         reads=[pk], writes=["ra_send"])
            S.op("sp", lambda e: e.dma_start(out=self.bounce[0:16 * 128, :].rearrange("(b p) e -> p b e", p=128), in_=send[:]), reads=["ra_send"], writes=["bounce"], dma=True)
        S.barrier()
        if self.mode == "A":
            pass
        elif NOCOLL:
            S.op("sp", lambda e: e.dma_start(out=self.gath[0:17 * 128, :], in_=self.bounce), reads=["bounce"], writes=["gath"], dma=True)
        else:
            S.op("pool", lambda e: e.collective_compute("AllGather", ALU.bypass, replica_groups=[list(range(8))], ins=[self.bounce.opt()], outs=[self.gath.opt()]),
                 reads=["bounce"], writes=["gath"], dma=True, n_inc=1)
            S.op("pool", lambda e: e.nop(), reads=["gath"], writes=["gath"])
        S.barrier()

    def stage_pool_b(self, l):
        nc, S, W = self.nc, self.S, self.W[l]
        S.barrier()
        with ExitStack() as st:
            sb = lambda name, shape, dt: st.enter_context(nc.sbuf_tensor(self.un(name), shape, dt))
            E = sb("pb_E", [128, 8, 128], F32)
            S.op("sp", lambda e: e.dma_start(out=E[:], in_=self.gath.rearrange("(r b p) e -> p r b e", r=8, b=17)[:, :, 16, :]), reads=["gath"], writes=["pb_E"], dma=True)
            hal = sb("pb_hal", [128, 2, 8, 8], F32)
            selL, selR = self.ct("selL"), self.ct("selR")
            Ev = E[:].rearrange("p r (j s) -> p r j s", s=16)
            for side, sel, lo in ((0, selL, 8), (1, selR, 0)):
                for r in range(8):
                    if r == 0:
                        S.op("dve", lambda e, side=side, sel=sel, lo=lo, r=r: e.tensor_scalar(out=hal[:, side], in0=Ev[:, r, :, lo:lo + 8], scalar1=sel[:, r:r + 1], scalar2=None, op0=ALU.mult),
                             reads=["pb_E", "ctab"], writes=["pb_hal"])
                    else:
                        S.op("dve", lambda e, side=side, sel=sel, lo=lo, r=r: e.scalar_tensor_tensor(out=hal[:, side], in0=Ev[:, r, :, lo:lo + 8], scalar=sel[:, r:r + 1], in1=hal[:, side], op0=ALU.mult, op1=ALU.add),
                             reads=["pb_E", "ctab"], writes=["pb_hal"])
            pwf = sb("pb_pwf", [128, 8, 256], F32)
            pwb = sb("pb_pwb", [128, 8, 256], BF16)
            S.op("sp", lambda e: e.dma_start(out=pwf[:], in_=W["pool_w"].rearrange("g (ji p) o -> p (g ji) o", p=128)), writes=["pb_pwf"], dma=True)
            S.op("dve", lambda e: e.tensor_copy(out=pwb[:], in_=pwf[:]), reads=["pb_pwf"], writes=["pb_pwb"])
            psc = self.col_load(st, "pb_psc", W["pool_scale"], 8)
            X = [sb("pb_X%d" % i, [128, T + 16], F32) for i in range(2)]
            A = [sb("pb_A%d" % i, [128, T + 16], F32) for i in range(2)]
            mixed = sb("pb_mix", [128, 2, T], BF16)
            ot = sb("pb_o", [128, 2, T], BF16)
            corr = self.ct("corr")
            L = T + 16
            for g in range(4):
                w = (2, 4, 8, 16)[g]
                for ji in range(2):
                    j = g * 2 + ji
                    X_, Xk = X[ji], ("pb_X", ji)
                    S.op("sp", lambda e, X_=X_, j=j: e.dma_start(out=X_[:, 8:8 + T], in_=self.aTd[j]), reads=["aTd"], writes=[Xk], dma=True)
                    S.op("dve", lambda e, X_=X_, j=j: e.tensor_copy(out=X_[:, 0:8], in_=hal[:, 0, j, :]), reads=["pb_hal"], writes=[Xk])
                    S.op("dve", lambda e, X_=X_, j=j: e.tensor_copy(out=X_[:, 8 + T:16 + T], in_=hal[:, 1, j, :]), reads=["pb_hal"], writes=[Xk])
                    a0, a1 = A[0], A[1]
                    S.op("dve", lambda e, X_=X_: e.tensor_tensor(out=a0[:, 1:L], in0=X_[:, 0:L - 1], in1=X_[:, 1:L], op=ALU.add), reads=[Xk], writes=["pb_A0"])
                    cur, curk, oth, othk = a0, "pb_A0", a1, "pb_A1"
                    sh = 1
                    lo, hi = 1, L
                    while sh * 2 < w:
                        nlo, nhi = lo + sh, hi - sh
                        S.op("dve", lambda e, cur=cur, oth=oth, sh=sh, nlo=nlo, nhi=nhi: e.tensor_tensor(out=oth[:, nlo:nhi], in0=cur[:, nlo - sh:nhi - sh], in1=cur[:, nlo + sh:nhi + sh], op=ALU.add),
                             reads=[curk], writes=[othk])
                        cur, curk, oth, othk = oth, othk, cur, curk
                        lo, hi = nlo, nhi
                        sh *= 2
                    S.op("dve", lambda e, cur=cur, w=w: e.tensor_scalar(out=cur[:, 8:8 + T], in0=cur[:, 8:8 + T], scalar1=1.0 / w, scalar2=None, op0=ALU.mult), reads=[curk], writes=[curk])
                    S.op("dve", lambda e, cur=cur, g=g: e.tensor_tensor(out=cur[:, 8:16], in0=cur[:, 8:16], in1=corr[:, g * 16:g * 16 + 8], op=ALU.mult), reads=[curk, "ctab"], writes=[curk])
                    S.op("dve", lambda e, cur=cur, g=g: e.tensor_tensor(out=cur[:, T:T + 8], in0=cur[:, T:T + 8], in1=corr[:, g * 16 + 8:g * 16 + 16], op=ALU.mult), reads=[curk, "ctab"], writes=[curk])
                    S.op("dve", lambda e, cur=cur, X_=X_, ji=ji: e.tensor_tensor(out=mixed[:, ji, :], in0=cur[:, 8:8 + T], in1=X_[:, 8:8 + T], op=ALU.subtract), reads=[curk, Xk], writes=[("pb_mix", ji)])
                for jo in range(2):
                    j = g * 2 + jo
                    for t in range(NTT):
                        ps, pk = self.next_ps()
                        for ji in range(2):
                            S.op("pe", lambda e, ps=ps, g=g, ji=ji, jo=jo, t=t: e.matmul(ps[:], lhsT=pwb[:, g * 2 + ji, jo * 128:(jo + 1) * 128], rhs=mixed[:, ji, t * 512:(t + 1) * 512], start=(ji == 0), stop=(ji == 1)),
                                 reads=["pb_pwb", ("pb_mix", ji)], writes=[pk])
                        S.op("dve", lambda e, ps=ps, jo=jo, t=t, j=j: e.tensor_scalar(out=ot[:, jo, t * 512:(t + 1) * 512], in0=ps[:], scalar1=psc[:, j:j + 1], scalar2=None, op0=ALU.mult), reads=[pk, "pb_psc"], writes=["pb_o"])
                S.op("sp", lambda e, g=g: e.dma_start(out=self.yb[0][g * 2:g * 2 + 2].rearrange("k p t -> p k t"), in_=ot[:]), reads=["pb_o"], writes=["yb0"], dma=True)
        S.barrier()

    def stage_ret_b(self, l):
        nc, S, W = self.nc, self.S, self.W[l]
        wk_ = self.wk
        S.barrier()
        with ExitStack() as st:
            sb = lambda name, shape, dt: st.enter_context(nc.sbuf_tensor(self.un(name), shape, dt))
            self.load_hT(st)
            rt = self.rot_tables(st)
            lg = self.lgbc
            gng = self.col_load(st, "rb_gng", W["ret_gn_g"], 8)
            Dm = sb("rb_D", [128, 8, 128], F32)
            wq = sb("rb_wq", [128, 2, 8, 128], F32)
            tq = sb("rb_tq", [128, 128], F32)
            s_ = 128.0 ** -0.5
            for h in range(8):
                S.op("act", lambda e, h=h: e.activation(out=Dm[:, h, :], in_=self.ct("dpos"), func=AF.Exp, scale=lg[:, h:h + 1]), reads=["ctab", "lgbc"], writes=["rb_D"])
                S.op("dve", lambda e, h=h: e.tensor_tensor(out=Dm[:, h, :], in0=Dm[:, h, :], in1=self.ct("mge"), op=ALU.mult), reads=["rb_D", "ctab"], writes=["rb_D"])
                S.op("act", lambda e, h=h: e.activation(out=tq[:], in_=self.ct("dneg"), func=AF.Exp, scale=lg[:, 8 + h:9 + h]), reads=["ctab", "lgbc"], writes=["rb_tq"])
                S.op("dve", lambda e: e.tensor_tensor(out=tq[:], in0=tq[:], in1=self.ct("mlt"), op=ALU.mult), reads=["rb_tq", "ctab"], writes=["rb_tq"])
                S.op("dve", lambda e, h=h: e.tensor_tensor(out=Dm[:, h, :], in0=Dm[:, h, :], in1=tq[:], op=ALU.add), reads=["rb_D", "rb_tq"], writes=["rb_D"])
                S.op("act", lambda e, h=h: e.activation(out=wq[:, 0, h, :], in_=self.ct("rowc1"), func=AF.Exp, scale=lg[:, h:h + 1]), reads=["ctab", "lgbc"], writes=["rb_wq"])
                S.op("act", lambda e, h=h: e.activation(out=wq[:, 1, h, :], in_=self.ct("row128mc"), func=AF.Exp, scale=lg[:, 8 + h:9 + h]), reads=["ctab", "lgbc"], writes=["rb_wq"])
            S.op("dve", lambda e: e.tensor_scalar(out=wq[:], in0=wq[:], scalar1=s_, scalar2=None, op0=ALU.mult), reads=["rb_wq"], writes=["rb_wq"])
            qpre = sb("rb_qpre", [128, T], BF16)
            kpre = sb("rb_kpre", [128, T], BF16)
            qrot = sb("rb_qrot", [128, T], BF16)
            krot = sb("rb_krot", [128, T], BF16)
            qw = sb("rb_qw", [128, 2, T], BF16)
            vT = sb("rb_vT", [128, T], BF16)
            sg = sb("rb_sg", [128, T], F32)
            tmps = [sb("rb_tmp%d" % i, [128, 512], F32) for i in range(3)]
            tm8 = [sb("rb_tm8%d" % i, [128, 1024], BF16) for i in range(2)]
            kl = sb("rb_kl", [128, 2, NCH, 128], BF16)
            vtm = sb("rb_vtm", [128, NCH, 128], BF16)
            Sg = sb("rb_Sg", [128, 2, 8, 128], F32)
            Scur = sb("rb_Scur", [128, 2, 128], F32)
            Sbf = sb("rb_Sbf", [128, 2, NCH, 128], BF16)
            sm = [sb("rb_sm%d" % i, [128, 512], BF16) for i in range(2)]
            y = tmps[2]
            ybf = sb("rb_ybf", [128, 512], BF16)
            ysq = sb("rb_ysq", [128, 512], BF16)
            mean = tmps[0]
            rstd = tmps[1]
            ot = sb("rb_o", [128, T], BF16)
            gv = self.gath.rearrange("(r b p) e -> p r b e", r=8, b=17)
            for h in range(8):
                for dr in range(2):
                    S.op("sp", lambda e, dr=dr, h=h: e.dma_start(out=Sg[:, dr], in_=gv[:, :, dr * 8 + h, :]), reads=["gath"], writes=[("rb_Sg", dr)], dma=True)
                    for r in range(8):
                        cfc = self.cf[:, dr * 8 + r, h:h + 1]
                        if r == 0:
                            S.op("dve", lambda e, dr=dr, r=r, cfc=cfc: e.tensor_scalar(out=Scur[:, dr, :], in0=Sg[:, dr, r, :], scalar1=cfc, scalar2=None, op0=ALU.mult),
                                 reads=[("rb_Sg", dr), "cf"], writes=[("rb_Scur", dr)])
                        else:
                            S.op("dve", lambda e, dr=dr, r=r, cfc=cfc: e.scalar_tensor_tensor(out=Scur[:, dr, :], in0=Sg[:, dr, r, :], scalar=cfc, in1=Scur[:, dr, :], op0=ALU.mult, op1=ALU.add),
                                 reads=[("rb_Sg", dr), "cf"], writes=[("rb_Scur", dr)])

                def epi_q(j, t, ps, pk):
                    S.op("act", lambda e: e.copy(out=qpre[:, t * 512:(t + 1) * 512], in_=ps[:]), reads=[pk], writes=[("rb_qpre", t)])
                    self.rotary(qpre, ("rb_qpre", t), qrot, ("rb_qrot", t), rt, tmps, t)

                def epi_k(j, t, ps, pk):
                    S.op("act", lambda e: e.copy(out=kpre[:, t * 512:(t + 1) * 512], in_=ps[:]), reads=[pk], writes=[("rb_kpre", t)])
                    self.rotary(kpre, ("rb_kpre", t), krot, ("rb_krot", t), rt, tmps, t)

                def epi_v(j, t, ps, pk):
                    S.op("act", lambda e: e.copy(out=vT[:, t * 512:(t + 1) * 512], in_=ps[:]), reads=[pk], writes=[("rb_vT", t)])

                def epi_g(j, t, ps, pk):
                    S.op("act", lambda e: e.activation(out=sg[:, t * 512:(t + 1) * 512], in_=ps[:], func=AF.Silu), reads=[pk], writes=[("rb_sg", t)])

                self.gemm(W["w_in"], D, 1024 + h * 128, 128, self.hT_in, NTT, epi_q, slab=128)
                self.gemm(W["w_in"], D, 2048 + h * 128, 128, self.hT_in, NTT, epi_k, slab=128)
                self.gemm(W["w_in"], D, 3072 + h * 128, 128, self.hT_in, NTT, epi_v, slab=128)
                self.gemm(W["w_in"], D, 4096 + h * 128, 128, self.hT_in, NTT, epi_g, slab=128)
                for n in range(NCH):
                    csl = slice(n * 128, (n + 1) * 128)
                    S.op("dve", lambda e, csl=csl, h=h: e.tensor_tensor(out=qw[:, 0, csl], in0=qrot[:, csl], in1=wq[:, 0, h, :], op=ALU.mult), reads=[("rb_qrot", n // 4), "rb_wq"], writes=[("rb_qw", n)])
                    S.op("pool", lambda e, csl=csl, h=h: e.tensor_tensor(out=qw[:, 1, csl], in0=qrot[:, csl], in1=wq[:, 1, h, :], op=ALU.mult), reads=[("rb_qrot", n // 4), "rb_wq"], writes=[("rb_qw", n)])
                for q4 in range(4):
                    pb, pbk = self.next_psb()
                    for i in range(4):
                        n = q4 * 4 + i
                        S.op("pe", lambda e, pb=pb, i=i, n=n: e.transpose(out=pb[:, i * 128:(i + 1) * 128], in_=krot[:, n * 128:(n + 1) * 128], identity=self.identb[:]),
                             reads=[("rb_krot", n // 4), "identb"], writes=[pbk])
                        S.op("pe", lambda e, pb=pb, i=i, n=n: e.transpose(out=pb[:, (4 + i) * 128:(5 + i) * 128], in_=vT[:, n * 128:(n + 1) * 128], identity=self.identb[:]),
                             reads=[("rb_vT", n // 4), "identb"], writes=[pbk])
                    t8, t8k = tm8[q4 % 2], ("rb_tm8", q4 % 2)
                    S.op("dve", lambda e, pb=pb, t8=t8: e.tensor_copy(out=t8[:], in_=pb[:]), reads=[pbk], writes=[t8k])
                    S.op("pool", lambda e, t8=t8, q4=q4: e.tensor_copy(out=vtm[:, q4 * 4:q4 * 4 + 4, :], in_=t8[:, 512:1024].rearrange("p (a b) -> p a b", a=4)), reads=[t8k], writes=[("rb_vtm", q4)])
                    S.op("dve", lambda e, t8=t8, q4=q4, h=h: e.tensor_scalar(out=kl[:, 0, q4 * 4:q4 * 4 + 4, :].rearrange("p a b -> p (a b)"), in0=t8[:, 0:512], scalar1=wk_[:, 0, h:h + 1], scalar2=None, op0=ALU.mult),
                         reads=[t8k, "wk"], writes=[("rb_kl", q4)])
                    S.op("dve", lambda e, t8=t8, q4=q4, h=h: e.tensor_scalar(out=kl[:, 1, q4 * 4:q4 * 4 + 4, :].rearrange("p a b -> p (a b)"), in0=t8[:, 0:512], scalar1=wk_[:, 1, h:h + 1], scalar2=None, op0=ALU.mult),
                         reads=[t8k, "wk"], writes=[("rb_kl", q4)])
                for dr in range(2):
                    order = list(range(NCH)) if dr == 0 else list(range(NCH - 1, -1, -1))
                    gcol = self.g128[:, dr * 8 + h:dr * 8 + h + 1]
                    for n in order:
                        S.op("act", lambda e, dr=dr, n=n: e.copy(out=Sbf[:, dr, n, :], in_=Scur[:, dr, :]), reads=[("rb_Scur", dr)], writes=[("rb_Sbf", dr, n)])
                        ps, pk = self.next_ps()
                        S.op("pe", lambda e, ps=ps, dr=dr, n=n: e.matmul(ps[:, 0:128], lhsT=kl[:, dr, n, :], rhs=vtm[:, n, :], start=True, stop=True),
                             reads=[("rb_kl", n // 4), ("rb_vtm", n // 4)], writes=[pk])
                        S.op("dve", lambda e, ps=ps, dr=dr, gcol=gcol: e.scalar_tensor_tensor(out=Scur[:, dr, :], in0=Scur[:, dr, :], scalar=gcol, in1=ps[:, 0:128], op0=ALU.mult, op1=ALU.add),
                             reads=[("rb_Scur", dr), pk, "g128", ("rb_Sbf", dr, n)], writes=[("rb_Scur", dr)])
                for t in range(NTT):
                    tsl = slice(t * 512, (t + 1) * 512)
                    ps_s, pk_s = self.next_ps()
                    for i in range(4):
                        n = t * 4 + i
                        S.op("pe", lambda e, ps_s=ps_s, i=i, n=n: e.matmul(ps_s[:, i * 128:(i + 1) * 128], lhsT=krot[:, n * 128:(n + 1) * 128], rhs=qrot[:, n * 128:(n + 1) * 128], start=True, stop=True),
                             reads=[("rb_krot", t), ("rb_qrot", t)], writes=[pk_s])
                    sm_, smk = sm[t % 2], ("rb_sm", t % 2)
                    for i in range(4):
                        S.op("dve", lambda e, ps_s=ps_s, sm_=sm_, i=i, h=h: e.tensor_tensor(out=sm_[:, i * 128:(i + 1) * 128], in0=ps_s[:, i * 128:(i + 1) * 128], in1=Dm[:, h, :], op=ALU.mult),
                             reads=[pk_s, "rb_D"], writes=[smk])
                    ps_y, pk_y = self.next_ps()
                    for i in range(4):
                        n = t * 4 + i
                        csl = slice(n * 128, (n + 1) * 128)
                        S.op("pe", lambda e, ps_y=ps_y, sm_=sm_, i=i, n=n: e.matmul(ps_y[:, i * 128:(i + 1) * 128], lhsT=vtm[:, n, :], rhs=sm_[:, i * 128:(i + 1) * 128], start=True, stop=False),
                             reads=[("rb_vtm", n // 4), smk], writes=[pk_y])
                        S.op("pe", lambda e, ps_y=ps_y, i=i, n=n, csl=csl: e.matmul(ps_y[:, i * 128:(i + 1) * 128], lhsT=Sbf[:, 0, n, :], rhs=qw[:, 0, csl], start=False, stop=False),
                             reads=[("rb_Sbf", 0, n), ("rb_qw", n)], writes=[pk_y])
                        S.op("pe", lambda e, ps_y=ps_y, i=i, n=n, csl=csl: e.matmul(ps_y[:, i * 128:(i + 1) * 128], lhsT=Sbf[:, 1, n, :], rhs=qw[:, 1, csl], start=False, stop=True),
                             reads=[("rb_Sbf", 1, n), ("rb_qw", n)], writes=[pk_y])
                    S.op("act", lambda e, ps_y=ps_y: e.copy(out=y[:], in_=ps_y[:]), reads=[pk_y], writes=[("gelu_tmp", 2)])
                    S.op("dve", lambda e: e.tensor_copy(out=ybf[:], in_=y[:]), reads=[("gelu_tmp", 2)], writes=["rb_ybf"])
                    S.op("pool", lambda e: e.tensor_tensor(out=ysq[:], in0=y[:], in1=y[:], op=ALU.mult), reads=[("gelu_tmp", 2)], writes=["rb_ysq"])
                    ps_m, pk_m = self.next_ps()
                    S.op("pe", lambda e, ps_m=ps_m: e.matmul(ps_m[:], lhsT=self.onesb[:], rhs=ybf[:], start=True, stop=True), reads=["onesb", "rb_ybf"], writes=[pk_m])
                    ps_q, pk_q = self.next_ps()
                    S.op("pe", lambda e, ps_q=ps_q: e.matmul(ps_q[:], lhsT=self.onesb[:], rhs=ysq[:], start=True, stop=True), reads=["onesb", "rb_ysq"], writes=[pk_q])
                    S.op("act", lambda e, ps_m=ps_m: e.mul(out=mean[:], in_=ps_m[:], mul=1.0 / 128), reads=[pk_m], writes=[("gelu_tmp", 0)])
                    S.op("dve", lambda e: e.tensor_tensor(out=rstd[:], in0=mean[:], in1=mean[:], op=ALU.mult), reads=[("gelu_tmp", 0)], writes=[("gelu_tmp", 1)])
                    S.op("dve", lambda e, ps_q=ps_q: e.scalar_tensor_tensor(out=rstd[:], in0=ps_q[:], scalar=1.0 / 128, in1=rstd[:], op0=ALU.mult, op1=ALU.subtract), reads=[pk_q, ("gelu_tmp", 1)], writes=[("gelu_tmp", 1)])
                    S.op("dve", lambda e: e.tensor_scalar(out=rstd[:], in0=rstd[:], scalar1=LN_EPS, scalar2=None, op0=ALU.add), reads=[("gelu_tmp", 1)], writes=[("gelu_tmp", 1)])
                    S.op("act", lambda e: e.activation(out=rstd[:], in_=rstd[:], func=AF.Sqrt), reads=[("gelu_tmp", 1)], writes=[("gelu_tmp", 1)])
                    S.op("dve", lambda e: e.reciprocal(out=rstd[:], in_=rstd[:]), reads=[("gelu_tmp", 1)], writes=[("gelu_tmp", 1)])
                    S.op("dve", lambda e: e.tensor_tensor(out=y[:], in0=y[:], in1=mean[:], op=ALU.subtract), reads=[("gelu_tmp", 2), ("gelu_tmp", 0), "rb_ybf", "rb_ysq"], writes=[("gelu_tmp", 2)])
                    S.op("dve", lambda e: e.tensor_tensor(out=y[:], in0=y[:], in1=rstd[:], op=ALU.mult), reads=[("gelu_tmp", 2), ("gelu_tmp", 1)], writes=[("gelu_tmp", 2)])
                    S.op("dve", lambda e, tsl=tsl, h=h: e.scalar_tensor_tensor(out=ot[:, tsl], in0=y[:], scalar=gng[:, h:h + 1], in1=sg[:, tsl], op0=ALU.mult, op1=ALU.mult),
                         reads=[("gelu_tmp", 2), "rb_gng", ("rb_sg", t)], writes=["rb_o"])
                S.op("sp", lambda e, h=h: e.dma_start(out=self.yb[1][h], in_=ot[:]), reads=["rb_o"], writes=["yb1"], dma=True)
        S.barrier()

    def load_slab(self, Wap, K, col0, slab=256):
        S = self.S
        kc_n = K // 128
        si = self.wsn % len(self.wslots)
        self.wsn += 1
        slot = self.wslots[si]
        skey = ("wslot", si)
        src = Wap[:, col0:col0 + slab].rearrange("(kc p) n -> p kc n", p=128)
        S.op("pool", lambda e: e.dma_start(out=slot[:, 0:kc_n, 0:slab], in_=src), writes=[skey], dma=True)
        return slot, skey

    def mm_acc(self, ps, pk, slot, skey, kc_n, jj, rhs_fn):
        S = self.S
        for kc in range(kc_n):
            rhs, rkeys = rhs_fn(kc)
            S.op("pe", lambda e, kc=kc, rhs=rhs: e.matmul(ps[:], lhsT=slot[:, kc, jj * 128:(jj + 1) * 128], rhs=rhs, start=(kc == 0), stop=(kc == kc_n - 1)),
                 reads=[skey] + rkeys, writes=[pk])

    def ln_fm(self, acc, ybuf, tmps, gcol, bcol, hf, want_alpha, write_hT):
        nc, S = self.nc, self.S
        sqb, xbf = tmps["sqb"], tmps["xbf"]
        mean, rstd, o32 = tmps["f0"], tmps["f1"], tmps["f2"]
        for t in range(2):
            tsl = slice(t * 512, (t + 1) * 512)
            gsl = slice(hf * 1024 + t * 512, hf * 1024 + (t + 1) * 512)
            ps_s, pk_s = self.next_ps()
            ps_q, pk_q = self.next_ps()
            for j in range(KC):
                S.op("act", lambda e, j=j, tsl=tsl: e.copy(out=xbf[:], in_=acc[:, j, tsl]), reads=[("acc", j, t)], writes=["ln_xbf"])
                S.op("pe", lambda e, ps_s=ps_s, j=j: e.matmul(ps_s[:], lhsT=self.onesb[:], rhs=xbf[:], start=(j == 0), stop=(j == KC - 1)), reads=["onesb", "ln_xbf"], writes=[pk_s])
                S.op("dve", lambda e, j=j, tsl=tsl: e.tensor_tensor(out=sqb[:], in0=acc[:, j, tsl], in1=acc[:, j, tsl], op=ALU.mult), reads=[("acc", j, t)], writes=["ln_sqb"])
                S.op("pe", lambda e, ps_q=ps_q, j=j: e.matmul(ps_q[:], lhsT=self.onesb[:], rhs=sqb[:], start=(j == 0), stop=(j == KC - 1)), reads=["onesb", "ln_sqb"], writes=[pk_q])
            S.op("act", lambda e, ps_s=ps_s: e.mul(out=mean[:], in_=ps_s[:], mul=1.0 / D), reads=[pk_s], writes=["ln_f0"])
            S.op("dve", lambda e: e.tensor_tensor(out=rstd[:], in0=mean[:], in1=mean[:], op=ALU.mult), reads=["ln_f0"], writes=["ln_f1"])
            S.op("dve", lambda e, ps_q=ps_q: e.scalar_tensor_tensor(out=rstd[:], in0=ps_q[:], scalar=1.0 / D, in1=rstd[:], op0=ALU.mult, op1=ALU.subtract), reads=[pk_q, "ln_f1"], writes=["ln_f1"])
            S.op("dve", lambda e: e.tensor_scalar(out=rstd[:], in0=rstd[:], scalar1=LN_EPS, scalar2=None, op0=ALU.add), reads=["ln_f1"], writes=["ln_f1"])
            S.op("act", lambda e: e.activation(out=rstd[:], in_=rstd[:], func=AF.Sqrt), reads=["ln_f1"], writes=["ln_f1"])
            S.op("dve", lambda e: e.reciprocal(out=rstd[:], in_=rstd[:]), reads=["ln_f1"], writes=["ln_f1"])
            for j in range(KC):
                S.op("dve", lambda e, j=j, tsl=tsl: e.tensor_tensor(out=o32[:], in0=acc[:, j, tsl], in1=mean[:], op=ALU.subtract), reads=[("acc", j, t), "ln_f0"], writes=["ln_f2"])
                S.op("dve", lambda e: e.tensor_tensor(out=o32[:], in0=o32[:], in1=rstd[:], op=ALU.mult), reads=["ln_f2", "ln_f1"], writes=["ln_f2"])
                S.op("act", lambda e, j=j: e.activation(out=o32[:], in_=o32[:], func=AF.Identity, scale=gcol[:, j:j + 1], bias=bcol[:, j:j + 1]), reads=["ln_f2", "ln_g", "ln_b"], writes=["ln_f2"])
                S.op("sp", lambda e, j=j, gsl=gsl: e.dma_start(out=self.hres[j, :, gsl], in_=o32[:]), reads=["ln_f2"], writes=["hres"], dma=True)
                if write_hT:
                    S.op("act", lambda e: e.copy(out=xbf[:], in_=o32[:]), reads=["ln_f2"], writes=["ln_xbf"])
                    S.op("sp", lambda e, j=j, gsl=gsl: e.dma_start(out=self.hTd[j, :, gsl], in_=xbf[:]), reads=["ln_xbf"], writes=["hTd"], dma=True)
                else:
                    S.op("act", lambda e, j=j, tsl=tsl: e.copy(out=ybuf[j // 8][:, j % 8, tsl], in_=o32[:]), reads=["ln_f2"], writes=[("ybuf", j // 8)])
                if want_alpha:
                    S.op("dve", lambda e, j=j, tsl=tsl: e.tensor_scalar(out=acc[:, j, tsl], in0=o32[:], scalar1=ALPHA, scalar2=None, op0=ALU.mult), reads=["ln_f2"], writes=[("acc", j, t)])

    def stage_post(self, l):
        nc, S, W = self.nc, self.S, self.W[l]
        S.barrier()
        with ExitStack() as st:
            sb = lambda name, shape, dt: st.enter_context(nc.sbuf_tensor(self.un(name), shape, dt))
            acc = sb("acc", [128, KC, 1024], F32)
            ybuf = [sb("ybuf%d" % i, [128, 8, 1024], BF16) for i in range(2)]
            f = [sb("pf%d" % i, [128, 512], F32) for i in range(4)]
            tm = {"sqb": sb("ln_sqb", [128, 512], BF16), "xbf": sb("ln_xbf", [128, 512], BF16), "f0": f[0], "f1": f[1], "f2": f[2]}
            bg = self.col_load(st, "bgate", W["b_gate"].rearrange("b d -> (b d)"), 64)
            l1g = self.col_load(st, "ln_g1", W["ln1_g"], KC)
            l1b = self.col_load(st, "ln_b1", W["ln1_b"], KC)
            l2g = self.col_load(st, "ln_g2", W["ln2_g"], KC)
            l2b = self.col_load(st, "ln_b2", W["ln2_b"], KC)
            bup = sb("bup", [128, NEXP, 12], F32)
            S.op("sp", lambda e: e.dma_start(out=bup[:], in_=W["b_up"].rearrange("e (c p) -> p e c", p=128), allow_slow_non_contiguous=True), writes=["bup"], dma=True)
            wrb = sb("wrb", [128, KC, 32], BF16)
            S.op("pool", lambda e: e.dma_start(out=wrb[:], in_=W["w_router"].rearrange("(c p) n -> p c n", p=128)), writes=["wrb"], dma=True)
            brt = self.bcast_load(st, "brt", W["b_router"], 32)
            bdb = sb("bdb", [32, D], BF16)
            S.op("pool", lambda e: e.dma_start(out=bdb[:], in_=W["b_down"]), writes=["bdb"], dma=True)
            lgt = sb("lgt", [128, 8, 32], F32)
            top8 = sb("top8", [128, 8], F32)
            msk = sb("msk", [128, 32], F32)
            sml = sb("sml", [128, 4], F32)
            GT = sb("GT", [32, 1024], F32)
            GTb = sb("GTb", [32, 1024], BF16)
            gbc = sb("gbc", [128, 1024], F32)
            gact = sb("gact", [128, 6, 1024], BF16)
            hrt = [sb("hrt%d" % i, [128, 1024], F32) for i in range(1)]
            hTh = sb("hTh", [128, KC, 1024], BF16)
            GTd = nc.dram_tensor(self.un("GTd"), [32, T], F32).ap()
            for hf in range(2):
                hsl = slice(hf * 1024, (hf + 1) * 1024)
                for kc in range(KC):
                    S.op("sp", lambda e, kc=kc, hsl=hsl: e.dma_start(out=hTh[:, kc, :], in_=self.hTd[kc, :, hsl]), reads=["hTd"], writes=[("hTh", kc)], dma=True)
                for b in range(4):
                    yb_, ybk = ybuf[b % 2], ("ybuf", b % 2)
                    S.op("sp", lambda e, b=b, yb_=yb_, hsl=hsl: e.dma_start(out=yb_[:], in_=self.yb[b][:, :, hsl].rearrange("k p t -> p k t")), reads=["yb%d" % b], writes=[ybk], dma=True)
                    for s in range(8):
                        gslot, gk = self.load_slab(W["w_gate"][b], D, s * 256)
                        bslot, bk = self.load_slab(W["w_branch"][b], 1024, s * 256)
                        for jj in range(2):
                            j = s * 2 + jj
                            for t in range(2):
                                tsl = slice(t * 512, (t + 1) * 512)
                                gt_ = hf * 2 + t
                                psg, pkg = self.next_ps()
                                self.mm_acc(psg, pkg, gslot, gk, KC, jj, lambda kc, tsl=tsl: (hTh[:, kc, tsl], [("hTh", kc)]))
                                S.op("act", lambda e, psg=psg, b=b, j=j: e.activation(out=f[3][:], in_=psg[:], func=AF.Sigmoid, bias=bg[:, b * 16 + j:b * 16 + j + 1]), reads=[pkg, "bgate"], writes=["pf3"])
                                psb_, pkb = self.next_ps()
                                self.mm_acc(psb_, pkb, bslot, bk, 8, jj, lambda kc, yb_=yb_, tsl=tsl, ybk=ybk: (yb_[:, kc, tsl], [ybk]))
                                if b == 0:
                                    S.op("dve", lambda e, psb_=psb_, j=j, tsl=tsl: e.tensor_tensor(out=acc[:, j, tsl], in0=f[3][:], in1=psb_[:], op=ALU.mult), reads=["pf3", pkb], writes=[("acc", j, t)])
                                else:
                                    S.op("dve", lambda e, psb_=psb_: e.tensor_tensor(out=f[2][:], in0=f[3][:], in1=psb_[:], op=ALU.mult), reads=["pf3", pkb], writes=["ln_f2"])
                                    S.op("dve", lambda e, j=j, tsl=tsl: e.tensor_tensor(out=acc[:, j, tsl], in0=acc[:, j, tsl], in1=f[2][:], op=ALU.add), reads=["ln_f2", ("acc", j, t)], writes=[("acc", j, t)])
                for j in range(KC):
                    S.op("act" if j % 2 else "dve",
                         (lambda e, j=j: e.copy(out=ybuf[j // 8][:, j % 8, :], in_=acc[:, j, :])) if j % 2 else
                         (lambda e, j=j: e.tensor_copy(out=ybuf[j // 8][:, j % 8, :], in_=acc[:, j, :])),
                         reads=[("acc", j, 0), ("acc", j, 1)], writes=[("ybuf", j // 8)])
                for s in range(8):
                    oslot, okk = self.load_slab(W["w_out"], D, s * 256)
                    for jj in range(2):
                        j = s * 2 + jj
                        hr_, hrk = hrt[0], ("hrt", 0)
                        S.op("sp", lambda e, j=j, hr_=hr_, hsl=hsl: e.dma_start(out=hr_[:], in_=self.hres[j, :, hsl]), reads=["hres"], writes=[hrk], dma=True)
                        for t in range(2):
                            tsl = slice(t * 512, (t + 1) * 512)
                            ps, pk = self.next_ps()
                            self.mm_acc(ps, pk, oslot, okk, KC, jj, lambda kc, tsl=tsl: (ybuf[kc // 8][:, kc % 8, tsl], [("ybuf", kc // 8)]))
                            S.op("dve", lambda e, ps=ps, j=j, tsl=tsl, hr_=hr_: e.scalar_tensor_tensor(out=acc[:, j, tsl], in0=hr_[:, tsl], scalar=ALPHA, in1=ps[:], op0=ALU.mult, op1=ALU.add),
                                 reads=[pk, hrk, ("ybuf", 0), ("ybuf", 1)], writes=[("acc", j, t)])
                self.ln_fm(acc, ybuf, tm, l1g, l1b, hf, True, False)
                ps, pk = self.next_ps()
                for g8 in range(8):
                    for kc in range(KC):
                        S.op("pe", lambda e, ps=ps, g8=g8, kc=kc: e.matmul(ps[:, g8 * 32:(g8 + 1) * 32], lhsT=ybuf[kc // 8][:, kc % 8, g8 * 128:(g8 + 1) * 128], rhs=wrb[:, kc, :], start=(kc == 0), stop=(kc == KC - 1)),
                             reads=[("ybuf", kc // 8), "wrb"], writes=[pk])
                for g8 in range(8):
                    S.op("dve", lambda e, ps=ps, g8=g8: e.tensor_tensor(out=lgt[:, g8, :], in0=ps[:, g8 * 32:(g8 + 1) * 32], in1=brt[:], op=ALU.add), reads=[pk, "brt"], writes=["lgt"])
                for g8 in range(8):
                    S.op("dve", lambda e, g8=g8: e.max(out=top8[:], in_=lgt[:, g8, :]), reads=["lgt"], writes=["top8"])
                    S.op("dve", lambda e, g8=g8: e.tensor_scalar(out=msk[:], in0=lgt[:, g8, :], scalar1=top8[:, 3:4], scalar2=None, op0=ALU.is_ge), reads=["lgt", "top8"], writes=["msk"])
                    S.op("dve", lambda e: e.tensor_scalar(out=sml[:, 0:1], in0=top8[:, 0:1], scalar1=-1.0, scalar2=None, op0=ALU.mult), reads=["top8"], writes=["sml"])
                    S.op("act", lambda e, g8=g8: e.activation(out=lgt[:, g8, :], in_=lgt[:, g8, :], func=AF.Exp, bias=sml[:, 0:1]), reads=["lgt", "sml"], writes=["lgt"])
                    S.op("dve", lambda e, g8=g8: e.tensor_tensor(out=lgt[:, g8, :], in0=lgt[:, g8, :], in1=msk[:], op=ALU.mult), reads=["lgt", "msk"], writes=["lgt"])
                    S.op("dve", lambda e, g8=g8: e.reduce_sum(out=sml[:, 1:2], in_=lgt[:, g8, :], axis=AX.X), reads=["lgt"], writes=["sml"])
                    S.op("dve", lambda e: e.reciprocal(out=sml[:, 2:3], in_=sml[:, 1:2]), reads=["sml"], writes=["sml"])
                    S.op("dve", lambda e, g8=g8: e.tensor_scalar(out=lgt[:, g8, :], in0=lgt[:, g8, :], scalar1=sml[:, 2:3], scalar2=None, op0=ALU.mult), reads=["lgt", "sml"], writes=["lgt"])
                for half2 in range(2):
                    ps, pk = self.next_ps()
                    for i in range(4):
                        g8 = half2 * 4 + i
                        S.op("pe", lambda e, ps=ps, g8=g8, i=i: e.transpose(out=ps[0:32, i * 128:(i + 1) * 128], in_=lgt[:, g8, :], identity=self.ct("ident")), reads=["lgt", "ctab"], writes=[pk])
                    S.op("dve", lambda e, ps=ps, half2=half2: e.tensor_copy(out=GT[:, half2 * 512:(half2 + 1) * 512], in_=ps[0:32, :]), reads=[pk], writes=["GT"])
                S.op("dve", lambda e: e.tensor_copy(out=GTb[:], in_=GT[:]), reads=["GT"], writes=["GTb"])
                S.op("sp", lambda e, hsl=hsl: e.dma_start(out=GTd[:, hsl], in_=GT[:]), reads=["GT"], writes=["GTd"], dma=True)
                for ex in range(NEXP):
                    S.op("sp", lambda e, ex=ex, hsl=hsl: e.dma_start(out=gbc[:], in_=GTd[ex, hsl].partition_broadcast(128)), reads=["GTd"], writes=["gbc"], dma=True)
                    for s in range(3):
                        gslot, gk = self.load_slab(W["w_up"][ex], D, s * 256)
                        lslot, lk = self.load_slab(W["w_up"][ex], D, 768 + s * 256)
                        for jj in range(2):
                            c = s * 2 + jj
                            for t in range(2):
                                tsl = slice(t * 512, (t + 1) * 512)
                                hin = lambda kc, tsl=tsl: (ybuf[kc // 8][:, kc % 8, tsl], [("ybuf", kc // 8)])
                                psg, pkg = self.next_ps()
                                self.mm_acc(psg, pkg, gslot, gk, KC, jj, hin)
                                psl, pkl = self.next_ps()
                                self.mm_acc(psl, pkl, lslot, lk, KC, jj, hin)
                                S.op("dve", lambda e, psg=psg, ex=ex, c=c: e.tensor_scalar(out=f[0][:], in0=psg[:], scalar1=bup[:, ex, c:c + 1], scalar2=7.0, op0=ALU.add, op1=ALU.min), reads=[pkg, "bup"], writes=["ln_f0"])
                                S.op("act", lambda e: e.activation(out=f[1][:], in_=f[0][:], func=AF.Sigmoid, scale=1.702), reads=["ln_f0"], writes=["ln_f1"])
                                S.op("dve", lambda e, psl=psl, ex=ex, c=c: e.tensor_scalar(out=f[2][:], in0=psl[:], scalar1=bup[:, ex, 6 + c:7 + c], scalar2=7.0, op0=ALU.add, op1=ALU.min), reads=[pkl, "bup"], writes=["ln_f2"])
                                S.op("dve", lambda e: e.tensor_scalar(out=f[2][:], in0=f[2][:], scalar1=-7.0, scalar2=1.0, op0=ALU.max, op1=ALU.add), reads=["ln_f2"], writes=["ln_f2"])
                                S.op("pool", lambda e: e.tensor_tensor(out=f[0][:], in0=f[0][:], in1=f[1][:], op=ALU.mult), reads=["ln_f0", "ln_f1"], writes=["ln_f0"])
                                S.op("pool", lambda e: e.tensor_tensor(out=f[0][:], in0=f[0][:], in1=f[2][:], op=ALU.mult), reads=["ln_f0", "ln_f2"], writes=["ln_f0"])
                                S.op("dve", lambda e, c=c, tsl=tsl: e.tensor_tensor(out=gact[:, c, tsl], in0=f[0][:], in1=gbc[:, tsl], op=ALU.mult), reads=["ln_f0", "gbc"], writes=[("gact", c, t)])
                    for s in range(8):
                        dslot, dk = self.load_slab(W["w_down"][ex], EDIM, s * 256)
                        for jj in range(2):
                            j = s * 2 + jj
                            for t in range(2):
                                tsl = slice(t * 512, (t + 1) * 512)
                                ps, pk = self.next_ps()
                                self.mm_acc(ps, pk, dslot, dk, 6, jj, lambda kc, tsl=tsl, t=t: (gact[:, kc, tsl], [("gact", kc, t)]))
                                S.op("dve", lambda e, ps=ps, j=j, tsl=tsl: e.tensor_tensor(out=acc[:, j, tsl], in0=acc[:, j, tsl], in1=ps[:], op=ALU.add), reads=[pk, ("acc", j, t)], writes=[("acc", j, t)])
                for j in range(KC):
                    for t in range(2):
                        tsl = slice(t * 512, (t + 1) * 512)
                        ps, pk = self.next_ps()
                        S.op("pe", lambda e, ps=ps, j=j, tsl=tsl: e.matmul(ps[:], lhsT=bdb[:, j * 128:(j + 1) * 128], rhs=GTb[:, tsl], start=True, stop=True), reads=["bdb", "GTb"], writes=[pk])
                        S.op("dve", lambda e, ps=ps, j=j, tsl=tsl: e.tensor_tensor(out=acc[:, j, tsl], in0=acc[:, j, tsl], in1=ps[:], op=ALU.add), reads=[pk, ("acc", j, t)], writes=[("acc", j, t)])
                self.ln_fm(acc, ybuf, tm, l2g, l2b, hf, False, True)
        S.barrier()

    def gemm_n(self, Wap, K, col0, ncols, in_fn, ntt, epi, n, slab=256):
        S = self.S
        kc_n = K // 128
        per = slab // 128
        for s in range(ncols // slab):
            si = self.wsn % len(self.wslots)
            self.wsn += 1
            slot = self.wslots[si]
            skey = ("wslot", si)
            src = Wap[:, col0 + s * slab: col0 + (s + 1) * slab].rearrange("(kc p) n -> p kc n", p=128)
            S.op("pool", lambda e, slot=slot, src=src: e.dma_start(out=slot[:, 0:kc_n, 0:slab], in_=src), writes=[skey], dma=True)
            for jj in range(per):
                j = s * per + jj
                for t in range(ntt):
                    ps, pk = self.next_ps()
                    for kc in range(kc_n):
                        rhs, rkeys = in_fn(kc, t)
                        S.op("pe", lambda e, ps=ps, slot=slot, kc=kc, jj=jj, rhs=rhs: e.matmul(
                            ps[:, 0:n], lhsT=slot[:, kc, jj * 128:(jj + 1) * 128], rhs=rhs, start=(kc == 0), stop=(kc == kc_n - 1)),
                            reads=[skey] + rkeys, writes=[pk])
                    epi(j, t, ps, pk)


def make_in_maps(inputs, nlayers, used=None):
    f = lambda a: np.ascontiguousarray(np.asarray(a, dtype=np.float32))
    x = f(inputs["x"]).reshape(8, T, D)
    mem = f(inputs["mem"])
    ln_in = np.stack([f(inputs["ln_in_g"]), f(inputs["ln_in_b"])], 0)
    shared = {}
    for l in range(nlayers):
        for n in WNAMES:
            if used is not None and ("%s_%d" % (n, l)) not in used:
                continue
            if n == "sgu_wT":
                a = np.ascontiguousarray(np.swapaxes(f(inputs["sgu_w"][l]), 1, 2))
            else:
                a = f(inputs[n][l])
            shared["%s_%d" % (n, l)] = np.ascontiguousarray(a.reshape(WSHAPES[n]))
    maps = []
    for c in range(8):
        m = dict(shared)
        m["x"] = x[c]
        m["mem"] = mem[c // 4]
        m["ln_in"] = ln_in
        m["ctab"] = make_ctab(c)
        if used is None or "rot" in used:
            m["rot"] = make_rot(c)
        maps.append(m)
    return maps


_CACHE = {}


def _get_prog(mode, first, last):
    key = (mode, first, last)
    if key not in _CACHE:
        b = Builder(1, mode=mode, first=first, last=last)
        nc = b.build()
        _CACHE[key] = (nc, set(b.used_w))
    return _CACHE[key]


def _layer_weights(inputs, l, used):
    f = lambda a: np.ascontiguousarray(np.asarray(a, dtype=np.float32))
    out = {}
    for n in WNAMES:
        nm = "%s_0" % n
        if nm not in used:
            continue
        if n == "sgu_wT":
            a = np.ascontiguousarray(np.swapaxes(f(inputs["sgu_w"][l]), 1, 2))
        else:
            a = f(inputs[n][l])
        out[nm] = np.ascontiguousarray(a.reshape(WSHAPES[n]))
    return out


def kernel(**inputs):
    f = lambda a: np.ascontiguousarray(np.asarray(a, dtype=np.float32))
    x = f(inputs["x"]).reshape(8, T, D)
    mem = f(inputs["mem"])
    ln_in = np.stack([f(inputs["ln_in_g"]), f(inputs["ln_in_b"])], 0)
    ctabs = [make_ctab(c) for c in range(8)]
    rots = [make_rot(c) for c in range(8)]
    cores = list(range(8))
    hTd = hres = None
    out = None
    for l in range(DEPTH):
        first, last = (l == 0), (l == DEPTH - 1)
        ncA, usedA = _get_prog("A", first, False)
        wA = _layer_weights(inputs, l, usedA)
        mapsA = []
        for c in cores:
            m = dict(wA)
            m["ctab"] = ctabs[c]
            if "rot" in usedA:
                m["rot"] = rots[c]
            if first:
                m["x"] = x[c]
                m["ln_in"] = ln_in
            else:
                m["hTd_i"] = hTd[c]
            mapsA.append(m)
        rA = run_bass_kernel_spmd(ncA, mapsA, core_ids=cores).results
        if first:
            hTd = [rA[c]["hTd_o"] for c in cores]
            hres = [rA[c]["hres_o"] for c in cores]
        gath = np.ascontiguousarray(np.concatenate([np.asarray(rA[c]["bounce_o"]) for c in cores], 0))
        ncB, usedB = _get_prog("B", False, last)
        wB = _layer_weights(inputs, l, usedB)
        mapsB = []
        for c in cores:
            m = dict(wB)
            m["ctab"] = ctabs[c]
            if "rot" in usedB:
                m["rot"] = rots[c]
            m["mem"] = mem[c // 4]
            m["hTd_i"] = hTd[c]
            m["hres_i"] = hres[c]
            m["aTd_i"] = rA[c]["aTd_o"]
            m["gath_i"] = gath
            mapsB.append(m)
        rB = run_bass_kernel_spmd(ncB, mapsB, core_ids=cores).results
        hTd = [rB[c]["hTd_o"] for c in cores]
        hres = [rB[c]["hres_o"] for c in cores]
        if last:
            out = np.stack([np.asarray(rB[c]["out"], dtype=np.float32) for c in cores], 0)
    return out.reshape(2, SEQ, D)
```
